# Optimizing a Trainium2 kernel written in Bass

```python
import functools
import jax, jax.numpy as jnp
from jax import lax
import numpy as np

D_MODEL = 1024
BATCH = 16
SEQ = 2048
DEPTH = 1
DEC_BATCH = 128
DEC_SEQ = 1
PAST_LEN = 16384
PAGE_SIZE = 128

HEAD_DIM = 64
SWA_WINDOW = 128
SWA_Q_HEADS = 8
SWA_KV_HEADS = 2
SWA_GROUP = SWA_Q_HEADS // SWA_KV_HEADS
DIL_PAIRS = ((128, 1), (512, 4), (2048, 16))
N_DIL = len(DIL_PAIRS)
DIL_HEADS = 4
BLOCK = 128
Q_A = SWA_Q_HEADS * HEAD_DIM
KV_A = SWA_KV_HEADS * HEAD_DIM
QKV_B = N_DIL * DIL_HEADS * HEAD_DIM
O_B = DIL_HEADS * HEAD_DIM
N_IN = Q_A + 2 * KV_A + 3 * QKV_B + 2 * D_MODEL
N_EXPERTS = 64
TOP_K = 8
N_EXPERT_GROUPS = 8
EXPERTS_PER_GROUP = N_EXPERTS // N_EXPERT_GROUPS
TOPK_GROUPS = 4
D_EXPERT = 256
D_SHARED = 256
ROUTED_SCALE = 2.5
MOE_BLOCK = 128
LN_EPS = 1e-5
NEG_INF = -1e30
ALPHA = (2.0 * DEPTH) ** 0.25
BETA = (8.0 * DEPTH) ** -0.25

kernel_name = 'hybrid_swa_dilated_moe_decoder_step'


def _alibi_slopes(n):
    return jnp.asarray(2.0 ** (-8.0 * np.arange(1, n + 1) / n), dtype=jnp.float32)


def _layer_norm(x, gain=None, bias=None):
    xf = x.astype(jnp.float32)
    mu = xf.mean(-1, keepdims=True)
    var = jnp.square(xf - mu).mean(-1, keepdims=True)
    y = (xf - mu) * lax.rsqrt(var + LN_EPS)
    if gain is not None:
        y = y * gain.astype(jnp.float32) + bias.astype(jnp.float32)
    return y.astype(x.dtype)


def _adaln(c, w_ada, b_ada):
    mod = jax.nn.silu(c) @ w_ada + b_ada
    return tuple(jnp.split(mod[:, None, :], 6, axis=-1))


def _modulate(x, shift, scale):
    return _layer_norm(x) * (1 + scale) + shift


def _masked_softmax(s, valid, sink):
    s = jnp.where(valid, s, NEG_INF)
    m = s.max(-1)
    if sink is not None:
        m = jnp.maximum(m, sink[..., None])
    p = jnp.exp(s - m[..., None])
    denom = p.sum(-1)
    if sink is not None:
        denom = denom + jnp.exp(sink[..., None] - m)
    return p, denom, m + jnp.log(denom)


def _banded_window_attention(q, k, v, span, step, slopes, sink):
    B, L, Hk, G, Dh = q.shape
    nb = -(-L // BLOCK)
    pad = nb * BLOCK - L
    qb = jnp.pad(q, ((0, 0), (0, pad), (0, 0), (0, 0), (0, 0))).reshape(B, nb, BLOCK, Hk, G, Dh)
    kp = jnp.pad(k, ((0, 0), (BLOCK, pad), (0, 0), (0, 0))).reshape(B, nb + 1, BLOCK, Hk, Dh)
    vp = jnp.pad(v, ((0, 0), (BLOCK, pad), (0, 0), (0, 0))).reshape(B, nb + 1, BLOCK, Hk, Dh)
    kband = jnp.concatenate([kp[:, :-1], kp[:, 1:]], axis=2)
    vband = jnp.concatenate([vp[:, :-1], vp[:, 1:]], axis=2)
    s = jnp.einsum('bnqhgd,bnkhd->bnhgqk', qb, kband, preferred_element_type=jnp.float32) * Dh ** -0.5
    rel = (jnp.arange(BLOCK)[:, None] + BLOCK) - jnp.arange(2 * BLOCK)[None, :]
    kpos = (jnp.arange(nb)[:, None] - 1) * BLOCK + jnp.arange(2 * BLOCK)[None, :]
    valid = (rel >= 0) & (rel <= span) & (kpos >= 0)[:, None, :]
    s = s - slopes[:, :, None, None] * (step * rel).astype(jnp.float32)
    p, denom, lse = _masked_softmax(s, valid[None, :, None, None], sink)
    o = jnp.einsum('bnhgqk,bnkhd->bnqhgd', p, vband.astype(jnp.float32))
    o = o / jnp.moveaxis(denom, -1, 2)[..., None]
    o = o.reshape(B, nb * BLOCK, Hk, G, Dh)[:, :L].astype(q.dtype)
    lse = jnp.moveaxis(lse, -1, 2).reshape(B, nb * BLOCK, Hk, G)[:, :L]
    return o, lse


def _gathered_window_attention(q, k_all, v_all, q_pos, k_start, n_keys, step, slopes, sink):
    Dh = q.shape[-1]
    dist = step * jnp.arange(n_keys)
    idx = q_pos[:, None] - dist[None, :] - k_start
    valid = idx >= 0
    idx = jnp.maximum(idx, 0)
    kg = jnp.take(k_all, idx, axis=1)
    vg = jnp.take(v_all, idx, axis=1)
    s = jnp.einsum('bqhgd,bqkhd->bhgqk', q, kg, preferred_element_type=jnp.float32) * Dh ** -0.5
    s = s - slopes[:, :, None, None] * dist.astype(jnp.float32)
    p, denom, lse = _masked_softmax(s, valid, sink)
    o = jnp.einsum('bhgqk,bqkhd->bqhgd', p, vg.astype(jnp.float32)) / jnp.moveaxis(denom, -1, 1)[..., None]
    return o.astype(q.dtype), jnp.moveaxis(lse, -1, 1)


def _to_residues(t, dil):
    B, S = t.shape[:2]
    rest = t.shape[2:]
    return t.reshape(B, S // dil, dil, *rest).swapaxes(1, 2).reshape(B * dil, S // dil, *rest)


def _from_residues(t, dil):
    Bd, L = t.shape[:2]
    rest = t.shape[2:]
    return t.reshape(Bd // dil, dil, L, *rest).swapaxes(1, 2).reshape(Bd // dil, L * dil, *rest)


def _combine_by_denominator(outs, lses):
    w = jax.nn.softmax(jnp.stack(lses), axis=0)
    o = jnp.einsum('gbsh,gbshd->bshd', w, jnp.stack(outs).astype(jnp.float32))
    return o.astype(outs[0].dtype)


def _prompt_attend(qa, ka, va, qb, kb, vb, sinks):
    S = qa.shape[1]
    o_swa, _ = _banded_window_attention(qa, ka, va, SWA_WINDOW, 1,
                                        _alibi_slopes(SWA_Q_HEADS).reshape(SWA_KV_HEADS, SWA_GROUP), sinks)
    keep = min(SWA_WINDOW, S)
    states = [jnp.stack([ka[:, S - keep:], va[:, S - keep:]], axis=2)]
    slopes = _alibi_slopes(N_DIL * DIL_HEADS).reshape(N_DIL, DIL_HEADS)
    outs, lses = [], []
    for g, (win, dil) in enumerate(DIL_PAIRS):
        qg = _to_residues(qb[:, :, g, :, None], dil)
        kg = _to_residues(kb[:, :, g], dil)
        vg = _to_residues(vb[:, :, g], dil)
        o, lse = _banded_window_attention(qg, kg, vg, win // dil, dil, slopes[g][:, None], None)
        outs.append(_from_residues(o[:, :, :, 0], dil))
        lses.append(_from_residues(lse[..., 0], dil))
        keep = min(win, S)
        states.append(jnp.stack([kb[:, S - keep:, g], vb[:, S - keep:, g]], axis=2))
    return o_swa, _combine_by_denominator(outs, lses), states


def _sample_window(q, cache_kv, k_new, v_new, win, dil, slopes, sink):
    L_buf = cache_kv.shape[1]
    n = q.shape[1]
    k_all = jnp.concatenate([cache_kv[:, :, 0], k_new], axis=1)
    v_all = jnp.concatenate([cache_kv[:, :, 1], v_new], axis=1)
    q_pos = PAST_LEN + jnp.arange(n)
    o, lse = _gathered_window_attention(q, k_all, v_all, q_pos, PAST_LEN - L_buf, win // dil + 1, dil, slopes, sink)
    keep = min(win, PAST_LEN + n)
    return o, lse, jnp.stack([k_all[:, -keep:], v_all[:, -keep:]], axis=2)


def _sample_attend(qa, ka, va, qb, kb, vb, sinks, cache_swa, cache_dils):
    o_swa, _, st = _sample_window(qa, cache_swa, ka, va, SWA_WINDOW, 1,
                                  _alibi_slopes(SWA_Q_HEADS).reshape(SWA_KV_HEADS, SWA_GROUP), sinks)
    states = [st]
    slopes = _alibi_slopes(N_DIL * DIL_HEADS).reshape(N_DIL, DIL_HEADS)
    outs, lses = [], []
    for g, (win, dil) in enumerate(DIL_PAIRS):
        o, lse, st = _sample_window(qb[:, :, g, :, None], cache_dils[g], kb[:, :, g], vb[:, :, g],
                                    win, dil, slopes[g][:, None], None)
        outs.append(o[:, :, :, 0])
        lses.append(lse[..., 0])
        states.append(st)
    return o_swa, _combine_by_denominator(outs, lses), states


def _project_in(h, w_in):
    B, S = h.shape[:2]
    z = h @ w_in
    sizes = (Q_A, KV_A, KV_A, QKV_B, QKV_B, QKV_B, D_MODEL, D_MODEL)
    offs, acc = [], 0
    for sz in sizes[:-1]:
        acc += sz
        offs.append(acc)
    qa, ka, va, qb, kb, vb, ga, gb = jnp.split(z, offs, axis=-1)
    qa = qa.reshape(B, S, SWA_KV_HEADS, SWA_GROUP, HEAD_DIM)
    ka = ka.reshape(B, S, SWA_KV_HEADS, HEAD_DIM)
    va = va.reshape(B, S, SWA_KV_HEADS, HEAD_DIM)
    qb = qb.reshape(B, S, N_DIL, DIL_HEADS, HEAD_DIM)
    kb = kb.reshape(B, S, N_DIL, DIL_HEADS, HEAD_DIM)
    vb = vb.reshape(B, S, N_DIL, DIL_HEADS, HEAD_DIM)
    return qa, ka, va, qb, kb, vb, ga, gb


def _merge(o_swa, o_dil, ga, gb, w_br_swa, w_br_dil, w_out):
    B, S = o_swa.shape[:2]
    y_a = o_swa.reshape(B, S, Q_A) @ w_br_swa
    y_b = o_dil.reshape(B, S, O_B) @ w_br_dil
    return (jax.nn.sigmoid(ga) * y_a + jax.nn.sigmoid(gb) * y_b) @ w_out


def _route(t, w_router, router_bias):
    T = t.shape[0]
    scores = jax.nn.sigmoid(jnp.matmul(t, w_router, preferred_element_type=jnp.float32))
    biased = scores + router_bias.astype(jnp.float32)
    group_score = lax.top_k(biased.reshape(T, N_EXPERT_GROUPS, EXPERTS_PER_GROUP), 2)[0].sum(-1)
    _, top_groups = lax.top_k(group_score, TOPK_GROUPS)
    group_mask = jax.nn.one_hot(top_groups, N_EXPERT_GROUPS, dtype=jnp.float32).sum(1) > 0
    expert_mask = jnp.repeat(group_mask, EXPERTS_PER_GROUP, axis=1)
    _, idx = lax.top_k(jnp.where(expert_mask, biased, -jnp.inf), TOP_K)
    w = jnp.take_along_axis(scores, idx, axis=1)
    return idx, w / w.sum(-1, keepdims=True) * ROUTED_SCALE


def _routed_experts(h, idx, wts, w_gate, w_up, w_down):
    T, D = h.shape
    A = T * TOP_K
    e_flat = idx.reshape(A)
    tok_flat = jnp.arange(A, dtype=jnp.int32) // TOP_K
    order = jnp.argsort(e_flat)
    e_sorted = e_flat[order]
    counts = jnp.zeros((N_EXPERTS,), jnp.int32).at[e_flat].add(1)
    starts = jnp.cumsum(counts) - counts
    padded = (counts + MOE_BLOCK - 1) // MOE_BLOCK * MOE_BLOCK
    pends = jnp.cumsum(padded)
    dest = (pends - padded)[e_sorted] + jnp.arange(A) - starts[e_sorted]
    n_blocks = -(-A // MOE_BLOCK) + N_EXPERTS
    P = n_blocks * MOE_BLOCK
    slot_tok = jnp.full((P,), T, jnp.int32).at[dest].set(tok_flat[order])
    slot_w = jnp.zeros((P,), jnp.float32).at[dest].set(wts.reshape(A)[order])
    block_e = jnp.minimum(jnp.searchsorted(pends, jnp.arange(n_blocks) * MOE_BLOCK, side='right'), N_EXPERTS - 1)
    h_pad = jnp.concatenate([h, jnp.zeros((1, D), h.dtype)], axis=0)

    def run_block(args):
        tok, e, wb = args
        xb = h_pad[tok]
        a = jax.nn.silu(xb @ w_gate[e]) * (xb @ w_up[e])
        return (a @ w_down[e]) * wb[:, None]

    out = lax.map(run_block, (slot_tok.reshape(n_blocks, MOE_BLOCK), block_e,
                              slot_w.reshape(n_blocks, MOE_BLOCK).astype(h.dtype)))
    return jax.ops.segment_sum(out.reshape(P, D), slot_tok, num_segments=T + 1)[:T]


def _moe(h, w_router, router_bias, w_exp_gate, w_exp_up, w_exp_down, w_sh_gate, w_sh_up, w_sh_down):
    B, S, D = h.shape
    t = h.reshape(B * S, D)
    idx, wts = _route(t, w_router, router_bias)
    y = _routed_experts(t, idx, wts, w_exp_gate, w_exp_up, w_exp_down)
    y = y + (jax.nn.silu(t @ w_sh_gate) * (t @ w_sh_up)) @ w_sh_down
    return y.reshape(B, S, D)


def _trunk_layer(x, c, attend, w_ada, b_ada, w_in, w_br_swa, w_br_dil, w_out, ln1_g, ln1_b,
                 w_router, router_bias, w_exp_gate, w_exp_up, w_exp_down, w_sh_gate, w_sh_up, w_sh_down,
                 ln2_g, ln2_b):
    shift1, scale1, gate1, shift2, scale2, gate2 = _adaln(c, w_ada, b_ada)
    qa, ka, va, qb, kb, vb, ga, gb = _project_in(_modulate(x, shift1, scale1), w_in)
    o_swa, o_dil, states = attend(qa, ka, va, qb, kb, vb)
    mix = _merge(o_swa, o_dil, ga, gb, w_br_swa, w_br_dil, w_out)
    x = _layer_norm(ALPHA * x + gate1 * mix, ln1_g, ln1_b)
    ffn = _moe(_modulate(x, shift2, scale2), w_router, router_bias, w_exp_gate, w_exp_up, w_exp_down,
               w_sh_gate, w_sh_up, w_sh_down)
    x = _layer_norm(ALPHA * x + gate2 * ffn, ln2_g, ln2_b)
    return x, states


def setup_inputs(seed: int = 0) -> dict:
    key = jax.random.key(seed)
    ks = jax.random.split(key, 27)
    f32 = jnp.float32

    def nrm(k, shape, scale):
        return jax.random.normal(k, shape, f32) * scale

    def kv_buf(k, win, heads):
        return nrm(k, (DEPTH, DEC_BATCH, min(win, PAST_LEN), 2, heads, HEAD_DIM), 1.0)

    v_scale = jnp.concatenate([jnp.ones((Q_A + KV_A,), f32), jnp.full((KV_A,), BETA, f32),
                               jnp.ones((2 * QKV_B,), f32), jnp.full((QKV_B,), BETA, f32),
                               jnp.ones((2 * D_MODEL,), f32)])
    return {
        'x_prompt': nrm(ks[0], (BATCH, SEQ, D_MODEL), 1.0),
        'x_sample': nrm(ks[1], (DEC_BATCH, DEC_SEQ, D_MODEL), 1.0),
        'cache_swa_kv': kv_buf(ks[2], SWA_WINDOW, SWA_KV_HEADS),
        'cache_dil1_kv': kv_buf(ks[3], DIL_PAIRS[0][0], DIL_HEADS),
        'cache_dil2_kv': kv_buf(ks[4], DIL_PAIRS[1][0], DIL_HEADS),
        'cache_dil3_kv': kv_buf(ks[5], DIL_PAIRS[2][0], DIL_HEADS),
        'c_prompt': nrm(ks[6], (BATCH, D_MODEL), 1.0),
        'c_sample': nrm(ks[7], (DEC_BATCH, D_MODEL), 1.0),
        'w_ada': nrm(ks[8], (DEPTH, D_MODEL, 6 * D_MODEL), 0.5 * D_MODEL ** -0.5),
        'b_ada': nrm(ks[9], (DEPTH, 6 * D_MODEL), 0.02),
        'w_in': nrm(ks[10], (DEPTH, D_MODEL, N_IN), D_MODEL ** -0.5) * v_scale,
        'attn_sinks': nrm(ks[11], (DEPTH, SWA_Q_HEADS), 0.5),
        'w_br_swa': nrm(ks[12], (DEPTH, Q_A, D_MODEL), Q_A ** -0.5),
        'w_br_dil': nrm(ks[13], (DEPTH, O_B, D_MODEL), O_B ** -0.5),
        'w_out': nrm(ks[14], (DEPTH, D_MODEL, D_MODEL), BETA * D_MODEL ** -0.5),
        'ln1_g': 1.0 + nrm(ks[15], (DEPTH, D_MODEL), 0.02),
        'ln1_b': nrm(ks[16], (DEPTH, D_MODEL), 0.02),
        'w_router': nrm(ks[17], (DEPTH, D_MODEL, N_EXPERTS), D_MODEL ** -0.5),
        'router_bias': nrm(ks[18], (DEPTH, N_EXPERTS), 0.01),
        'w_exp_gate': nrm(ks[19], (DEPTH, N_EXPERTS, D_MODEL, D_EXPERT), D_MODEL ** -0.5),
        'w_exp_up': nrm(ks[20], (DEPTH, N_EXPERTS, D_MODEL, D_EXPERT), D_MODEL ** -0.5),
        'w_exp_down': nrm(ks[21], (DEPTH, N_EXPERTS, D_EXPERT, D_MODEL), BETA * D_EXPERT ** -0.5),
        'w_sh_gate': nrm(ks[22], (DEPTH, D_MODEL, D_SHARED), D_MODEL ** -0.5),
        'w_sh_up': nrm(ks[23], (DEPTH, D_MODEL, D_SHARED), D_MODEL ** -0.5),
        'w_sh_down': nrm(ks[24], (DEPTH, D_SHARED, D_MODEL), BETA * D_SHARED ** -0.5),
        'ln2_g': 1.0 + nrm(ks[25], (DEPTH, D_MODEL), 0.02),
        'ln2_b': nrm(ks[26], (DEPTH, D_MODEL), 0.02),
    }


def reference(x_prompt, x_sample, cache_swa_kv, cache_dil1_kv, cache_dil2_kv, cache_dil3_kv, c_prompt, c_sample,
              w_ada, b_ada, w_in, attn_sinks, w_br_swa, w_br_dil, w_out, ln1_g, ln1_b, w_router, router_bias,
              w_exp_gate, w_exp_up, w_exp_down, w_sh_gate, w_sh_up, w_sh_down, ln2_g, ln2_b):
    xp, xs = x_prompt, x_sample
    p_states, s_states = [], []
    for l in range(DEPTH):
        sinks = attn_sinks[l].reshape(SWA_KV_HEADS, SWA_GROUP).astype(jnp.float32)
        w_l = (w_ada[l], b_ada[l], w_in[l], w_br_swa[l], w_br_dil[l], w_out[l], ln1_g[l], ln1_b[l],
               w_router[l], router_bias[l], w_exp_gate[l], w_exp_up[l], w_exp_down[l],
               w_sh_gate[l], w_sh_up[l], w_sh_down[l], ln2_g[l], ln2_b[l])
        xp, st_p = _trunk_layer(xp, c_prompt, functools.partial(_prompt_attend, sinks=sinks), *w_l)
        attend_s = functools.partial(_sample_attend, sinks=sinks, cache_swa=cache_swa_kv[l],
                                     cache_dils=(cache_dil1_kv[l], cache_dil2_kv[l], cache_dil3_kv[l]))
        xs, st_s = _trunk_layer(xs, c_sample, attend_s, *w_l)
        p_states.append(st_p)
        s_states.append(st_s)
    swa_p = jnp.stack([s[0] for s in p_states])
    dil1_p = jnp.stack([s[1] for s in p_states])
    dil2_p = jnp.stack([s[2] for s in p_states])
    dil3_p = jnp.stack([s[3] for s in p_states])
    swa_s = jnp.stack([s[0] for s in s_states])
    dil1_s = jnp.stack([s[1] for s in s_states])
    dil2_s = jnp.stack([s[2] for s in s_states])
    dil3_s = jnp.stack([s[3] for s in s_states])
    return (xp, xs, swa_p, dil1_p, dil2_p, dil3_p, swa_s, dil1_s, dil2_s, dil3_s)
```

```python
import math
from contextlib import ExitStack

import numpy as np
import concourse.bass as bass
import concourse.mybir as mybir
from concourse.bass_utils import run_bass_kernel_spmd

F32 = mybir.dt.float32
BF16 = mybir.dt.bfloat16
I32 = mybir.dt.int32
AF = mybir.ActivationFunctionType
ALU = mybir.AluOpType
AX = mybir.AxisListType

NCORES = 8
D = 1024
SEQ = 2048
NSAMP = 16
TC = SEQ + NSAMP
NEXP = 64
ALPHA = 2.0 ** 0.25
LN_EPS = 1e-5
NEGBIG = -30000.0
import os
SAME_ENGINE_SYNC = os.environ.get('SES', '0') == '1'
NS = 8

TYPES = [
    dict(d=1, hq=8, hk=2, qo=0, ko=512, vo=640, W=128),
    dict(d=1, hq=4, hk=4, qo=768, ko=1536, vo=2304, W=128),
    dict(d=4, hq=4, hk=4, qo=1024, ko=1792, vo=2560, W=512),
    dict(d=16, hq=4, hk=4, qo=1280, ko=2048, vo=2816, W=2048),
]


def slopes_for(a):
    if a == 0:
        return [2.0 ** (-8.0 * (i + 1) / 8) for i in range(8)]
    g = a - 1
    return [2.0 ** (-8.0 * (4 * g + i + 1) / 12) for i in range(4)]


class Rec:
    def __init__(self):
        self.ops = []
        self.alias = {}
        self.stopped = False

    @staticmethod
    def _excl(r, w):
        r, w = list(r), list(w)
        w = w + [k for k in r if k.startswith('ps') and k not in w]
        r = [k for k in r if not k.startswith('ps')]
        return r, w

    def op(self, eng, fn, r=(), w=()):
        if self.stopped:
            return
        r, w = self._excl(r, w)
        self.ops.append(dict(eng=eng, fn=fn, r=tuple(r), w=tuple(w), dma=False, bulk=False))

    def dma(self, q, fn, r=(), w=(), bulk=False):
        if self.stopped:
            return
        r, w = self._excl(r, w)
        self.ops.append(dict(eng=q, fn=fn, r=tuple(r), w=tuple(w), dma=True, bulk=bulk))

    def emit(self, nc, stack):
        ops = self.ops
        last_w = {}
        readers = {}
        dreaders = {}
        by_base = {}
        def conf(k):
            b = k.split(':')[0]
            if k == b:
                return list(by_base.get(b, ())) + [k]
            return [k, b]

        for i, op in enumerate(ops):
            deps = set()
            for k in op['r']:
                for kk in conf(k):
                    if kk in last_w:
                        deps.add(last_w[kk])
            raw = set(deps)
            for k in op['w']:
                ks = conf(k)
                base = k.split(':')[0]
                for ob in self.alias.get(base, ()):
                    ks.extend(by_base.get(ob, ()))
                    ks.append(ob)
                for kk in ks:
                    if kk in last_w:
                        deps.add(last_w[kk])
                    deps.update(readers.get(kk, {}).values())
                    deps.update(dreaders.get(kk, ()))
            deps.discard(i)
            fdeps = set()
            for j in deps:
                oj = ops[j]
                if (not oj['dma']) and (not op['dma']) and oj['eng'] == op['eng']:
                    if op['eng'] == 'pe' or (op['eng'] != 'pool' and not SAME_ENGINE_SYNC and j not in raw):
                        continue
                fdeps.add(j)
            op['deps'] = fdeps
            for k in op['r']:
                by_base.setdefault(k.split(':')[0], set()).add(k)
                if op['dma']:
                    dreaders.setdefault(k, []).append(i)
                else:
                    readers.setdefault(k, {})[op['eng']] = i
            for k in op['w']:
                by_base.setdefault(k.split(':')[0], set()).add(k)
                last_w[k] = i
                readers[k] = {}
                dreaders[k] = []
        fin = ops[-1]
        lastc = {}
        lastd = {}
        for i, op in enumerate(ops[:-1]):
            if op['dma']:
                lastd.setdefault(op['eng'], []).append(i)
            else:
                lastc[op['eng']] = i
        fin['deps'].update(lastc.values())
        for q, lst in lastd.items():
            fin['deps'].update(lst[-NS:])
        needs = set()
        for op in ops:
            needs.update(op['deps'])
        engs = ['pe', 'act', 'dve', 'pool', 'sp']
        esem = {e: stack.enter_context(nc.semaphore('es_' + e)) for e in engs}
        dsem = {q: [stack.enter_context(nc.semaphore('ds_%s%d' % (q, i))) for i in range(NS)]
                for q in ['sp', 'act', 'pool']}
        bsem = stack.enter_context(nc.semaphore('bulk'))
        cnt = {e: 0 for e in engs}
        dcnt = {q: 0 for q in dsem}
        nbulk = sum(1 for op in ops if op['bulk'])
        for i, op in enumerate(ops):
            if op['bulk']:
                op['sig'] = (bsem, 16 * nbulk)
                op['pre'] = None
            elif op['dma']:
                q = op['eng']
                k = dcnt[q]
                dcnt[q] += 1
                op['sig'] = (dsem[q][k % NS], 16 * (k // NS + 1))
                op['pre'] = (dsem[q][k % NS], 16 * (k // NS)) if k >= NS else None
            elif i in needs:
                cnt[op['eng']] += 1
                op['sig'] = (esem[op['eng']], cnt[op['eng']])
            else:
                op['sig'] = None
        self.maxcnt = dict(cnt)
        block = stack.enter_context(nc.Block())

        def run(ename, eng):
            waited = {}
            for op in ops:
                if op['eng'] != ename:
                    continue
                ws = {}
                for j in op['deps']:
                    s, v = ops[j]['sig']
                    ws[id(s)] = (s, max(v, ws.get(id(s), (s, 0))[1]))
                if op['dma'] and op['pre'] is not None:
                    s, v = op['pre']
                    ws[id(s)] = (s, max(v, ws.get(id(s), (s, 0))[1]))
                for sid, (s, v) in ws.items():
                    if waited.get(sid, 0) >= v:
                        continue
                    eng.wait_ge(s, v)
                    waited[sid] = v
                inst = op['fn'](eng)
                if op['sig'] is not None and inst is not None:
                    inst.then_inc(op['sig'][0], 16 if op['dma'] else 1)

        @block.tensor
        def _(e):
            run('pe', e)

        @block.scalar
        def _(e):
            run('act', e)

        @block.vector
        def _(e):
            run('dve', e)

        @block.gpsimd
        def _(e):
            run('pool', e)

        @block.sync
        def _(e):
            run('sp', e)


class Buf:
    def __init__(self, key, ap, lo, hi):
        self.key, self.ap, self.lo, self.hi = key, ap, lo, hi

    def k(self, *sub):
        return self.key if not sub else self.key + ':' + '_'.join(str(s) for s in sub)


class Arena:
    def __init__(self, R, arena_ap, nbytes):
        self.R, self.arena, self.nbytes = R, arena_ap, nbytes
        self.live = []
        self.n = 0

    def carve(self, name, off, shape, dt):
        n = 1
        for s in shape[1:]:
            n *= s
        esz = 4 if dt in (F32, I32) else 2
        assert off % 4 == 0 and off + n * esz <= self.nbytes, (name, off, n * esz, self.nbytes)
        P = shape[0]
        if esz == 4:
            a = self.arena[0:P, off // 2: off // 2 + 2 * n].bitcast(dt)
        else:
            a = self.arena[0:P, off // 2: off // 2 + n]
        if len(shape) == 3:
            a = a.rearrange("p (a b) -> p a b", a=shape[1])
        elif len(shape) == 4:
            a = a.rearrange("p (a b c) -> p a b c", a=shape[1], b=shape[2])
        self.n += 1
        key = '%s#%d' % (name, self.n)
        lo, hi = off, off + n * esz
        al = [k for (k, l, h) in self.live if l < hi and lo < h]
        if al:
            self.R.alias[key] = al
        self.live.append((key, lo, hi))
        return Buf(key, a, lo, hi)


class Region:
    def __init__(self, arena, lo, hi):
        self.A, self.lo, self.hi, self.p = arena, lo, hi, lo

    def alloc(self, name, shape, dt):
        n = 1
        for s in shape[1:]:
            n *= s
        esz = 4 if dt in (F32, I32) else 2
        off = (self.p + 31) // 32 * 32
        assert off + n * esz <= self.hi, (name, off, n * esz, self.hi)
        self.p = off + n * esz
        return self.A.carve(name, off, shape, dt)


class _Stop(Exception):
    pass


STAGE = None


def build_nc():
    nc = bass.Bass("TRN2", target_bir_lowering=False)

    def din(name, shape):
        return nc.dram_tensor(name, list(shape), F32, kind="ExternalInput").ap()

    def dout(name, shape):
        return nc.dram_tensor(name, list(shape), F32, kind="ExternalOutput").ap()

    xp = din("xp", [2, SEQ, D])
    xs = din("xs", [NSAMP, D])
    c_all = din("c_all", [18, D])
    caches = [din("cache%d" % a, [NSAMP, T['W'], 2, T['hk'], 64]) for a, T in enumerate(TYPES)]
    w_ada = din("w_ada", [D, 6 * D])
    b_ada = din("b_ada", [1, 6 * D])
    w_in = din("w_in", [D, 5120])
    sinks = din("sinks", [1, 8])
    w_br_swa = din("w_br_swa", [512, D])
    w_br_dil = din("w_br_dil", [256, D])
    w_out = din("w_out", [D, D])
    ln1_g = din("ln1_g", [1, D])
    ln1_b = din("ln1_b", [1, D])
    w_router = din("w_router", [D, NEXP])
    router_bias = din("router_bias", [1, NEXP])
    w_eg = din("w_eg", [NEXP, D, 256])
    w_eu = din("w_eu", [NEXP, D, 256])
    w_ed = din("w_ed", [NEXP, 256, D])
    w_sg = din("w_sg", [D, 256])
    w_su = din("w_su", [D, 256])
    w_sd = din("w_sd", [256, D])
    ln2_g = din("ln2_g", [1, D])
    ln2_b = din("ln2_b", [1, D])

    yp = dout("yp", [2, SEQ, D])
    ys = dout("ys", [NSAMP, D])
    st_p = [dout("stp%d" % a, [2, T['W'], 2, T['hk'], 64]) for a, T in enumerate(TYPES)]
    st_s = [dout("sts%d" % a, [NSAMP, T['W'], 2, T['hk'], 64]) for a, T in enumerate(TYPES)]

    R = Rec()
    outkeys = []
    with ExitStack() as st:
        ARENA_BYTES = 206 * 1024
        arena_t = st.enter_context(nc.sbuf_tensor("arena", [128, ARENA_BYTES // 2], BF16))
        A = Arena(R, arena_t[:], ARENA_BYTES)
        ps = [st.enter_context(nc.psum_tensor("ps%d" % i, [128, 512], F32)) for i in range(8)]
        psb = [p[:].bitcast(BF16) for p in ps]

        def chk(name):
            if STAGE == name:
                R.stopped = True

        def mm(out, lhsT, rhs, start, stop, r, w):
            R.op('pe', lambda e: e.matmul(out, lhsT=lhsT, rhs=rhs, start=start, stop=stop), r, w)

        def tp(out, in_, ident, r, w):
            R.op('pe', lambda e: e.transpose(out=out, in_=in_, identity=ident), r, w)

        def act(out, in_, func, r, w, bias=None, scale=None, accum=None):
            kw = {}
            if bias is not None:
                kw['bias'] = bias
            if scale is not None:
                kw['scale'] = scale
            if accum is not None:
                kw['accum_out'] = accum
            R.op('act', lambda e: e.activation(out=out, in_=in_, func=func, **kw), r, w)

        def V(name, r, w, *args, **kw):
            R.op('dve', lambda e: getattr(e, name)(*args, **kw), r, w)

        def DM(q, out, in_, r=(), w=(), bulk=False):
            R.dma(q, lambda e: e.dma_start(out=out, in_=in_), r, w, bulk)

        def PL(name, r, w, *args, **kw):
            R.op('pool', lambda e: getattr(e, name)(*args, **kw), r, w)

        def evac(i, out, in_, r, w):
            if i % 2 == 0:
                R.op('act', lambda e: e.copy(out=out, in_=in_), r, w)
            else:
                R.op('dve', lambda e: e.tensor_copy(out=out, in_=in_), r, w)

        PS = Region(A, 0, 17408)
        identb = PS.alloc("identb", [128, 128], BF16)
        identf = PS.alloc("identf", [128, 128], F32)
        modT = PS.alloc("modT", [128, 48, 18], F32)
        opsc1T = PS.alloc("opsc1T", [128, 8, 18], F32)
        opsc2T = PS.alloc("opsc2T", [128, 8, 18], F32)
        sink = PS.alloc("sink", [128, 8], F32)
        nsink = PS.alloc("nsink", [128, 8], F32)
        modg = PS.alloc("modg", [18, 2048], F32)
        Gm = PS.alloc("Gm", [128, 128], F32)
        epst = PS.alloc("epst", [128, 1], F32)
        selH = [PS.alloc("selH%d" % c, [4, 128], F32) for c in range(2)]
        selP = [PS.alloc("selP%d" % p, [18, 128], F32) for p in range(2)]
        stat = PS.alloc("stat", [128, 64], F32)
        HT_LO = 17408
        hT = A.carve("hT", HT_LO, [128, 8, TC], BF16)
        R1_LO = HT_LO + 8 * TC * 2
        WORK_LO_A = R1_LO + 66048
        END = ARENA_BYTES

        def hk(c0, c1):
            return [hT.k(g) for g in range(c0 // 512, min((c1 - 1) // 512, 4) + 1)]


        PL('memset', [], [identf.k()], identf.ap, 0.0)
        PL('affine_select', [identf.k()], [identf.k()], out=identf.ap, in_=identf.ap, pattern=[[-1, 128]],
           compare_op=ALU.not_equal, fill=1.0, base=0, channel_multiplier=1)
        V('tensor_copy', [identf.k()], [identb.k()], out=identb.ap, in_=identf.ap)
        V('memset', [], [epst.k()], epst.ap, LN_EPS)
        e16 = stat.ap[:, 0:16]
        V('tensor_reduce', [identf.k()], [stat.k()], out=e16, in_=identf.ap.rearrange("p (kg b) -> p b kg", kg=8),
          axis=AX.X, op=ALU.add)
        V('tensor_copy', [stat.k()], [Gm.k()], out=Gm.ap.rearrange("p (kg b) -> p kg b", kg=8),
          in_=e16.unsqueeze(1).to_broadcast([128, 8, 16]))
        for c in range(2):
            V('tensor_copy', [identf.k()], [selH[c].k()], out=selH[c].ap.rearrange("p (h d) -> p h d", h=2),
              in_=identf.ap[0:4, 2 * c:2 * c + 2].unsqueeze(2).to_broadcast([4, 2, 64]))
        for p in range(2):
            V('tensor_copy', [identf.k()], [selP[p].k()], out=selP[p].ap,
              in_=identf.ap[0:18, 16 + p:17 + p].to_broadcast([18, 128]))
        DM('sp', sink.ap, sinks.to_broadcast([128, 8]), w=[sink.k()])
        V('tensor_scalar', [sink.k()], [nsink.k()], out=nsink.ap, in0=sink.ap, scalar1=-1.0, scalar2=None, op0=ALU.mult)

        B0 = Region(A, R1_LO, END)
        mod_tm = B0.alloc("mod_tm", [18, 6144], F32)
        bada = B0.alloc("bada", [1, 6144], F32)
        wa = [B0.alloc("wa%d" % i, [128, 8, 512], F32) for i in range(2)]
        cs = B0.alloc("cs", [18, 1024], F32)
        scT = B0.alloc("scT", [128, 8, 18], F32)
        ones18 = B0.alloc("ones18", [1, 18], F32)
        DM('sp', cs.ap, c_all[:, :], w=[cs.k()])
        DM('sp', bada.ap, b_ada[:, :], w=[bada.k()])
        V('memset', [], [ones18.k()], ones18.ap, 1.0)
        act(cs.ap, cs.ap, AF.Silu, [cs.k()], [cs.k()])
        for kc in range(8):
            tp(ps[0][:, kc * 18:(kc + 1) * 18], cs.ap[0:18, kc * 128:(kc + 1) * 128], identf.ap[0:18, 0:18],
               [cs.k(), identf.k()], ['ps0'])
        V('tensor_copy', ['ps0'], [scT.k()], out=scT.ap.rearrange("p a b -> p (a b)"), in_=ps[0][:, 0:144])
        for n in range(12):
            wb_ = wa[n % 2]
            DM('sp', wb_.ap, w_ada[:, n * 512:(n + 1) * 512].rearrange("(k p) n -> p k n", p=128), w=[wb_.k()])
            pk = 'ps%d' % (1 + n % 2)
            pt = ps[1 + n % 2]
            for kc in range(8):
                mm(pt[0:18, :], scT.ap[:, kc, :], wb_.ap[:, kc, :], kc == 0, False, [scT.k(), wb_.k()], [pk])
            mm(pt[0:18, :], ones18.ap, bada.ap[0:1, n * 512:(n + 1) * 512], False, True, [ones18.k(), bada.k()], [pk])
            evac(n, mod_tm.ap[:, n * 512:(n + 1) * 512], pt[0:18, :], [pk], [mod_tm.k(n)])
        mt_all = [mod_tm.k(n) for n in range(12)]
        V('tensor_copy', mt_all, [modg.k()], out=modg.ap[:, 0:1024], in_=mod_tm.ap[:, 2048:3072])
        V('tensor_copy', mt_all + [modg.k()], [modg.k()], out=modg.ap[:, 1024:2048], in_=mod_tm.ap[:, 5120:6144])
        for g3 in range(3):
            pk = 'ps%d' % (3 + g3 % 2)
            pt = ps[3 + g3 % 2]
            for j in range(16):
                c = g3 * 16 + j
                tp(pt[:, j * 18:(j + 1) * 18], mod_tm.ap[0:18, c * 128:(c + 1) * 128], identf.ap[0:18, 0:18],
                   mt_all + [identf.k()], [pk])
            V('tensor_copy', [pk], [modT.k()], out=modT.ap[:, g3 * 16:(g3 + 1) * 16, :].rearrange("p a b -> p (a b)"),
              in_=pt[:, 0:288])
        V('tensor_scalar', [modT.k()], [opsc1T.k()], out=opsc1T.ap, in0=modT.ap[:, 8:16, :], scalar1=1.0, scalar2=None,
          op0=ALU.add)
        V('tensor_scalar', [modT.k()], [opsc2T.k()], out=opsc2T.ap, in0=modT.ap[:, 32:40, :], scalar1=1.0, scalar2=None,
          op0=ALU.add)

        stat_slot = [0]

        def ln_stats(xap, P, rkeys):
            s = stat_slot[0] % 2
            stat_slot[0] += 1
            base = 16 + s * 24
            st6 = stat.ap[0:P, base:base + 12]
            mv = stat.ap[0:P, base + 12:base + 14]
            lnv = stat.ap[0:P, base + 14:base + 15]
            rstd = stat.ap[0:P, base + 15:base + 16]
            nmr = stat.ap[0:P, base + 16:base + 17]
            k = stat.k('ln', s)
            V('bn_stats', rkeys, [k], out=st6[:, 0:6], in_=xap[:, 0:512])
            V('bn_stats', rkeys + [k], [k], out=st6[:, 6:12], in_=xap[:, 512:1024])
            V('bn_aggr', [k], [k], out=mv, in_=st6)
            act(lnv, mv[:, 1:2], AF.Ln, [k, epst.k()], [k], bias=epst.ap[0:P, :], scale=1.0)
            act(rstd, lnv, AF.Exp, [k], [k], scale=-0.5)
            V('tensor_scalar', [k], [k], out=nmr, in0=mv[:, 0:1], scalar1=rstd, scalar2=-1.0, op0=ALU.mult, op1=ALU.mult)
            return rstd, nmr, k

        chk('p0')
        for p in range(2):
            has_s = (p == 1)
            ncol = TC if has_s else SEQ
            pcol = 16 + p
            mtiles = [(tt * 512, 512) for tt in range(4)] + ([(SEQ, NSAMP)] if has_s else [])
            stiles = [(t * 128, 128) for t in range(16)] + ([(SEQ, NSAMP)] if has_s else [])

            oT = [A.carve("oT0", R1_LO, [128, 4, TC], BF16)]
            off = R1_LO + 4 * TC * 2
            for g in range(3):
                oT.append(A.carve("oT%d" % (g + 1), off, [128, 2, TC], BF16))
                off += 2 * TC * 2
            lseT = A.carve("lseT", off, [4, 3, TC], F32)
            off += 3 * TC * 4
            assert off == WORK_LO_A

            RA = Region(A, WORK_LO_A, END)
            xt = [RA.alloc("xt%d" % i, [128, 1024], F32) for i in range(2)]
            xnb = [RA.alloc("xnb%d" % i, [128, 1024], BF16) for i in range(4)]
            for g4 in range(4):
                for j in range(4):
                    t = g4 * 4 + j
                    xb_ = xt[t % 2]
                    DM('sp', xb_.ap, xp[p, t * 128:(t + 1) * 128, :], w=[xb_.k()])
                    rstd, nmr, k = ln_stats(xb_.ap, 128, [xb_.k()])
                    chk('Aa')
                    act(xnb[j].ap, xb_.ap, AF.Identity, [xb_.k(), k], [xnb[j].k()], bias=nmr, scale=rstd)
                    chk('Ab')
                for c in range(8):
                    for j in range(4):
                        tp(psb[c][:, j * 128:(j + 1) * 128],
                           xnb[j].ap[:, c * 128:(c + 1) * 128], identb.ap, [xnb[j].k(), identb.k()], ['ps%d' % c])
                chk('Ac')
                for c in range(8):
                    src = psb[c][:, 0:512]
                    dst = hT.ap[:, c, g4 * 512:(g4 + 1) * 512]
                    sc_ = opsc1T.ap[:, c, pcol:pcol + 1]
                    sh_ = modT.ap[:, c, pcol:pcol + 1]
                    rk = ['ps%d' % c, opsc1T.k(), modT.k()]
                    if c % 2 == 0:
                        act(dst, src, AF.Identity, rk, [hT.k(g4)], bias=sh_, scale=sc_)
                    else:
                        V('tensor_scalar', rk, [hT.k(g4)], out=dst, in0=src, scalar1=sc_, scalar2=sh_, op0=ALU.mult, op1=ALU.add)
                chk('Ad')
            if has_s:
                xs_t = xt[0]
                DM('sp', xs_t.ap[0:16, :], xs[:, :], w=[xs_t.k()])
                rstd, nmr, k = ln_stats(xs_t.ap[0:16, :], 16, [xs_t.k()])
                act(xnb[0].ap[0:16, :], xs_t.ap[0:16, :], AF.Identity, [xs_t.k(), k], [xnb[0].k()], bias=nmr, scale=rstd)
                for c in range(8):
                    tp(psb[0][:, c * 16:(c + 1) * 16], xnb[0].ap[0:16, c * 128:(c + 1) * 128], identb.ap[0:16, 0:16],
                       [xnb[0].k(), identb.k()], ['ps0'])
                tmp_s = RA.alloc("tmp_s", [128, 8, 16], F32)
                V('tensor_tensor', ['ps0', opsc1T.k()], [tmp_s.k()], out=tmp_s.ap,
                  in0=psb[0][:, 0:128].rearrange("p (c s) -> p c s", c=8), in1=opsc1T.ap[:, :, 0:16], op=ALU.mult)
                V('tensor_tensor', [tmp_s.k(), modT.k()], [hT.k(4)], out=hT.ap[:, :, SEQ:TC], in0=tmp_s.ap,
                  in1=modT.ap[:, 0:8, 0:16], op=ALU.add)

            chk('A%d' % p)
            for a, T in enumerate(TYPES):
                d, hq, hk_, W = T['d'], T['hq'], T['hk'], T['W']
                nq, nk = hq * 64, hk_ * 64
                nb = 16 // d
                slopes = slopes_for(a)
                RB = Region(A, WORK_LO_A, END)
                wblk = RB.alloc("wblk", [128, 8, 768], BF16)
                nqc = nq // 128
                nkc = 1 if a == 0 else 2
                o_tok = [RB.alloc("o_tok%d" % i, [128, 512], BF16) for i in range(2)]
                lse_tok = [RB.alloc("lse_tok%d" % i, [128, 4], F32) for i in range(2)]
                hst = [RB.alloc("hst%d" % i, [128, 16], F32) for i in range(6)]
                RS_LO = RB.p
                QK = RB.alloc("QK", [128, nqc + nkc, TC], BF16)
                Vt = RB.alloc("Vt", [128, 16, hk_, 65], BF16)
                bias = RB.alloc("bias", [128, hq, 256], F32)
                rv = RB.alloc("rv", [128, 256], F32)
                pen = RB.alloc("pen", [128, 256], F32)
                S_sb = [RB.alloc("S_sb%d" % i, [128, 512], F32) for i in range(3)]
                Pb = [RB.alloc("Pb%d" % i, [128, 512], BF16) for i in range(3)]
                PT = [RB.alloc("PT%d" % i, [128, 512], BF16) for i in range(3)]
                kvst = [RB.alloc("kvst%d" % i, [128, 512], F32) for i in range(2)]

                for (o0, n0, c0) in ((T['qo'], nq, 0), (T['ko'], nk, nq), (T['vo'], nk, nq + nk)):
                    if a == 0 and c0 == 0:
                        for hh in range(8):
                            dc = ((hh % 4) * 2 + hh // 4) * 64
                            DM('pool', wblk.ap[:, :, dc:dc + 64],
                               w_in[:, hh * 64:(hh + 1) * 64].rearrange("(k p) n -> p k n", p=128), w=[wblk.k()])
                        continue
                    DM('pool', wblk.ap[:, :, c0:c0 + n0], w_in[:, o0:o0 + n0].rearrange("(k p) n -> p k n", p=128), w=[wblk.k()])
                ko_, vo_ = nq, nq + nk

                PL('iota', [], [rv.k()], rv.ap, pattern=[[-1, 256]], base=128, channel_multiplier=1,
                   allow_small_or_imprecise_dtypes=True)
                V('tensor_scalar', [rv.k()], [pen.k()], out=pen.ap, in0=rv.ap, scalar1=0.0, scalar2=None, op0=ALU.is_ge)
                V('tensor_scalar', [rv.k()], [S_sb[0].k()], out=S_sb[0].ap[:, 0:256], in0=rv.ap, scalar1=128.0, scalar2=None, op0=ALU.is_le)
                V('tensor_tensor', [pen.k(), S_sb[0].k()], [pen.k()], out=pen.ap, in0=pen.ap, in1=S_sb[0].ap[:, 0:256], op=ALU.mult)
                V('tensor_tensor', [rv.k(), pen.k()], [rv.k()], out=rv.ap, in0=rv.ap, in1=pen.ap, op=ALU.mult)
                V('tensor_scalar', [pen.k()], [pen.k()], out=pen.ap, in0=pen.ap, scalar1=-NEGBIG, scalar2=NEGBIG,
                  op0=ALU.mult, op1=ALU.add)
                for h in range(hq):
                    V('scalar_tensor_tensor', [rv.k(), pen.k()], [bias.k(h)], out=bias.ap[:, h, :], in0=rv.ap,
                      scalar=-slopes[h] * d, in1=pen.ap, op0=ALU.mult, op1=ALU.add)
                V('memset', [], [Vt.k('ones')], Vt.ap[:, :, :, 64:65], 1.0)

                chk('Ba')
                def qk_lhsT(ci, kc):
                    if a == 0:
                        if ci < 4:
                            return wblk.ap[:, kc, ci * 128:(ci + 1) * 128]
                        return wblk.ap[:, kc, 512:640]
                    if ci < 2:
                        return wblk.ap[:, kc, ci * 128:(ci + 1) * 128]
                    return wblk.ap[:, kc, 256 + (ci - 2) * 128:256 + (ci - 1) * 128]

                ei = 0
                for ci in range(nqc + nkc):
                    for (c0, wd) in mtiles:
                        bk = ei % 2
                        for kc in range(8):
                            mm(ps[bk][:, 0:wd], qk_lhsT(ci, kc), hT.ap[:, kc, c0:c0 + wd], kc == 0, kc == 7,
                               [wblk.k()] + hk(c0, c0 + wd), ['ps%d' % bk])
                        evac(ei, QK.ap[:, ci, c0:c0 + wd], ps[bk][:, 0:wd], ['ps%d' % bk], [QK.k(ci, c0 // 512)])
                        ei += 1

                chk('Bb')

                def qkk(ci, c0, c1):
                    return [QK.k(ci, g) for g in range(c0 // 512, (c1 - 1) // 512 + 1)]

                vi = 0
                for r_ in range(d):
                    for n in range(nb):
                        idx = r_ * nb + n
                        t0 = r_ + d * 128 * n
                        t1 = t0 + d * 127 + 1
                        need = (n == nb - 1)
                        bk = 2 + vi % 2
                        if need:
                            c0_, nn = ko_, 2 * nk
                        else:
                            c0_, nn = vo_, nk
                        for kc in range(8):
                            mm(ps[bk][:, 0:nn], hT.ap[:, kc, t0:t1:d], wblk.ap[:, kc, c0_:c0_ + nn], kc == 0, kc == 7,
                               [wblk.k()] + hk(t0, t1), ['ps%d' % bk])
                        vsrc = ps[bk][:, nn - nk:nn].rearrange("p (h d) -> p h d", h=hk_)
                        import os
                        EXP = os.environ.get('EXP', '0')
                        if EXP != 'a':
                            evac(vi, Vt.ap[:, idx, :, 0:64], vsrc, ['ps%d' % bk], [Vt.k(idx)])
                        if need and EXP != 'b':
                            kb = kvst[vi % 2]
                            evac(vi + 1, kb.ap[:, 0:nn], ps[bk][:, 0:nn], ['ps%d' % bk], [kb.k()])
                            keep = W
                            rs = t0 - (SEQ - keep)
                            dst = st_p[a][p, rs:rs + d * 127 + 1:d].rearrange("w t h d -> w (t h d)")
                            ok = 'stp%d_%d_%d' % (a, p, idx)
                            DM('sp', dst, kb.ap[:, 0:nn], r=[kb.k()], w=[ok])
                            outkeys.append(ok)
                        vi += 1

                chk('Bc')
                SA, PB = [4, 5, 0], [6, 7, 1]
                pairs = [(0, 1), (2, 1), (4, 1), (6, 1)] if a == 0 else [(0, 2), (1, 2)]

                def make_unit(u, idx, t0, t1, k0, nkeys, boff, vt_idx, ot, lt, h0, hstep, last):
                    sl = u % 3
                    sb_, pb_, ptb = S_sb[sl], Pb[sl], PT[sl]
                    hs = hst[u % 6]
                    negm2, es2, den2, rden2, lnd2, rs2 = (hs.ap[:, 2 * i:2 * i + 2] for i in range(6))
                    hsl = slice(h0, h0 + hstep + 1, hstep)
                    nbk = nkeys // 128
                    bS, bP = SA[sl], PB[sl]
                    pS, pT = 'ps%d' % bS, 'ps%d' % bP
                    heads = []
                    for j in range(2):
                        h = h0 + j * hstep
                        if a == 0:
                            heads.append((h % 4, 4, 64 * (h // 4), h // 4))
                        else:
                            heads.append((h // 2, 2 + h // 2, 64 * (h % 2), h))
                    sb3 = sb_.ap.rearrange("p (j k) -> p j k", j=2)[:, :, 0:nkeys]
                    pb3 = pb_.ap.rearrange("p (j k) -> p j k", j=2)

                    def s1():
                        for j, (qci, kci, base, kvh) in enumerate(heads):
                            mm(ps[bS][:, j * 256:j * 256 + nkeys], QK.ap[base:base + 64, qci, t0:t1:d],
                               QK.ap[base:base + 64, kci, k0:t1:d], True, True, qkk(qci, t0, t1) + qkk(kci, k0, t1), [pS])
                        V('scalar_tensor_tensor', [pS, bias.k(h0), bias.k(h0 + hstep)], [sb_.k()], out=sb3,
                          in0=ps[bS][:, :].rearrange("p (j k) -> p j k", j=2)[:, :, 0:nkeys],
                          scalar=0.125, in1=bias.ap[:, hsl, boff:boff + nkeys], op0=ALU.mult, op1=ALU.add)
                        V('tensor_reduce', [sb_.k()], [hs.k()], out=negm2, in_=sb3, axis=AX.X, op=ALU.max, negate=True)
                        if a == 0:
                            V('tensor_tensor', [hs.k(), nsink.k()], [hs.k()], out=negm2, in0=negm2, in1=nsink.ap[:, hsl], op=ALU.min)
                            V('tensor_tensor', [hs.k(), sink.k()], [hs.k()], out=es2, in0=negm2, in1=sink.ap[:, hsl], op=ALU.add)
                        for j in range(2):
                            act(pb3[:, j, 0:nkeys], sb3[:, j, :], AF.Exp, [sb_.k(), hs.k()], [pb_.k(), hs.k('rs')], bias=negm2[:, j:j + 1],
                                scale=1.0, accum=rs2[:, j:j + 1])
                        if a == 0:
                            act(es2, es2, AF.Exp, [hs.k()], [hs.k()])
                            V('tensor_tensor', [hs.k(), hs.k('rs')], [hs.k()], out=den2, in0=rs2, in1=es2, op=ALU.add)
                        else:
                            V('tensor_copy', [hs.k(), hs.k('rs')], [hs.k()], out=den2, in_=rs2)
                        V('reciprocal', [hs.k()], [hs.k()], out=rden2, in_=den2)
                        if a > 0:
                            act(lnd2, den2, AF.Ln, [hs.k()], [hs.k()])
                            V('tensor_tensor', [hs.k()], [lt.k()], out=lt.ap[:, hsl], in0=lnd2, in1=negm2, op=ALU.subtract)

                    def s2():
                        for j in range(2):
                            for bi in range(nbk):
                                tp(psb[bP][:, (j * nbk + bi) * 128:(j * nbk + bi + 1) * 128], pb3[:, j, bi * 128:(bi + 1) * 128],
                                   identb.ap, [pb_.k(), identb.k()], [pT])
                        evac(u, ptb.ap[:, 0:2 * nkeys], psb[bP][:, 0:2 * nkeys], [pT], [ptb.k()])
                        for j, (qci, kci, base, kvh) in enumerate(heads):
                            for bi, vix in enumerate(vt_idx):
                                mm(ps[bS][:, j * 128:j * 128 + 64], ptb.ap[:, (j * nbk + bi) * 128:(j * nbk + bi + 1) * 128],
                                   Vt.ap[:, vix, kvh, 0:64], bi == 0, bi == len(vt_idx) - 1, [ptb.k(), Vt.k(vix)], [pS])
                        po3 = ps[bS][:, 0:256].rearrange("p (j k) -> p j k", j=2)
                        V('tensor_tensor', [pS, hs.k()], [ot.k()], out=ot.ap[:, 0:hq * 64].rearrange("p (j k) -> p j k", j=hq)[:, hsl, :],
                          in0=po3[:, :, 0:64], in1=rden2.unsqueeze(2).to_broadcast([128, 2, 64]), op=ALU.mult)
                        if last:
                            for c in range(nq // 128):
                                tp(psb[3][:, c * 128:(c + 1) * 128], ot.ap[:, c * 128:(c + 1) * 128], identb.ap,
                                   [ot.k(), identb.k()], ['ps3'])
                            for c in range(nq // 128):
                                evac(c + idx, oT[a].ap[:, c, t0:t1:d], psb[3][:, c * 128:(c + 1) * 128], ['ps3'], [oT[a].k(idx)])
                            if a > 0:
                                tp(ps[2][0:4, 0:128], lt.ap[:, 0:4], identf.ap, [lt.k(), identf.k()], ['ps2'])
                                V('tensor_copy', ['ps2'], [lseT.k(a, idx)], out=lseT.ap[0:4, a - 1, t0:t1:d], in_=ps[2][0:4, 0:128])
                    return s1, s2

                units = []
                for r_ in range(d):
                    for n in range(nb):
                        idx = r_ * nb + n
                        t0 = r_ + d * 128 * n
                        t1 = t0 + d * 127 + 1
                        if n > 0:
                            k0, nkeys, boff, vt_idx = t0 - d * 128, 256, 0, [idx - 1, idx]
                        else:
                            k0, nkeys, boff, vt_idx = t0, 128, 128, [idx]
                        for pi_, (h0, hstep) in enumerate(pairs):
                            units.append(make_unit(len(units), idx, t0, t1, k0, nkeys, boff, vt_idx, o_tok[idx % 2], lse_tok[idx % 2],
                                                   h0, hstep, pi_ == len(pairs) - 1))
                for ui in range(len(units) + 2):
                    if ui < len(units):
                        units[ui][0]()
                    if ui >= 2:
                        units[ui - 2][1]()
                chk('B%d_%dp' % (p, a))
                if has_s:
                    RS = Region(A, RS_LO, END)
                    hsrep = RS.alloc("hsrep", [128, 8, 128], BF16)
                    zrep = RS.alloc("zrep", [128, 768], F32)
                    Ks = RS.alloc("Ks", [128, 16, nk], F32)
                    Vs = RS.alloc("Vs", [128, 16, nk], F32)
                    prod = RS.alloc("prod", [128, 16, 256], F32)
                    Ss = RS.alloc("Ss", [128, 16, hq], F32)
                    sbs = RS.alloc("sbs", [128, 16, hq], F32)
                    sm = RS.alloc("sm", [128, 160], F32)
                    opart = RS.alloc("opart", [128, 512], F32)
                    pi = RS.alloc("pi", [128, 2], I32)
                    V('tensor_copy', [hT.k(4)], [hsrep.k()], out=hsrep.ap.rearrange("p c (kg b) -> p c kg b", kg=8),
                      in_=hT.ap[:, :, SEQ:TC].unsqueeze(2).to_broadcast([128, 8, 8, 16]))
                    for (c0_, nn, bk) in ((0, 512, 0), (512, 256, 1)):
                        for kc in range(8):
                            mm(ps[bk][:, 0:nn], hsrep.ap[:, kc, :], wblk.ap[:, kc, c0_:c0_ + nn], kc == 0, kc == 7,
                               [hsrep.k(), wblk.k()], ['ps%d' % bk])
                        evac(bk, zrep.ap[:, c0_:c0_ + nn], ps[bk][:, 0:nn], ['ps%d' % bk], [zrep.k()])
                    ok = 'stsnew%d' % a
                    dstn = st_s[a][:, W - 1].rearrange("b t h d -> b (t h d)")
                    DM('sp', dstn, zrep.ap[0:16, ko_:ko_ + 2 * nk], r=[zrep.k()], w=[ok])
                    outkeys.append(ok)
                    for kg in range(8):
                        r0 = kg * 16 * d
                        srcK = caches[a][:, r0:r0 + 15 * d + 1:d, 0].rearrange("b j h d -> b j (h d)")
                        srcV = caches[a][:, r0:r0 + 15 * d + 1:d, 1].rearrange("b j h d -> b j (h d)")
                        DM('sp', Ks.ap[kg * 16:(kg + 1) * 16], srcK, w=[Ks.k(kg)])
                        DM('sp', Vs.ap[kg * 16:(kg + 1) * 16], srcV, w=[Vs.k(kg)])
                    Kk = [Ks.k(kg) for kg in range(8)]
                    Vk = [Vs.k(kg) for kg in range(8)]
                    PL('iota', [], [pi.k()], pi.ap[:, 0:1], pattern=[[0, 1]], base=0, channel_multiplier=1)
                    V('tensor_single_scalar', [pi.k()], [pi.k()], out=pi.ap[:, 1:2], in_=pi.ap[:, 0:1], scalar=4, op=ALU.arith_shift_right)
                    kgf = sm.ap[:, 150:151]
                    V('tensor_copy', [pi.k()], [sm.k('kg')], out=kgf, in_=pi.ap[:, 1:2])
                    dist = prod.ap[:, 0, 0:16]
                    PL('iota', [], [prod.k()], dist, pattern=[[-1, 16]], base=128, channel_multiplier=0,
                       allow_small_or_imprecise_dtypes=True)
                    V('tensor_scalar', [sm.k('kg')], [sm.k('kg')], out=kgf, in0=kgf, scalar1=16.0, scalar2=None, op0=ALU.mult)
                    V('tensor_scalar', [prod.k(), sm.k('kg')], [prod.k()], out=dist, in0=dist, scalar1=kgf, scalar2=None, op0=ALU.subtract)
                    for h in range(hq):
                        V('tensor_scalar', [prod.k()], [sbs.k()], out=sbs.ap[:, :, h], in0=dist, scalar1=-slopes[h] * d, scalar2=None,
                          op0=ALU.mult)
                    snew, lm, negM, enew, lsum, tot, denr, rdn, lnd_s = (sm.ap[:, i * 8:i * 8 + hq] for i in range(9))
                    smk = sm.k('s')
                    qz = zrep.ap[:, 0:nq]
                    kz = zrep.ap[:, ko_:ko_ + nk]
                    vz = zrep.ap[:, vo_:vo_ + nk]
                    if a == 0:
                        for kv in range(2):
                            q4 = qz.rearrange("p (g two d) -> p two g d", g=4, two=2, d=64)[:, kv]
                            V('tensor_tensor', Kk + [zrep.k(), sbs.k()], [prod.k()], out=prod.ap.rearrange("p k (h d) -> p k h d", h=4),
                              in0=Ks.ap[:, :, kv * 64:(kv + 1) * 64].unsqueeze(2).to_broadcast([128, 16, 4, 64]),
                              in1=q4.unsqueeze(1).to_broadcast([128, 16, 4, 64]), op=ALU.mult)
                            V('tensor_reduce', [prod.k()], [Ss.k()], out=Ss.ap[:, :, kv * 4:(kv + 1) * 4],
                              in_=prod.ap.rearrange("p k (h d) -> p k h d", h=4), axis=AX.X, op=ALU.add)
                            V('tensor_tensor', [zrep.k(), Ss.k()], [prod.k()], out=prod.ap[:, 0, :].rearrange("p (h d) -> p h d", h=4),
                              in0=q4, in1=kz[:, kv * 64:(kv + 1) * 64].unsqueeze(1).to_broadcast([128, 4, 64]), op=ALU.mult)
                            V('tensor_reduce', [prod.k()], [smk], out=snew[:, kv * 4:(kv + 1) * 4],
                              in_=prod.ap[:, 0, :].rearrange("p (h d) -> p h d", h=4), axis=AX.X, op=ALU.add)
                    else:
                        V('tensor_tensor', Kk + [zrep.k(), sbs.k()], [prod.k()], out=prod.ap, in0=Ks.ap,
                          in1=qz.unsqueeze(1).to_broadcast([128, 16, 256]), op=ALU.mult)
                        V('tensor_reduce', [prod.k()], [Ss.k()], out=Ss.ap, in_=prod.ap.rearrange("p k (h d) -> p k h d", h=4),
                          axis=AX.X, op=ALU.add)
                        V('tensor_tensor', [zrep.k(), Ss.k()], [prod.k()], out=prod.ap[:, 0, :], in0=qz, in1=kz, op=ALU.mult)
                        V('tensor_reduce', [prod.k()], [smk], out=snew, in_=prod.ap[:, 0, :].rearrange("p (h d) -> p h d", h=4),
                          axis=AX.X, op=ALU.add)
                    V('scalar_tensor_tensor', [Ss.k(), sbs.k()], [Ss.k()], out=Ss.ap, in0=Ss.ap, scalar=0.125, in1=sbs.ap,
                      op0=ALU.mult, op1=ALU.add)
                    V('tensor_scalar', [smk], [smk], out=snew, in0=snew, scalar1=0.125, scalar2=None, op0=ALU.mult)
                    V('tensor_reduce', [Ss.k()], [smk], out=lm, in_=Ss.ap.rearrange("p k h -> p h k"), axis=AX.X, op=ALU.max)
                    V('tensor_tensor', [smk], [smk], out=lm, in0=lm, in1=snew, op=ALU.max)
                    if a == 0:
                        V('tensor_tensor', [smk, sink.k()], [smk], out=lm, in0=lm, in1=sink.ap, op=ALU.max)
                    tp(ps[2][0:hq, 0:128], lm, identf.ap, [smk, identf.k()], ['ps2'])
                    gm = opart.ap[0:hq, 0:16]
                    V('tensor_reduce', ['ps2'], [opart.k()], out=gm, in_=ps[2][0:hq, 0:128].rearrange("h (kg b) -> h b kg", kg=8),
                      axis=AX.X, op=ALU.max)
                    gmr = opart.ap[0:hq, 128:256]
                    V('tensor_copy', [opart.k()], [opart.k()], out=gmr.rearrange("h (kg b) -> h kg b", kg=8),
                      in_=gm.unsqueeze(1).to_broadcast([hq, 8, 16]))
                    tp(ps[2][:, 256:256 + hq], gmr, identf.ap[0:hq, 0:hq], [opart.k(), identf.k()], ['ps2'])
                    V('tensor_scalar', ['ps2'], [smk], out=negM, in0=ps[2][:, 256:256 + hq], scalar1=-1.0, scalar2=None, op0=ALU.mult)
                    V('tensor_tensor', [Ss.k(), smk], [Ss.k()], out=Ss.ap, in0=Ss.ap, in1=negM.unsqueeze(1).to_broadcast([128, 16, hq]),
                      op=ALU.add)
                    act(Ss.ap, Ss.ap, AF.Exp, [Ss.k()], [Ss.k()])
                    V('tensor_tensor', [smk], [smk], out=enew, in0=snew, in1=negM, op=ALU.add)
                    act(enew, enew, AF.Exp, [smk], [smk])
                    V('tensor_reduce', [Ss.k()], [smk], out=lsum, in_=Ss.ap.rearrange("p k h -> p h k"), axis=AX.X, op=ALU.add)
                    V('tensor_scalar', [smk], [smk], out=enew, in0=enew, scalar1=0.125, scalar2=None, op0=ALU.mult)
                    V('tensor_tensor', [smk], [smk], out=tot, in0=lsum, in1=enew, op=ALU.add)
                    if a == 0:
                        esk = sm.ap[:, 80:88]
                        V('tensor_tensor', [smk, sink.k()], [smk], out=esk, in0=sink.ap, in1=negM, op=ALU.add)
                        act(esk, esk, AF.Exp, [smk], [smk])
                        V('scalar_tensor_tensor', [smk], [smk], out=tot, in0=esk, scalar=0.125, in1=tot, op0=ALU.mult, op1=ALU.add)
                    mm(ps[3][:, 0:hq], Gm.ap, tot, True, True, [Gm.k(), smk], ['ps3'])
                    V('tensor_copy', ['ps3'], [smk], out=denr, in_=ps[3][:, 0:hq])
                    V('reciprocal', [smk], [smk], out=rdn, in_=denr)
                    if a == 0:
                        for kv in range(2):
                            V('tensor_tensor', Vk + [Ss.k(), smk], [prod.k()], out=prod.ap.rearrange("p k (h d) -> p k h d", h=4),
                              in0=Vs.ap[:, :, kv * 64:(kv + 1) * 64].unsqueeze(2).to_broadcast([128, 16, 4, 64]),
                              in1=Ss.ap[:, :, kv * 4:(kv + 1) * 4].unsqueeze(3).to_broadcast([128, 16, 4, 64]), op=ALU.mult)
                            V('tensor_reduce', [prod.k()], [opart.k()], out=opart.ap[:, kv * 256:(kv + 1) * 256],
                              in_=prod.ap.rearrange("p k f -> p f k"), axis=AX.X, op=ALU.add)
                            V('tensor_tensor', [zrep.k(), smk, opart.k()], [prod.k()], out=prod.ap[:, 0, :].rearrange("p (h d) -> p h d", h=4),
                              in0=vz[:, kv * 64:(kv + 1) * 64].unsqueeze(1).to_broadcast([128, 4, 64]),
                              in1=enew[:, kv * 4:(kv + 1) * 4].unsqueeze(2).to_broadcast([128, 4, 64]), op=ALU.mult)
                            V('tensor_tensor', [prod.k(), opart.k()], [opart.k()], out=opart.ap[:, kv * 256:(kv + 1) * 256],
                              in0=opart.ap[:, kv * 256:(kv + 1) * 256], in1=prod.ap[:, 0, :], op=ALU.add)
                    else:
                        V('tensor_tensor', Vk + [Ss.k(), smk], [prod.k()], out=prod.ap.rearrange("p k (h d) -> p k h d", h=4),
                          in0=Vs.ap.rearrange("p k (h d) -> p k h d", h=4), in1=Ss.ap.unsqueeze(3).to_broadcast([128, 16, 4, 64]),
                          op=ALU.mult)
                        V('tensor_reduce', [prod.k()], [opart.k()], out=opart.ap[:, 0:256], in_=prod.ap.rearrange("p k f -> p f k"),
                          axis=AX.X, op=ALU.add)
                        V('tensor_tensor', [zrep.k(), smk, opart.k()], [prod.k()], out=prod.ap[:, 0, :].rearrange("p (h d) -> p h d", h=4),
                          in0=vz.rearrange("p (h d) -> p h d", h=4), in1=enew.unsqueeze(2).to_broadcast([128, 4, 64]), op=ALU.mult)
                        V('tensor_tensor', [prod.k(), opart.k()], [opart.k()], out=opart.ap[:, 0:256], in0=opart.ap[:, 0:256],
                          in1=prod.ap[:, 0, :], op=ALU.add)
                    mm(ps[0][:, 0:nq], Gm.ap, opart.ap[:, 0:nq], True, True, [Gm.k(), opart.k()], ['ps0'])
                    ots = o_tok[0]
                    V('tensor_tensor', ['ps0', smk], [ots.k()], out=ots.ap[0:16, 0:nq].rearrange("p (h d) -> p h d", h=hq),
                      in0=ps[0][0:16, 0:nq].rearrange("p (h d) -> p h d", h=hq),
                      in1=rdn[0:16, :].unsqueeze(2).to_broadcast([16, hq, 64]), op=ALU.mult)
                    for c in range(nq // 128):
                        tp(psb[3][:, c * 16:(c + 1) * 16], ots.ap[0:16, c * 128:(c + 1) * 128], identb.ap[0:16, 0:16],
                           [ots.k(), identb.k()], ['ps3'])
                    V('tensor_copy', ['ps3'], [oT[a].k('s')], out=oT[a].ap[:, :, SEQ:TC],
                      in_=psb[3][:, 0:(nq // 128) * 16].rearrange("p (c s) -> p c s", c=nq // 128))
                    if a > 0:
                        act(lnd_s, denr, AF.Ln, [smk], [smk])
                        lts = lse_tok[0]
                        V('tensor_tensor', [smk], [lts.k()], out=lts.ap[0:16, 0:4], in0=lnd_s[0:16, :], in1=negM[0:16, :], op=ALU.subtract)
                        tp(ps[2][0:4, 0:16], lts.ap[0:16, 0:4], identf.ap[0:16, 0:16], [lts.k(), identf.k()], ['ps2'])
                        V('tensor_copy', ['ps2'], [lseT.k(a, 's')], out=lseT.ap[0:4, a - 1, SEQ:TC], in_=ps[2][0:4, 0:16])

            chk('B%d' % p)
            GT_LO = R1_LO + 17 * 4096
            gT = A.carve("gT", GT_LO, [128, 8, TC], BF16)
            RM = Region(A, GT_LO + 8 * TC * 2, END)
            wbs = RM.alloc("wbs", [128, 4, 1024], BF16)
            wbd = RM.alloc("wbd", [128, 2, 1024], BF16)
            wga = RM.alloc("wga", [128, 8, 512], BF16)
            wgb = RM.alloc("wgb", [128, 8, 512], BF16)
            cM = RM.alloc("cM", [4, 512], F32)
            cS = RM.alloc("cS", [4, 512], F32)
            tf = [RM.alloc("tf%d" % i, [128, 512], F32) for i in range(4)]
            DM('pool', wbs.ap, w_br_swa.rearrange("(k p) n -> p k n", p=128), w=[wbs.k()])
            DM('pool', wbd.ap, w_br_dil.rearrange("(k p) n -> p k n", p=128), w=[wbd.k()])
            for ti, (c0, wd) in enumerate(mtiles):
                lk = [lseT.k()]
                L = lseT.ap[0:4, :, c0:c0 + wd]
                lse_keys = []
                for a_ in (1, 2, 3):
                    dd = TYPES[a_]['d']
                    nbb = 16 // dd
                    if c0 >= SEQ:
                        lse_keys.append(lseT.k(a_, 's'))
                    else:
                        for r_ in range(dd):
                            for n in range(nbb):
                                t0 = r_ + dd * 128 * n
                                if t0 < c0 + wd and t0 + dd * 127 >= c0:
                                    lse_keys.append(lseT.k(a_, r_ * nbb + n))
                ck = lseT.k('c', ti)
                V('tensor_tensor', lse_keys, [cM.k()], out=cM.ap[:, 0:wd], in0=L[:, 0, :], in1=L[:, 1, :], op=ALU.max)
                V('tensor_tensor', lse_keys + [cM.k()], [cM.k()], out=cM.ap[:, 0:wd], in0=cM.ap[:, 0:wd], in1=L[:, 2, :], op=ALU.max)
                V('tensor_tensor', lse_keys + [cM.k()], [ck], out=L, in0=L, in1=cM.ap[:, 0:wd].unsqueeze(1).to_broadcast([4, 3, wd]),
                  op=ALU.subtract)
                act(L, L, AF.Exp, [ck], [ck])
                V('tensor_tensor', [ck], [cS.k()], out=cS.ap[:, 0:wd], in0=L[:, 0, :], in1=L[:, 1, :], op=ALU.add)
                V('tensor_tensor', [ck, cS.k()], [cS.k()], out=cS.ap[:, 0:wd], in0=cS.ap[:, 0:wd], in1=L[:, 2, :], op=ALU.add)
                V('reciprocal', [cS.k()], [cS.k()], out=cS.ap[:, 0:wd], in_=cS.ap[:, 0:wd])
                V('tensor_tensor', [ck, cS.k()], [ck], out=L, in0=L, in1=cS.ap[:, 0:wd].unsqueeze(1).to_broadcast([4, 3, wd]), op=ALU.mult)

                def okeys(a_):
                    dd = TYPES[a_]['d']
                    nbb = 16 // dd
                    if c0 >= SEQ:
                        return [oT[a_].k('s')]
                    out = []
                    for r_ in range(dd):
                        for n in range(nbb):
                            t0 = r_ + dd * 128 * n
                            if t0 < c0 + wd and t0 + dd * 127 >= c0:
                                out.append(oT[a_].k(r_ * nbb + n))
                    return out
                for c in range(2):
                    for g in range(3):
                        mm(ps[g][:, 0:wd], selH[c].ap, lseT.ap[0:4, g, c0:c0 + wd], True, True, [selH[c].k(), ck], ['ps%d' % g])
                    t0_, t1_ = tf[0], tf[1]
                    V('tensor_tensor', okeys(1) + ['ps0'], [t0_.k()], out=t0_.ap[:, 0:wd], in0=oT[1].ap[:, c, c0:c0 + wd], in1=ps[0][:, 0:wd], op=ALU.mult)
                    V('tensor_tensor', okeys(2) + ['ps1'], [t1_.k()], out=t1_.ap[:, 0:wd], in0=oT[2].ap[:, c, c0:c0 + wd], in1=ps[1][:, 0:wd], op=ALU.mult)
                    V('tensor_tensor', [t0_.k(), t1_.k()], [t0_.k()], out=t0_.ap[:, 0:wd], in0=t0_.ap[:, 0:wd], in1=t1_.ap[:, 0:wd], op=ALU.add)
                    V('tensor_tensor', okeys(3) + ['ps2'], [t1_.k()], out=t1_.ap[:, 0:wd], in0=oT[3].ap[:, c, c0:c0 + wd], in1=ps[2][:, 0:wd], op=ALU.mult)
                    V('tensor_tensor', [t0_.k(), t1_.k()] + okeys(1), [oT[1].k('comb', ti, c)], out=oT[1].ap[:, c, c0:c0 + wd],
                      in0=t0_.ap[:, 0:wd], in1=t1_.ap[:, 0:wd], op=ALU.add)

            def swa_keys(c0, wd):
                if c0 >= SEQ:
                    return [oT[0].k('s')]
                return [oT[0].k(t) for t in range(c0 // 128, (c0 + wd - 1) // 128 + 1)]

            it = 0
            for q4 in range(2):
                DM('pool', wga.ap, w_in[:, 3072 + q4 * 512:3072 + (q4 + 1) * 512].rearrange("(k p) n -> p k n", p=128), w=[wga.k()])
                DM('pool', wgb.ap, w_in[:, 4096 + q4 * 512:4096 + (q4 + 1) * 512].rearrange("(k p) n -> p k n", p=128), w=[wgb.k()])
                for ti, (c0, wd) in enumerate(mtiles):
                    for oc4 in range(4):
                        oc = q4 * 4 + oc4
                        b0 = (it % 2) * 4
                        it += 1
                        pk = ['ps%d' % (b0 + i) for i in range(4)]
                        for kc in range(8):
                            mm(ps[b0][:, 0:wd], wga.ap[:, kc, oc4 * 128:(oc4 + 1) * 128], hT.ap[:, kc, c0:c0 + wd], kc == 0, kc == 7,
                               [wga.k()] + hk(c0, c0 + wd), [pk[0]])
                        for kc in range(8):
                            mm(ps[b0 + 1][:, 0:wd], wgb.ap[:, kc, oc4 * 128:(oc4 + 1) * 128], hT.ap[:, kc, c0:c0 + wd], kc == 0, kc == 7,
                               [wgb.k()] + hk(c0, c0 + wd), [pk[1]])
                        for k4 in range(4):
                            mm(ps[b0 + 2][:, 0:wd], wbs.ap[:, k4, oc * 128:(oc + 1) * 128], oT[0].ap[:, k4, c0:c0 + wd], k4 == 0, k4 == 3,
                               [wbs.k()] + swa_keys(c0, wd), [pk[2]])
                        for k2 in range(2):
                            mm(ps[b0 + 3][:, 0:wd], wbd.ap[:, k2, oc * 128:(oc + 1) * 128], oT[1].ap[:, k2, c0:c0 + wd], k2 == 0, k2 == 1,
                               [wbd.k(), oT[1].k('comb', ti, 0), oT[1].k('comb', ti, 1)], [pk[3]])
                        sa, sb2, ta, tb2 = tf[0], tf[1], tf[2], tf[3]
                        act(sa.ap[:, 0:wd], ps[b0][:, 0:wd], AF.Sigmoid, [pk[0]], [sa.k()])
                        act(sb2.ap[:, 0:wd], ps[b0 + 1][:, 0:wd], AF.Sigmoid, [pk[1]], [sb2.k()])
                        V('tensor_tensor', [sa.k(), pk[2]], [ta.k()], out=ta.ap[:, 0:wd], in0=sa.ap[:, 0:wd], in1=ps[b0 + 2][:, 0:wd], op=ALU.mult)
                        V('tensor_tensor', [sb2.k(), pk[3]], [tb2.k()], out=tb2.ap[:, 0:wd], in0=sb2.ap[:, 0:wd], in1=ps[b0 + 3][:, 0:wd], op=ALU.mult)
                        V('tensor_tensor', [ta.k(), tb2.k()], [gT.k(oc, ti)], out=gT.ap[:, oc, c0:c0 + wd], in0=ta.ap[:, 0:wd],
                          in1=tb2.ap[:, 0:wd], op=ALU.add)

            chk('M1_%d' % p)
            Yacc = A.carve("Yacc", R1_LO, [128, 17, 1024], F32)
            RM2 = Region(A, GT_LO + 8 * TC * 2, END)
            gbc = RM2.alloc("gbc", [128, 2, 1024], F32)
            lnbc = RM2.alloc("lnbc", [128, 2, 1024], F32)
            xt2 = RM2.alloc("xt2", [128, 1024], F32)
            xm = RM2.alloc("xm", [128, 1024], F32)
            wts = RM2.alloc("wts", [128, 17, 64], F32)
            RE2_LO = RM2.p
            wout = RM2.alloc("wout", [128, 8, 1024], BF16)
            h2f = RM2.alloc("h2f", [128, 8, 128], F32)
            wr = RM2.alloc("wr", [128, 8, 64], F32)
            rbias = RM2.alloc("rbias", [128, 64], F32)
            rt = RM2.alloc("rt", [128, 6, 64], F32)
            DM('pool', wout.ap, w_out.rearrange("(k p) n -> p k n", p=128), w=[wout.k()])
            DM('sp', lnbc.ap[:, 0, :], ln1_g.to_broadcast([128, 1024]), w=[lnbc.k()])
            DM('sp', lnbc.ap[:, 1, :], ln1_b.to_broadcast([128, 1024]), w=[lnbc.k()])
            DM('sp', wr.ap, w_router.rearrange("(k p) n -> p k n", p=128), w=[wr.k()])
            DM('sp', rbias.ap, router_bias.to_broadcast([128, 64]), w=[rbias.k()])
            for n4 in range(4):
                mm(ps[n4][:, :], selP[p].ap, modg.ap[0:18, n4 * 512:(n4 + 1) * 512], True, True, [selP[p].k(), modg.k()], ['ps%d' % n4])
                evac(n4, gbc.ap[:, n4 // 2, (n4 % 2) * 512:(n4 % 2 + 1) * 512], ps[n4][:, :], ['ps%d' % n4], [gbc.k()])
            for t, (c0, P_) in enumerate(stiles):
                smp = (c0 >= SEQ)
                ti = 4 if smp else c0 // 512
                if smp:
                    DM('sp', xt2.ap[0:16, :], xs[:, :], w=[xt2.k()])
                else:
                    DM('sp', xt2.ap, xp[p, c0:c0 + 128, :], w=[xt2.k()])
                b0 = (t % 2) * 4
                for nh in range(2):
                    for kc in range(8):
                        mm(ps[b0 + nh][0:P_, :], gT.ap[:, kc, c0:c0 + P_], wout.ap[:, kc, nh * 512:(nh + 1) * 512], kc == 0, kc == 7,
                           [gT.k(kc, ti), wout.k()], ['ps%d' % (b0 + nh)])
                    g1 = modg.ap[0:16, nh * 512:(nh + 1) * 512] if smp else gbc.ap[:, 0, nh * 512:(nh + 1) * 512]
                    V('tensor_tensor', ['ps%d' % (b0 + nh), gbc.k(), modg.k()], [xm.k()], out=xm.ap[0:P_, nh * 512:(nh + 1) * 512],
                      in0=ps[b0 + nh][0:P_, :], in1=g1, op=ALU.mult)
                V('scalar_tensor_tensor', [xt2.k(), xm.k()], [xm.k()], out=xm.ap[0:P_, :], in0=xt2.ap[0:P_, :], scalar=ALPHA,
                  in1=xm.ap[0:P_, :], op0=ALU.mult, op1=ALU.add)
                rstd, nmr, k = ln_stats(xm.ap[0:P_, :], P_, [xm.k()])
                act(xm.ap[0:P_, :], xm.ap[0:P_, :], AF.Identity, [xm.k(), k], [xm.k()], bias=nmr, scale=rstd)
                V('tensor_tensor', [xm.k(), lnbc.k()], [xm.k()], out=xm.ap[0:P_, :], in0=xm.ap[0:P_, :], in1=lnbc.ap[0:P_, 0, :], op=ALU.mult)
                V('tensor_tensor', [xm.k(), lnbc.k()], [xm.k()], out=xm.ap[0:P_, :], in0=xm.ap[0:P_, :], in1=lnbc.ap[0:P_, 1, :], op=ALU.add)
                act(Yacc.ap[0:P_, t, :], xm.ap[0:P_, :], AF.Identity, [xm.k()], [Yacc.k(t)], scale=ALPHA)
                rstd, nmr, k = ln_stats(xm.ap[0:P_, :], P_, [xm.k()])
                act(xt2.ap[0:P_, :], xm.ap[0:P_, :], AF.Identity, [xm.k(), k], [xt2.k()], bias=nmr, scale=rstd)
                for c in range(8):
                    tp(ps[b0 + 2 + c // 4][:, (c % 4) * 128:(c % 4) * 128 + P_], xt2.ap[0:P_, c * 128:(c + 1) * 128],
                       identf.ap[0:P_, 0:P_], [xt2.k(), identf.k()], ['ps%d' % (b0 + 2 + c // 4)])
                for c in range(8):
                    src = ps[b0 + 2 + c // 4][:, (c % 4) * 128:(c % 4) * 128 + P_]
                    pk = 'ps%d' % (b0 + 2 + c // 4)
                    if smp:
                        V('tensor_tensor', [pk, opsc2T.k()], [h2f.k(c)], out=h2f.ap[:, c, 0:16], in0=src, in1=opsc2T.ap[:, c, 0:16], op=ALU.mult)
                        V('tensor_tensor', [h2f.k(c), modT.k()], [h2f.k(c)], out=h2f.ap[:, c, 0:16], in0=h2f.ap[:, c, 0:16],
                          in1=modT.ap[:, 24 + c, 0:16], op=ALU.add)
                    else:
                        sc_ = opsc2T.ap[:, c, pcol:pcol + 1]
                        sh_ = modT.ap[:, 24 + c, pcol:pcol + 1]
                        if c % 2 == 0:
                            act(h2f.ap[:, c, :], src, AF.Identity, [pk, opsc2T.k(), modT.k()], [h2f.k(c)], bias=sh_, scale=sc_)
                        else:
                            V('tensor_scalar', [pk, opsc2T.k(), modT.k()], [h2f.k(c)], out=h2f.ap[:, c, :], in0=src, scalar1=sc_,
                              scalar2=sh_, op0=ALU.mult, op1=ALU.add)
                h2k = [h2f.k(c) for c in range(8)]
                V('tensor_copy', h2k + [gT.k(kc, ti) for kc in range(0)], [hT.k(ti)], out=hT.ap[:, :, c0:c0 + P_], in_=h2f.ap[:, :, 0:P_])
                pr = 'ps%d' % (b0 + 2)
                for c in range(8):
                    mm(ps[b0 + 2][0:P_, 0:64], h2f.ap[:, c, 0:P_], wr.ap[:, c, :], c == 0, c == 7, h2k + [wr.k()], [pr])
                sc = rt.ap[0:P_, 0, :]
                bi = rt.ap[0:P_, 1, :]
                eq = rt.ap[0:P_, 2, :]
                msk = rt.ap[0:P_, 3, :]
                m1 = rt.ap[0:P_, 4, 0:8]
                m2 = rt.ap[0:P_, 4, 8:16]
                gs = rt.ap[0:P_, 4, 16:24]
                gs8 = rt.ap[0:P_, 4, 24:32]
                gmk = rt.ap[0:P_, 4, 32:40]
                s8 = rt.ap[0:P_, 4, 40:48]
                wsum = rt.ap[0:P_, 4, 48:49]
                rk_ = rt.k()
                act(sc, ps[b0 + 2][0:P_, 0:64], AF.Sigmoid, [pr], [rk_])
                V('tensor_tensor', [rk_, rbias.k()], [rk_], out=bi, in0=sc, in1=rbias.ap[0:P_, :], op=ALU.add)
                bi3 = bi.rearrange("p (g e) -> p g e", g=8)
                V('tensor_reduce', [rk_], [rk_], out=m1, in_=bi3, axis=AX.X, op=ALU.max)
                V('tensor_tensor', [rk_], [rk_], out=eq.rearrange("p (g e) -> p g e", g=8), in0=bi3,
                  in1=m1.unsqueeze(2).to_broadcast([P_, 8, 8]), op=ALU.is_equal)
                V('scalar_tensor_tensor', [rk_], [rk_], out=eq, in0=eq, scalar=-1e9, in1=bi, op0=ALU.mult, op1=ALU.add)
                V('tensor_reduce', [rk_], [rk_], out=m2, in_=eq.rearrange("p (g e) -> p g e", g=8), axis=AX.X, op=ALU.max)
                V('tensor_tensor', [rk_], [rk_], out=gs, in0=m1, in1=m2, op=ALU.add)
                V('max', [rk_], [rk_], out=gs8, in_=gs)
                V('tensor_scalar', [rk_], [rk_], out=gmk, in0=gs, scalar1=gs8[:, 3:4], scalar2=None, op0=ALU.is_ge)
                V('tensor_scalar', [rk_], [rk_], out=gmk, in0=gmk, scalar1=1e9, scalar2=-1e9, op0=ALU.mult, op1=ALU.add)
                V('tensor_tensor', [rk_], [rk_], out=msk.rearrange("p (g e) -> p g e", g=8), in0=bi3,
                  in1=gmk.unsqueeze(2).to_broadcast([P_, 8, 8]), op=ALU.add)
                V('max', [rk_], [rk_], out=s8, in_=msk)
                V('tensor_scalar', [rk_], [rk_], out=msk, in0=msk, scalar1=s8[:, 7:8], scalar2=None, op0=ALU.is_ge)
                V('tensor_tensor', [rk_], [rk_], out=msk, in0=msk, in1=sc, op=ALU.mult)
                V('tensor_reduce', [rk_], [rk_], out=wsum, in_=msk, axis=AX.X, op=ALU.add)
                V('reciprocal', [rk_], [rk_], out=wsum, in_=wsum)
                V('tensor_scalar', [rk_], [wts.k(t)], out=wts.ap[0:P_, t, :], in0=msk, scalar1=wsum, scalar2=2.5, op0=ALU.mult, op1=ALU.mult)

            chk('M2_%d' % p)
            RE = Region(A, GT_LO, GT_LO + 8 * TC * 2)
            Wg = [RE.alloc("Wg%d" % i, [128, 8, 256], BF16) for i in range(2)]
            Wu = [RE.alloc("Wu%d" % i, [128, 8, 256], BF16) for i in range(2)]
            Wd = [RE.alloc("Wd%d" % i, [128, 2, 1024], BF16) for i in range(2)]
            sg = [RE.alloc("sg%d" % i, [128, 512], F32) for i in range(2)]
            Hb = [RE.alloc("Hb%d" % i, [128, 2, 512], BF16) for i in range(2)]
            RE2 = Region(A, RE2_LO, END)
            Wds = [RE2.alloc("Wds%d" % i, [128, 2, 1024], BF16) for i in range(2)]
            ytmp = RE2.alloc("ytmp", [16, 512], F32)
            hi = 0
            yi = 0
            for e_ in range(NEXP + 1):
                sl = e_ % 2
                if p == 0 and e_ == 3:
                    for a_, T_ in enumerate(TYPES):
                        W_ = T_['W']
                        src = caches[a_][:, 1:W_].rearrange("b w t h d -> b (w t h d)")
                        dst = st_s[a_][:, 0:W_ - 1].rearrange("b w t h d -> b (w t h d)")
                        DM('act', dst, src, w=['bulkout%d' % a_], bulk=True)
                        outkeys.append('bulkout%d' % a_)
                if e_ < NEXP:
                    srcs = (w_eg[e_], w_eu[e_], w_ed[e_])
                else:
                    srcs = (w_sg, w_su, w_sd)
                DM('pool', Wg[sl].ap, srcs[0].rearrange("(k p) n -> p k n", p=128), w=[Wg[sl].k()])
                DM('pool', Wu[sl].ap, srcs[1].rearrange("(k p) n -> p k n", p=128), w=[Wu[sl].k()])
                DM('pool', Wd[sl].ap, srcs[2].rearrange("(k p) n -> p k n", p=128), w=[Wd[sl].k()])
                V('tensor_tensor', [Wd[sl].k(), gbc.k()], [Wds[sl].k()], out=Wds[sl].ap, in0=Wd[sl].ap,
                  in1=gbc.ap[:, 1, :].unsqueeze(1).to_broadcast([128, 2, 1024]), op=ALU.mult)
                for ti, (c0, wd) in enumerate(mtiles):
                    smp = (c0 >= SEQ)
                    hb = Hb[hi % 2]
                    hi += 1
                    for oc in range(2):
                        pg, pu = 'ps%d' % (oc * 2), 'ps%d' % (oc * 2 + 1)
                        for kc in range(8):
                            mm(ps[oc * 2][:, 0:wd], Wg[sl].ap[:, kc, oc * 128:(oc + 1) * 128], hT.ap[:, kc, c0:c0 + wd], kc == 0, kc == 7,
                               [Wg[sl].k(), hT.k(ti)], [pg])
                        for kc in range(8):
                            mm(ps[oc * 2 + 1][:, 0:wd], Wu[sl].ap[:, kc, oc * 128:(oc + 1) * 128], hT.ap[:, kc, c0:c0 + wd], kc == 0, kc == 7,
                               [Wu[sl].k(), hT.k(ti)], [pu])
                        act(sg[oc].ap[:, 0:wd], ps[oc * 2][:, 0:wd], AF.Silu, [pg], [sg[oc].k()])
                        V('tensor_tensor', [sg[oc].k(), pu], [hb.k(oc)], out=hb.ap[:, oc, 0:wd], in0=sg[oc].ap[:, 0:wd], in1=ps[oc * 2 + 1][:, 0:wd],
                          op=ALU.mult)
                    nsub = 1 if smp else 4
                    for j in range(nsub):
                        t = 16 if smp else ti * 4 + j
                        P_ = 16 if smp else 128
                        wsc = 1.0 if e_ == NEXP else wts.ap[0:P_, t, e_:e_ + 1]
                        wk = [] if e_ == NEXP else [wts.k(t)]
                        for nh in range(2):
                            bk = 4 + yi % 4
                            yi += 1
                            py = 'ps%d' % bk
                            wdn = Wd[sl] if smp else Wds[sl]
                            for k2 in range(2):
                                mm(ps[bk][0:P_, :], hb.ap[:, k2, j * 128:j * 128 + P_], wdn.ap[:, k2, nh * 512:(nh + 1) * 512], k2 == 0, k2 == 1,
                                   [hb.k(0), hb.k(1), wdn.k()], [py])
                            ydst = Yacc.ap[0:P_, t, nh * 512:(nh + 1) * 512]
                            if smp:
                                V('tensor_tensor', [py, modg.k()], [ytmp.k()], out=ytmp.ap, in0=ps[bk][0:16, :],
                                  in1=modg.ap[0:16, 1024 + nh * 512:1024 + (nh + 1) * 512], op=ALU.mult)
                                V('scalar_tensor_tensor', [ytmp.k(), Yacc.k(t, nh), Yacc.k(t)] + wk, [Yacc.k(t, nh)], out=ydst, in0=ytmp.ap, scalar=wsc,
                                  in1=ydst, op0=ALU.mult, op1=ALU.add)
                            else:
                                V('scalar_tensor_tensor', [py, Yacc.k(t, nh), Yacc.k(t)] + wk, [Yacc.k(t, nh)], out=ydst, in0=ps[bk][0:P_, :],
                                  scalar=wsc, in1=ydst, op0=ALU.mult, op1=ALU.add)

            chk('E_%d' % p)
            DM('sp', lnbc.ap[:, 0, :], ln2_g.to_broadcast([128, 1024]), w=[lnbc.k()])
            DM('sp', lnbc.ap[:, 1, :], ln2_b.to_broadcast([128, 1024]), w=[lnbc.k()])
            obuf = [xt2, xm]
            for t, (c0, P_) in enumerate(stiles):
                smp = (c0 >= SEQ)
                yk = [Yacc.k(t), Yacc.k(t, 0), Yacc.k(t, 1)]
                ya = Yacc.ap[0:P_, t, :]
                rstd, nmr, k = ln_stats(ya, P_, yk)
                ob = obuf[t % 2]
                act(ob.ap[0:P_, :], ya, AF.Identity, yk + [k], [ob.k()], bias=nmr, scale=rstd)
                V('tensor_tensor', [ob.k(), lnbc.k()], [ob.k()], out=ob.ap[0:P_, :], in0=ob.ap[0:P_, :], in1=lnbc.ap[0:P_, 0, :], op=ALU.mult)
                V('tensor_tensor', [ob.k(), lnbc.k()], [ob.k()], out=ob.ap[0:P_, :], in0=ob.ap[0:P_, :], in1=lnbc.ap[0:P_, 1, :], op=ALU.add)
                ok = 'y_%d_%d' % (p, t)
                if smp:
                    DM('sp', ys[:, :], ob.ap[0:16, :], r=[ob.k()], w=[ok])
                else:
                    DM('sp', yp[p, c0:c0 + 128, :], ob.ap, r=[ob.k()], w=[ok])
                outkeys.append(ok)

        R.stopped = False
        R.op('sp', lambda e: None, r=outkeys)
        R.emit(nc, st)
    return nc, R


_CACHE = {}


def kernel(x_prompt, x_sample, cache_swa_kv, cache_dil1_kv, cache_dil2_kv, cache_dil3_kv, c_prompt, c_sample,
           w_ada, b_ada, w_in, attn_sinks, w_br_swa, w_br_dil, w_out, ln1_g, ln1_b, w_router, router_bias,
           w_exp_gate, w_exp_up, w_exp_down, w_sh_gate, w_sh_up, w_sh_down, ln2_g, ln2_b):
    f = lambda a_: np.ascontiguousarray(np.asarray(a_, dtype=np.float32))
    if 'nc' not in _CACHE:
        _CACHE['nc'] = build_nc()[0]
    nc = _CACHE['nc']
    cach = [f(cache_swa_kv)[0], f(cache_dil1_kv)[0], f(cache_dil2_kv)[0], f(cache_dil3_kv)[0]]
    xpr, xsa = f(x_prompt), f(x_sample)
    cp, cs_ = f(c_prompt), f(c_sample)
    shared = dict(
        w_ada=f(w_ada)[0], b_ada=f(b_ada), w_in=f(w_in)[0], sinks=f(attn_sinks), w_br_swa=f(w_br_swa)[0],
        w_br_dil=f(w_br_dil)[0], w_out=f(w_out)[0], ln1_g=f(ln1_g), ln1_b=f(ln1_b), w_router=f(w_router)[0],
        router_bias=f(router_bias), w_eg=f(w_exp_gate)[0], w_eu=f(w_exp_up)[0], w_ed=f(w_exp_down)[0],
        w_sg=f(w_sh_gate)[0], w_su=f(w_sh_up)[0], w_sd=f(w_sh_down)[0], ln2_g=f(ln2_g), ln2_b=f(ln2_b))
    in_maps = []
    for c in range(NCORES):
        m = dict(shared)
        m['xp'] = xpr[2 * c:2 * c + 2]
        m['xs'] = xsa[16 * c:16 * c + 16, 0]
        m['c_all'] = np.ascontiguousarray(np.concatenate([cs_[16 * c:16 * c + 16], cp[2 * c:2 * c + 2]], axis=0))
        for a in range(4):
            m['cache%d' % a] = cach[a][16 * c:16 * c + 16]
        in_maps.append(m)
    res = run_bass_kernel_spmd(nc, in_maps, core_ids=list(range(NCORES)))
    rs = res.results
    y_prompt = np.concatenate([r['yp'] for r in rs], axis=0)
    y_sample = np.concatenate([r['ys'] for r in rs], axis=0)[:, None, :]
    outs = [y_prompt, y_sample]
    for a in range(4):
        outs.append(np.concatenate([r['stp%d' % a] for r in rs], axis=0)[None])
    for a in range(4):
        outs.append(np.concatenate([r['sts%d' % a] for r in rs], axis=0)[None])
    return tuple(np.ascontiguousarray(o, dtype=np.float32) for o in outs)
```

```python
import math
from contextlib import ExitStack

import numpy as np
import concourse.bass as bass
import concourse.mybir as mybir
from concourse.bass_utils import run_bass_kernel_spmd

F32 = mybir.dt.float32
BF16 = mybir.dt.bfloat16
I32 = mybir.dt.int32
AF = mybir.ActivationFunctionType
ALU = mybir.AluOpType
AX = mybir.AxisListType

NCORES = 8
D = 1024
SEQ = 2048
NSAMP = 16
TC = SEQ + NSAMP
NEXP = 64
ALPHA = 2.0 ** 0.25
LN_EPS = 1e-5
NEGBIG = -30000.0
import os
SAME_ENGINE_SYNC = os.environ.get('SES', '0') == '1'
NS = 8

TYPES = [
    dict(d=1, hq=8, hk=2, qo=0, ko=512, vo=640, W=128),
    dict(d=1, hq=4, hk=4, qo=768, ko=1536, vo=2304, W=128),
    dict(d=4, hq=4, hk=4, qo=1024, ko=1792, vo=2560, W=512),
    dict(d=16, hq=4, hk=4, qo=1280, ko=2048, vo=2816, W=2048),
]


def slopes_for(a):
    if a == 0:
        return [2.0 ** (-8.0 * (i + 1) / 8) for i in range(8)]
    g = a - 1
    return [2.0 ** (-8.0 * (4 * g + i + 1) / 12) for i in range(4)]


class Rec:
    def __init__(self):
        self.ops = []
        self.alias = {}
        self.stopped = False

    @staticmethod
    def _excl(r, w):
        r, w = list(r), list(w)
        w = w + [k for k in r if k.startswith('ps') and k not in w]
        r = [k for k in r if not k.startswith('ps')]
        return r, w

    def op(self, eng, fn, r=(), w=()):
        if self.stopped:
            return
        r, w = self._excl(r, w)
        self.ops.append(dict(eng=eng, fn=fn, r=tuple(r), w=tuple(w), dma=False, bulk=False))

    def dma(self, q, fn, r=(), w=(), bulk=False):
        if self.stopped:
            return
        r, w = self._excl(r, w)
        self.ops.append(dict(eng=q, fn=fn, r=tuple(r), w=tuple(w), dma=True, bulk=bulk))

    def emit(self, nc, stack):
        ops = self.ops
        last_w = {}
        readers = {}
        dreaders = {}
        by_base = {}
        def conf(k):
            b = k.split(':')[0]
            if k == b:
                return list(by_base.get(b, ())) + [k]
            return [k, b]

        for i, op in enumerate(ops):
            deps = set()
            for k in op['r']:
                for kk in conf(k):
                    if kk in last_w:
                        deps.add(last_w[kk])
            raw = set(deps)
            for k in op['w']:
                ks = conf(k)
                base = k.split(':')[0]
                for ob in self.alias.get(base, ()):
                    ks.extend(by_base.get(ob, ()))
                    ks.append(ob)
                for kk in ks:
                    if kk in last_w:
                        deps.add(last_w[kk])
                    deps.update(readers.get(kk, {}).values())
                    deps.update(dreaders.get(kk, ()))
            deps.discard(i)
            fdeps = set()
            for j in deps:
                oj = ops[j]
                if (not oj['dma']) and (not op['dma']) and oj['eng'] == op['eng']:
                    if op['eng'] == 'pe' or (op['eng'] != 'pool' and not SAME_ENGINE_SYNC and j not in raw):
                        continue
                fdeps.add(j)
            op['deps'] = fdeps
            for k in op['r']:
                by_base.setdefault(k.split(':')[0], set()).add(k)
                if op['dma']:
                    dreaders.setdefault(k, []).append(i)
                else:
                    readers.setdefault(k, {})[op['eng']] = i
            for k in op['w']:
                by_base.setdefault(k.split(':')[0], set()).add(k)
                last_w[k] = i
                readers[k] = {}
                dreaders[k] = []
        fin = ops[-1]
        lastc = {}
        lastd = {}
        for i, op in enumerate(ops[:-1]):
            if op['dma']:
                lastd.setdefault(op['eng'], []).append(i)
            else:
                lastc[op['eng']] = i
        fin['deps'].update(lastc.values())
        for q, lst in lastd.items():
            fin['deps'].update(lst[-NS:])
        needs = set()
        for op in ops:
            needs.update(op['deps'])
        engs = ['pe', 'act', 'dve', 'pool', 'sp']
        esem = {e: stack.enter_context(nc.semaphore('es_' + e)) for e in engs}
        dsem = {q: [stack.enter_context(nc.semaphore('ds_%s%d' % (q, i))) for i in range(NS)]
                for q in ['sp', 'act', 'pool']}
        bsem = stack.enter_context(nc.semaphore('bulk'))
        cnt = {e: 0 for e in engs}
        dcnt = {q: 0 for q in dsem}
        nbulk = sum(1 for op in ops if op['bulk'])
        for i, op in enumerate(ops):
            if op['bulk']:
                op['sig'] = (bsem, 16 * nbulk)
                op['pre'] = None
            elif op['dma']:
                q = op['eng']
                k = dcnt[q]
                dcnt[q] += 1
                op['sig'] = (dsem[q][k % NS], 16 * (k // NS + 1))
                op['pre'] = (dsem[q][k % NS], 16 * (k // NS)) if k >= NS else None
            elif i in needs:
                cnt[op['eng']] += 1
                op['sig'] = (esem[op['eng']], cnt[op['eng']])
            else:
                op['sig'] = None
        self.maxcnt = dict(cnt)
        block = stack.enter_context(nc.Block())

        def run(ename, eng):
            waited = {}
            for op in ops:
                if op['eng'] != ename:
                    continue
                ws = {}
                for j in op['deps']:
                    s, v = ops[j]['sig']
                    ws[id(s)] = (s, max(v, ws.get(id(s), (s, 0))[1]))
                if op['dma'] and op['pre'] is not None:
                    s, v = op['pre']
                    ws[id(s)] = (s, max(v, ws.get(id(s), (s, 0))[1]))
                for sid, (s, v) in ws.items():
                    if waited.get(sid, 0) >= v:
                        continue
                    eng.wait_ge(s, v)
                    waited[sid] = v
                inst = op['fn'](eng)
                if op['sig'] is not None and inst is not None:
                    inst.then_inc(op['sig'][0], 16 if op['dma'] else 1)

        @block.tensor
        def _(e):
            run('pe', e)

        @block.scalar
        def _(e):
            run('act', e)

        @block.vector
        def _(e):
            run('dve', e)

        @block.gpsimd
        def _(e):
            run('pool', e)

        @block.sync
        def _(e):
            run('sp', e)


class Buf:
    def __init__(self, key, ap, lo, hi):
        self.key, self.ap, self.lo, self.hi = key, ap, lo, hi

    def k(self, *sub):
        return self.key if not sub else self.key + ':' + '_'.join(str(s) for s in sub)


class Arena:
    def __init__(self, R, arena_ap, nbytes):
        self.R, self.arena, self.nbytes = R, arena_ap, nbytes
        self.live = []
        self.n = 0

    def carve(self, name, off, shape, dt):
        n = 1
        for s in shape[1:]:
            n *= s
        esz = 4 if dt in (F32, I32) else 2
        assert off % 4 == 0 and off + n * esz <= self.nbytes, (name, off, n * esz, self.nbytes)
        P = shape[0]
        if esz == 4:
            a = self.arena[0:P, off // 2: off // 2 + 2 * n].bitcast(dt)
        else:
            a = self.arena[0:P, off // 2: off // 2 + n]
        if len(shape) == 3:
            a = a.rearrange("p (a b) -> p a b", a=shape[1])
        elif len(shape) == 4:
            a = a.rearrange("p (a b c) -> p a b c", a=shape[1], b=shape[2])
        self.n += 1
        key = '%s#%d' % (name, self.n)
        lo, hi = off, off + n * esz
        al = [k for (k, l, h) in self.live if l < hi and lo < h]
        if al:
            self.R.alias[key] = al
        self.live.append((key, lo, hi))
        return Buf(key, a, lo, hi)


class Region:
    def __init__(self, arena, lo, hi):
        self.A, self.lo, self.hi, self.p = arena, lo, hi, lo

    def alloc(self, name, shape, dt):
        n = 1
        for s in shape[1:]:
            n *= s
        esz = 4 if dt in (F32, I32) else 2
        off = (self.p + 31) // 32 * 32
        assert off + n * esz <= self.hi, (name, off, n * esz, self.hi)
        self.p = off + n * esz
        return self.A.carve(name, off, shape, dt)


class _Stop(Exception):
    pass


STAGE = None


def build_nc():
    nc = bass.Bass("TRN2", target_bir_lowering=False)

    def din(name, shape):
        return nc.dram_tensor(name, list(shape), F32, kind="ExternalInput").ap()

    def dout(name, shape):
        return nc.dram_tensor(name, list(shape), F32, kind="ExternalOutput").ap()

    xp = din("xp", [2, SEQ, D])
    xs = din("xs", [NSAMP, D])
    c_all = din("c_all", [18, D])
    caches = [din("cache%d" % a, [NSAMP, T['W'], 2, T['hk'], 64]) for a, T in enumerate(TYPES)]
    w_ada = din("w_ada", [D, 6 * D])
    b_ada = din("b_ada", [1, 6 * D])
    w_in = din("w_in", [D, 5120])
    sinks = din("sinks", [1, 8])
    w_br_swa = din("w_br_swa", [512, D])
    w_br_dil = din("w_br_dil", [256, D])
    w_out = din("w_out", [D, D])
    ln1_g = din("ln1_g", [1, D])
    ln1_b = din("ln1_b", [1, D])
    w_router = din("w_router", [D, NEXP])
    router_bias = din("router_bias", [1, NEXP])
    w_eg = din("w_eg", [NEXP, D, 256])
    w_eu = din("w_eu", [NEXP, D, 256])
    w_ed = din("w_ed", [NEXP, 256, D])
    w_sg = din("w_sg", [D, 256])
    w_su = din("w_su", [D, 256])
    w_sd = din("w_sd", [256, D])
    ln2_g = din("ln2_g", [1, D])
    ln2_b = din("ln2_b", [1, D])

    yp = dout("yp", [2, SEQ, D])
    ys = dout("ys", [NSAMP, D])
    st_p = [dout("stp%d" % a, [2, T['W'], 2, T['hk'], 64]) for a, T in enumerate(TYPES)]
    st_s = [dout("sts%d" % a, [NSAMP, T['W'], 2, T['hk'], 64]) for a, T in enumerate(TYPES)]

    R = Rec()
    outkeys = []
    with ExitStack() as st:
        ARENA_BYTES = 206 * 1024
        arena_t = st.enter_context(nc.sbuf_tensor("arena", [128, ARENA_BYTES // 2], BF16))
        A = Arena(R, arena_t[:], ARENA_BYTES)
        ps = [st.enter_context(nc.psum_tensor("ps%d" % i, [128, 512], F32)) for i in range(8)]
        psb = [p[:].bitcast(BF16) for p in ps]

        def chk(name):
            if STAGE == name:
                R.stopped = True

        def mm(out, lhsT, rhs, start, stop, r, w):
            R.op('pe', lambda e: e.matmul(out, lhsT=lhsT, rhs=rhs, start=start, stop=stop), r, w)

        def tp(out, in_, ident, r, w):
            R.op('pe', lambda e: e.transpose(out=out, in_=in_, identity=ident), r, w)

        def act(out, in_, func, r, w, bias=None, scale=None, accum=None):
            kw = {}
            if bias is not None:
                kw['bias'] = bias
            if scale is not None:
                kw['scale'] = scale
            if accum is not None:
                kw['accum_out'] = accum
            R.op('act', lambda e: e.activation(out=out, in_=in_, func=func, **kw), r, w)

        def V(name, r, w, *args, **kw):
            R.op('dve', lambda e: getattr(e, name)(*args, **kw), r, w)

        def DM(q, out, in_, r=(), w=(), bulk=False):
            R.dma(q, lambda e: e.dma_start(out=out, in_=in_), r, w, bulk)

        def PL(name, r, w, *args, **kw):
            R.op('pool', lambda e: getattr(e, name)(*args, **kw), r, w)

        def evac(i, out, in_, r, w):
            if i % 2 == 0:
                R.op('act', lambda e: e.copy(out=out, in_=in_), r, w)
            else:
                R.op('dve', lambda e: e.tensor_copy(out=out, in_=in_), r, w)

        PS = Region(A, 0, 17408)
        identb = PS.alloc("identb", [128, 128], BF16)
        identf = PS.alloc("identf", [128, 128], F32)
        modT = PS.alloc("modT", [128, 48, 18], F32)
        opsc1T = PS.alloc("opsc1T", [128, 8, 18], F32)
        opsc2T = PS.alloc("opsc2T", [128, 8, 18], F32)
        sink = PS.alloc("sink", [128, 8], F32)
        nsink = PS.alloc("nsink", [128, 8], F32)
        modg = PS.alloc("modg", [18, 2048], F32)
        Gm = PS.alloc("Gm", [128, 128], F32)
        epst = PS.alloc("epst", [128, 1], F32)
        selH = [PS.alloc("selH%d" % c, [4, 128], F32) for c in range(2)]
        selP = [PS.alloc("selP%d" % p, [18, 128], F32) for p in range(2)]
        stat = PS.alloc("stat", [128, 64], F32)
        HT_LO = 17408
        hT = A.carve("hT", HT_LO, [128, 8, TC], BF16)
        R1_LO = HT_LO + 8 * TC * 2
        WORK_LO_A = R1_LO + 66048
        END = ARENA_BYTES

        def hk(c0, c1):
            return [hT.k(g) for g in range(c0 // 512, min((c1 - 1) // 512, 4) + 1)]


        PL('memset', [], [identf.k()], identf.ap, 0.0)
        PL('affine_select', [identf.k()], [identf.k()], out=identf.ap, in_=identf.ap, pattern=[[-1, 128]],
           compare_op=ALU.not_equal, fill=1.0, base=0, channel_multiplier=1)
        V('tensor_copy', [identf.k()], [identb.k()], out=identb.ap, in_=identf.ap)
        V('memset', [], [epst.k()], epst.ap, LN_EPS)
        e16 = stat.ap[:, 0:16]
        V('tensor_reduce', [identf.k()], [stat.k()], out=e16, in_=identf.ap.rearrange("p (kg b) -> p b kg", kg=8),
          axis=AX.X, op=ALU.add)
        V('tensor_copy', [stat.k()], [Gm.k()], out=Gm.ap.rearrange("p (kg b) -> p kg b", kg=8),
          in_=e16.unsqueeze(1).to_broadcast([128, 8, 16]))
        for c in range(2):
            V('tensor_copy', [identf.k()], [selH[c].k()], out=selH[c].ap.rearrange("p (h d) -> p h d", h=2),
              in_=identf.ap[0:4, 2 * c:2 * c + 2].unsqueeze(2).to_broadcast([4, 2, 64]))
        for p in range(2):
            V('tensor_copy', [identf.k()], [selP[p].k()], out=selP[p].ap,
              in_=identf.ap[0:18, 16 + p:17 + p].to_broadcast([18, 128]))
        DM('sp', sink.ap, sinks.to_broadcast([128, 8]), w=[sink.k()])
        V('tensor_scalar', [sink.k()], [nsink.k()], out=nsink.ap, in0=sink.ap, scalar1=-1.0, scalar2=None, op0=ALU.mult)

        B0 = Region(A, R1_LO, END)
        mod_tm = B0.alloc("mod_tm", [18, 6144], F32)
        bada = B0.alloc("bada", [1, 6144], F32)
        wa = [B0.alloc("wa%d" % i, [128, 8, 512], F32) for i in range(2)]
        cs = B0.alloc("cs", [18, 1024], F32)
        scT = B0.alloc("scT", [128, 8, 18], F32)
        ones18 = B0.alloc("ones18", [1, 18], F32)
        DM('sp', cs.ap, c_all[:, :], w=[cs.k()])
        DM('sp', bada.ap, b_ada[:, :], w=[bada.k()])
        V('memset', [], [ones18.k()], ones18.ap, 1.0)
        act(cs.ap, cs.ap, AF.Silu, [cs.k()], [cs.k()])
        for kc in range(8):
            tp(ps[0][:, kc * 18:(kc + 1) * 18], cs.ap[0:18, kc * 128:(kc + 1) * 128], identf.ap[0:18, 0:18],
               [cs.k(), identf.k()], ['ps0'])
        V('tensor_copy', ['ps0'], [scT.k()], out=scT.ap.rearrange("p a b -> p (a b)"), in_=ps[0][:, 0:144])
        for n in range(12):
            wb_ = wa[n % 2]
            DM('sp', wb_.ap, w_ada[:, n * 512:(n + 1) * 512].rearrange("(k p) n -> p k n", p=128), w=[wb_.k()])
            pk = 'ps%d' % (1 + n % 2)
            pt = ps[1 + n % 2]
            for kc in range(8):
                mm(pt[0:18, :], scT.ap[:, kc, :], wb_.ap[:, kc, :], kc == 0, False, [scT.k(), wb_.k()], [pk])
            mm(pt[0:18, :], ones18.ap, bada.ap[0:1, n * 512:(n + 1) * 512], False, True, [ones18.k(), bada.k()], [pk])
            evac(n, mod_tm.ap[:, n * 512:(n + 1) * 512], pt[0:18, :], [pk], [mod_tm.k(n)])
        mt_all = [mod_tm.k(n) for n in range(12)]
        V('tensor_copy', mt_all, [modg.k()], out=modg.ap[:, 0:1024], in_=mod_tm.ap[:, 2048:3072])
        V('tensor_copy', mt_all + [modg.k()], [modg.k()], out=modg.ap[:, 1024:2048], in_=mod_tm.ap[:, 5120:6144])
        for g3 in range(3):
            pk = 'ps%d' % (3 + g3 % 2)
            pt = ps[3 + g3 % 2]
            for j in range(16):
                c = g3 * 16 + j
                tp(pt[:, j * 18:(j + 1) * 18], mod_tm.ap[0:18, c * 128:(c + 1) * 128], identf.ap[0:18, 0:18],
                   mt_all + [identf.k()], [pk])
            V('tensor_copy', [pk], [modT.k()], out=modT.ap[:, g3 * 16:(g3 + 1) * 16, :].rearrange("p a b -> p (a b)"),
              in_=pt[:, 0:288])
        V('tensor_scalar', [modT.k()], [opsc1T.k()], out=opsc1T.ap, in0=modT.ap[:, 8:16, :], scalar1=1.0, scalar2=None,
          op0=ALU.add)
        V('tensor_scalar', [modT.k()], [opsc2T.k()], out=opsc2T.ap, in0=modT.ap[:, 32:40, :], scalar1=1.0, scalar2=None,
          op0=ALU.add)

        stat_slot = [0]

        def ln_stats(xap, P, rkeys):
            s = stat_slot[0] % 2
            stat_slot[0] += 1
            base = 16 + s * 24
            st6 = stat.ap[0:P, base:base + 12]
            mv = stat.ap[0:P, base + 12:base + 14]
            lnv = stat.ap[0:P, base + 14:base + 15]
            rstd = stat.ap[0:P, base + 15:base + 16]
            nmr = stat.ap[0:P, base + 16:base + 17]
            k = stat.k('ln', s)
            V('bn_stats', rkeys, [k], out=st6[:, 0:6], in_=xap[:, 0:512])
            V('bn_stats', rkeys + [k], [k], out=st6[:, 6:12], in_=xap[:, 512:1024])
            V('bn_aggr', [k], [k], out=mv, in_=st6)
            act(lnv, mv[:, 1:2], AF.Ln, [k, epst.k()], [k], bias=epst.ap[0:P, :], scale=1.0)
            act(rstd, lnv, AF.Exp, [k], [k], scale=-0.5)
            V('tensor_scalar', [k], [k], out=nmr, in0=mv[:, 0:1], scalar1=rstd, scalar2=-1.0, op0=ALU.mult, op1=ALU.mult)
            return rstd, nmr, k

        chk('p0')
        for p in range(2):
            has_s = (p == 1)
            ncol = TC if has_s else SEQ
            pcol = 16 + p
            mtiles = [(tt * 512, 512) for tt in range(4)] + ([(SEQ, NSAMP)] if has_s else [])
            stiles = [(t * 128, 128) for t in range(16)] + ([(SEQ, NSAMP)] if has_s else [])

            oT = [A.carve("oT0", R1_LO, [128, 4, TC], BF16)]
            off = R1_LO + 4 * TC * 2
            for g in range(3):
                oT.append(A.carve("oT%d" % (g + 1), off, [128, 2, TC], BF16))
                off += 2 * TC * 2
            lseT = A.carve("lseT", off, [4, 3, TC], F32)
            off += 3 * TC * 4
            assert off == WORK_LO_A

            RA = Region(A, WORK_LO_A, END)
            xt = [RA.alloc("xt%d" % i, [128, 1024], F32) for i in range(2)]
            xnb = [RA.alloc("xnb%d" % i, [128, 1024], BF16) for i in range(4)]
            for g4 in range(4):
                for j in range(4):
                    t = g4 * 4 + j
                    xb_ = xt[t % 2]
                    DM('sp', xb_.ap, xp[p, t * 128:(t + 1) * 128, :], w=[xb_.k()])
                    rstd, nmr, k = ln_stats(xb_.ap, 128, [xb_.k()])
                    chk('Aa')
                    act(xnb[j].ap, xb_.ap, AF.Identity, [xb_.k(), k], [xnb[j].k()], bias=nmr, scale=rstd)
                    chk('Ab')
                for c in range(8):
                    for j in range(4):
                        tp(psb[c][:, j * 128:(j + 1) * 128],
                           xnb[j].ap[:, c * 128:(c + 1) * 128], identb.ap, [xnb[j].k(), identb.k()], ['ps%d' % c])
                chk('Ac')
                for c in range(8):
                    src = psb[c][:, 0:512]
                    dst = hT.ap[:, c, g4 * 512:(g4 + 1) * 512]
                    sc_ = opsc1T.ap[:, c, pcol:pcol + 1]
                    sh_ = modT.ap[:, c, pcol:pcol + 1]
                    rk = ['ps%d' % c, opsc1T.k(), modT.k()]
                    if c % 2 == 0:
                        act(dst, src, AF.Identity, rk, [hT.k(g4)], bias=sh_, scale=sc_)
                    else:
                        V('tensor_scalar', rk, [hT.k(g4)], out=dst, in0=src, scalar1=sc_, scalar2=sh_, op0=ALU.mult, op1=ALU.add)
                chk('Ad')
            if has_s:
                xs_t = xt[0]
                DM('sp', xs_t.ap[0:16, :], xs[:, :], w=[xs_t.k()])
                rstd, nmr, k = ln_stats(xs_t.ap[0:16, :], 16, [xs_t.k()])
                act(xnb[0].ap[0:16, :], xs_t.ap[0:16, :], AF.Identity, [xs_t.k(), k], [xnb[0].k()], bias=nmr, scale=rstd)
                for c in range(8):
                    tp(psb[0][:, c * 16:(c + 1) * 16], xnb[0].ap[0:16, c * 128:(c + 1) * 128], identb.ap[0:16, 0:16],
                       [xnb[0].k(), identb.k()], ['ps0'])
                tmp_s = RA.alloc("tmp_s", [128, 8, 16], F32)
                V('tensor_tensor', ['ps0', opsc1T.k()], [tmp_s.k()], out=tmp_s.ap,
                  in0=psb[0][:, 0:128].rearrange("p (c s) -> p c s", c=8), in1=opsc1T.ap[:, :, 0:16], op=ALU.mult)
                V('tensor_tensor', [tmp_s.k(), modT.k()], [hT.k(4)], out=hT.ap[:, :, SEQ:TC], in0=tmp_s.ap,
                  in1=modT.ap[:, 0:8, 0:16], op=ALU.add)

            chk('A%d' % p)
            for a, T in enumerate(TYPES):
                d, hq, hk_, W = T['d'], T['hq'], T['hk'], T['W']
                nq, nk = hq * 64, hk_ * 64
                nb = 16 // d
                slopes = slopes_for(a)
                RB = Region(A, WORK_LO_A, END)
                wblk = RB.alloc("wblk", [128, 8, 768], BF16)
                nqc = nq // 128
                nkc = 1 if a == 0 else 2
                o_tok = [RB.alloc("o_tok%d" % i, [128, 512], BF16) for i in range(2)]
                lse_tok = [RB.alloc("lse_tok%d" % i, [128, 4], F32) for i in range(2)]
                hst = [RB.alloc("hst%d" % i, [128, 16], F32) for i in range(6)]
                RS_LO = RB.p
                QK = RB.alloc("QK", [128, nqc + nkc, TC], BF16)
                Vt = RB.alloc("Vt", [128, 16, hk_, 65], BF16)
                bias = RB.alloc("bias", [128, hq, 256], F32)
                rv = RB.alloc("rv", [128, 256], F32)
                pen = RB.alloc("pen", [128, 256], F32)
                S_sb = [RB.alloc("S_sb%d" % i, [128, 512], F32) for i in range(3)]
                Pb = [RB.alloc("Pb%d" % i, [128, 512], BF16) for i in range(3)]
                PT = [RB.alloc("PT%d" % i, [128, 512], BF16) for i in range(3)]
                kvst = [RB.alloc("kvst%d" % i, [128, 512], F32) for i in range(2)]

                for (o0, n0, c0) in ((T['qo'], nq, 0), (T['ko'], nk, nq), (T['vo'], nk, nq + nk)):
                    if a == 0 and c0 == 0:
                        for hh in range(8):
                            dc = ((hh % 4) * 2 + hh // 4) * 64
                            DM('pool', wblk.ap[:, :, dc:dc + 64],
                               w_in[:, hh * 64:(hh + 1) * 64].rearrange("(k p) n -> p k n", p=128), w=[wblk.k()])
                        continue
                    DM('pool', wblk.ap[:, :, c0:c0 + n0], w_in[:, o0:o0 + n0].rearrange("(k p) n -> p k n", p=128), w=[wblk.k()])
                ko_, vo_ = nq, nq + nk

                PL('iota', [], [rv.k()], rv.ap, pattern=[[-1, 256]], base=128, channel_multiplier=1,
                   allow_small_or_imprecise_dtypes=True)
                V('tensor_scalar', [rv.k()], [pen.k()], out=pen.ap, in0=rv.ap, scalar1=0.0, scalar2=None, op0=ALU.is_ge)
                V('tensor_scalar', [rv.k()], [S_sb[0].k()], out=S_sb[0].ap[:, 0:256], in0=rv.ap, scalar1=128.0, scalar2=None, op0=ALU.is_le)
                V('tensor_tensor', [pen.k(), S_sb[0].k()], [pen.k()], out=pen.ap, in0=pen.ap, in1=S_sb[0].ap[:, 0:256], op=ALU.mult)
                V('tensor_tensor', [rv.k(), pen.k()], [rv.k()], out=rv.ap, in0=rv.ap, in1=pen.ap, op=ALU.mult)
                V('tensor_scalar', [pen.k()], [pen.k()], out=pen.ap, in0=pen.ap, scalar1=-NEGBIG, scalar2=NEGBIG,
                  op0=ALU.mult, op1=ALU.add)
                for h in range(hq):
                    V('scalar_tensor_tensor', [rv.k(), pen.k()], [bias.k(h)], out=bias.ap[:, h, :], in0=rv.ap,
                      scalar=-slopes[h] * d, in1=pen.ap, op0=ALU.mult, op1=ALU.add)
                V('memset', [], [Vt.k('ones')], Vt.ap[:, :, :, 64:65], 1.0)

                chk('Ba')
                def qk_lhsT(ci, kc):
                    if a == 0:
                        if ci < 4:
                            return wblk.ap[:, kc, ci * 128:(ci + 1) * 128]
                        return wblk.ap[:, kc, 512:640]
                    if ci < 2:
                        return wblk.ap[:, kc, ci * 128:(ci + 1) * 128]
                    return wblk.ap[:, kc, 256 + (ci - 2) * 128:256 + (ci - 1) * 128]

                ei = 0
                for ci in range(nqc + nkc):
                    for (c0, wd) in mtiles:
                        bk = ei % 2
                        for kc in range(8):
                            mm(ps[bk][:, 0:wd], qk_lhsT(ci, kc), hT.ap[:, kc, c0:c0 + wd], kc == 0, kc == 7,
                               [wblk.k()] + hk(c0, c0 + wd), ['ps%d' % bk])
                        evac(ei, QK.ap[:, ci, c0:c0 + wd], ps[bk][:, 0:wd], ['ps%d' % bk], [QK.k(ci, c0 // 512)])
                        ei += 1

                chk('Bb')

                def qkk(ci, c0, c1):
                    return [QK.k(ci, g) for g in range(c0 // 512, (c1 - 1) // 512 + 1)]

                vi = 0
                for r_ in range(d):
                    for n in range(nb):
                        idx = r_ * nb + n
                        t0 = r_ + d * 128 * n
                        t1 = t0 + d * 127 + 1
                        need = (n == nb - 1)
                        bk = 2 + vi % 2
                        if need:
                            c0_, nn = ko_, 2 * nk
                        else:
                            c0_, nn = vo_, nk
                        for kc in range(8):
                            mm(ps[bk][:, 0:nn], hT.ap[:, kc, t0:t1:d], wblk.ap[:, kc, c0_:c0_ + nn], kc == 0, kc == 7,
                               [wblk.k()] + hk(t0, t1), ['ps%d' % bk])
                        vsrc = ps[bk][:, nn - nk:nn].rearrange("p (h d) -> p h d", h=hk_)
                        import os
                        EXP = os.environ.get('EXP', '0')
                        if EXP != 'a':
                            evac(vi, Vt.ap[:, idx, :, 0:64], vsrc, ['ps%d' % bk], [Vt.k(idx)])
                        if need and EXP != 'b':
                            kb = kvst[vi % 2]
                            evac(vi + 1, kb.ap[:, 0:nn], ps[bk][:, 0:nn], ['ps%d' % bk], [kb.k()])
                            keep = W
                            rs = t0 - (SEQ - keep)
                            dst = st_p[a][p, rs:rs + d * 127 + 1:d].rearrange("w t h d -> w (t h d)")
                            ok = 'stp%d_%d_%d' % (a, p, idx)
                            DM('sp', dst, kb.ap[:, 0:nn], r=[kb.k()], w=[ok])
                            outkeys.append(ok)
                        vi += 1

                chk('Bc')
                SA, PB = [4, 5, 0], [6, 7, 1]
                pairs = [(0, 1), (2, 1), (4, 1), (6, 1)] if a == 0 else [(0, 2), (1, 2)]

                def make_unit(u, idx, t0, t1, k0, nkeys, boff, vt_idx, ot, lt, h0, hstep, last):
                    sl = u % 3
                    sb_, pb_, ptb = S_sb[sl], Pb[sl], PT[sl]
                    hs = hst[u % 6]
                    negm2, es2, den2, rden2, lnd2, rs2 = (hs.ap[:, 2 * i:2 * i + 2] for i in range(6))
                    hsl = slice(h0, h0 + hstep + 1, hstep)
                    nbk = nkeys // 128
                    bS, bP = SA[sl], PB[sl]
                    pS, pT = 'ps%d' % bS, 'ps%d' % bP
                    heads = []
                    for j in range(2):
                        h = h0 + j * hstep
                        if a == 0:
                            heads.append((h % 4, 4, 64 * (h // 4), h // 4))
                        else:
                            heads.append((h // 2, 2 + h // 2, 64 * (h % 2), h))
                    sb3 = sb_.ap.rearrange("p (j k) -> p j k", j=2)[:, :, 0:nkeys]
                    pb3 = pb_.ap.rearrange("p (j k) -> p j k", j=2)

                    def s1():
                        for j, (qci, kci, base, kvh) in enumerate(heads):
                            mm(ps[bS][:, j * 256:j * 256 + nkeys], QK.ap[base:base + 64, qci, t0:t1:d],
                               QK.ap[base:base + 64, kci, k0:t1:d], True, True, qkk(qci, t0, t1) + qkk(kci, k0, t1), [pS])
                        V('scalar_tensor_tensor', [pS, bias.k(h0), bias.k(h0 + hstep)], [sb_.k()], out=sb3,
                          in0=ps[bS][:, :].rearrange("p (j k) -> p j k", j=2)[:, :, 0:nkeys],
                          scalar=0.125, in1=bias.ap[:, hsl, boff:boff + nkeys], op0=ALU.mult, op1=ALU.add)
                        V('tensor_reduce', [sb_.k()], [hs.k()], out=negm2, in_=sb3, axis=AX.X, op=ALU.max, negate=True)
                        if a == 0:
                            V('tensor_tensor', [hs.k(), nsink.k()], [hs.k()], out=negm2, in0=negm2, in1=nsink.ap[:, hsl], op=ALU.min)
                            V('tensor_tensor', [hs.k(), sink.k()], [hs.k()], out=es2, in0=negm2, in1=sink.ap[:, hsl], op=ALU.add)
                        for j in range(2):
                            act(pb3[:, j, 0:nkeys], sb3[:, j, :], AF.Exp, [sb_.k(), hs.k()], [pb_.k(), hs.k('rs')], bias=negm2[:, j:j + 1],
                                scale=1.0, accum=rs2[:, j:j + 1])
                        if a == 0:
                            act(es2, es2, AF.Exp, [hs.k()], [hs.k()])

                    def s2():
                        if a == 0:
                            V('tensor_tensor', [hs.k(), hs.k('rs')], [hs.k()], out=den2, in0=rs2, in1=es2, op=ALU.add)
                        else:
                            V('tensor_copy', [hs.k(), hs.k('rs')], [hs.k()], out=den2, in_=rs2)
                        V('reciprocal', [hs.k()], [hs.k()], out=rden2, in_=den2)
                        if a > 0:
                            act(lnd2, den2, AF.Ln, [hs.k()], [hs.k()])
                            V('tensor_tensor', [hs.k()], [lt.k()], out=lt.ap[:, hsl], in0=lnd2, in1=negm2, op=ALU.subtract)
                        for j in range(2):
                            for bi in range(nbk):
                                tp(psb[bP][:, (j * nbk + bi) * 128:(j * nbk + bi + 1) * 128], pb3[:, j, bi * 128:(bi + 1) * 128],
                                   identb.ap, [pb_.k(), identb.k()], [pT])
                        evac(0, ptb.ap[:, 0:2 * nkeys], psb[bP][:, 0:2 * nkeys], [pT], [ptb.k()])
                        for j, (qci, kci, base, kvh) in enumerate(heads):
                            for bi, vix in enumerate(vt_idx):
                                mm(ps[bS][:, j * 128:j * 128 + 64], ptb.ap[:, (j * nbk + bi) * 128:(j * nbk + bi + 1) * 128],
                                   Vt.ap[:, vix, kvh, 0:64], bi == 0, bi == len(vt_idx) - 1, [ptb.k(), Vt.k(vix)], [pS])

                    def s3():
                        po3 = ps[bS][:, 0:256].rearrange("p (j k) -> p j k", j=2)
                        V('tensor_tensor', [pS, hs.k()], [ot.k()], out=ot.ap[:, 0:hq * 64].rearrange("p (j k) -> p j k", j=hq)[:, hsl, :],
                          in0=po3[:, :, 0:64], in1=rden2.unsqueeze(2).to_broadcast([128, 2, 64]), op=ALU.mult)
                        if last:
                            for c in range(nq // 128):
                                tp(psb[3][:, c * 128:(c + 1) * 128], ot.ap[:, c * 128:(c + 1) * 128], identb.ap,
                                   [ot.k(), identb.k()], ['ps3'])
                            for c in range(nq // 128):
                                evac(c + idx, oT[a].ap[:, c, t0:t1:d], psb[3][:, c * 128:(c + 1) * 128], ['ps3'], [oT[a].k(idx)])
                            if a > 0:
                                tp(ps[2][0:4, 0:128], lt.ap[:, 0:4], identf.ap, [lt.k(), identf.k()], ['ps2'])
                                V('tensor_copy', ['ps2'], [lseT.k(a, idx)], out=lseT.ap[0:4, a - 1, t0:t1:d], in_=ps[2][0:4, 0:128])
                    return s1, s2, s3

                units = []
                for r_ in range(d):
                    for n in range(nb):
                        idx = r_ * nb + n
                        t0 = r_ + d * 128 * n
                        t1 = t0 + d * 127 + 1
                        if n > 0:
                            k0, nkeys, boff, vt_idx = t0 - d * 128, 256, 0, [idx - 1, idx]
                        else:
                            k0, nkeys, boff, vt_idx = t0, 128, 128, [idx]
                        for pi_, (h0, hstep) in enumerate(pairs):
                            units.append(make_unit(len(units), idx, t0, t1, k0, nkeys, boff, vt_idx, o_tok[idx % 2], lse_tok[idx % 2],
                                                   h0, hstep, pi_ == len(pairs) - 1))
                for ui in range(len(units) + 2):
                    if ui < len(units):
                        units[ui][0]()
                    if 1 <= ui <= len(units):
                        units[ui - 1][1]()
                    if ui >= 2:
                        units[ui - 2][2]()
                chk('B%d_%dp' % (p, a))
                if has_s:
                    RS = Region(A, RS_LO, END)
                    hsrep = RS.alloc("hsrep", [128, 8, 128], BF16)
                    zrep = RS.alloc("zrep", [128, 768], F32)
                    Ks = RS.alloc("Ks", [128, 16, nk], F32)
                    Vs = RS.alloc("Vs", [128, 16, nk], F32)
                    prod = RS.alloc("prod", [128, 16, 256], F32)
                    Ss = RS.alloc("Ss", [128, 16, hq], F32)
                    sbs = RS.alloc("sbs", [128, 16, hq], F32)
                    sm = RS.alloc("sm", [128, 160], F32)
                    opart = RS.alloc("opart", [128, 512], F32)
                    pi = RS.alloc("pi", [128, 2], I32)
                    V('tensor_copy', [hT.k(4)], [hsrep.k()], out=hsrep.ap.rearrange("p c (kg b) -> p c kg b", kg=8),
                      in_=hT.ap[:, :, SEQ:TC].unsqueeze(2).to_broadcast([128, 8, 8, 16]))
                    for (c0_, nn, bk) in ((0, 512, 0), (512, 256, 1)):
                        for kc in range(8):
                            mm(ps[bk][:, 0:nn], hsrep.ap[:, kc, :], wblk.ap[:, kc, c0_:c0_ + nn], kc == 0, kc == 7,
                               [hsrep.k(), wblk.k()], ['ps%d' % bk])
                        evac(bk, zrep.ap[:, c0_:c0_ + nn], ps[bk][:, 0:nn], ['ps%d' % bk], [zrep.k()])
                    ok = 'stsnew%d' % a
                    dstn = st_s[a][:, W - 1].rearrange("b t h d -> b (t h d)")
                    DM('sp', dstn, zrep.ap[0:16, ko_:ko_ + 2 * nk], r=[zrep.k()], w=[ok])
                    outkeys.append(ok)
                    for kg in range(8):
                        r0 = kg * 16 * d
                        srcK = caches[a][:, r0:r0 + 15 * d + 1:d, 0].rearrange("b j h d -> b j (h d)")
                        srcV = caches[a][:, r0:r0 + 15 * d + 1:d, 1].rearrange("b j h d -> b j (h d)")
                        DM('sp', Ks.ap[kg * 16:(kg + 1) * 16], srcK, w=[Ks.k(kg)])
                        DM('sp', Vs.ap[kg * 16:(kg + 1) * 16], srcV, w=[Vs.k(kg)])
                    Kk = [Ks.k(kg) for kg in range(8)]
                    Vk = [Vs.k(kg) for kg in range(8)]
                    PL('iota', [], [pi.k()], pi.ap[:, 0:1], pattern=[[0, 1]], base=0, channel_multiplier=1)
                    V('tensor_single_scalar', [pi.k()], [pi.k()], out=pi.ap[:, 1:2], in_=pi.ap[:, 0:1], scalar=4, op=ALU.arith_shift_right)
                    kgf = sm.ap[:, 150:151]
                    V('tensor_copy', [pi.k()], [sm.k('kg')], out=kgf, in_=pi.ap[:, 1:2])
                    dist = prod.ap[:, 0, 0:16]
                    PL('iota', [], [prod.k()], dist, pattern=[[-1, 16]], base=128, channel_multiplier=0,
                       allow_small_or_imprecise_dtypes=True)
                    V('tensor_scalar', [sm.k('kg')], [sm.k('kg')], out=kgf, in0=kgf, scalar1=16.0, scalar2=None, op0=ALU.mult)
                    V('tensor_scalar', [prod.k(), sm.k('kg')], [prod.k()], out=dist, in0=dist, scalar1=kgf, scalar2=None, op0=ALU.subtract)
                    for h in range(hq):
                        V('tensor_scalar', [prod.k()], [sbs.k()], out=sbs.ap[:, :, h], in0=dist, scalar1=-slopes[h] * d, scalar2=None,
                          op0=ALU.mult)
                    snew, lm, negM, enew, lsum, tot, denr, rdn, lnd_s = (sm.ap[:, i * 8:i * 8 + hq] for i in range(9))
                    smk = sm.k('s')
                    qz = zrep.ap[:, 0:nq]
                    kz = zrep.ap[:, ko_:ko_ + nk]
                    vz = zrep.ap[:, vo_:vo_ + nk]
                    if a == 0:
                        for kv in range(2):
                            q4 = qz.rearrange("p (g two d) -> p two g d", g=4, two=2, d=64)[:, kv]
                            V('tensor_tensor', Kk + [zrep.k(), sbs.k()], [prod.k()], out=prod.ap.rearrange("p k (h d) -> p k h d", h=4),
                              in0=Ks.ap[:, :, kv * 64:(kv + 1) * 64].unsqueeze(2).to_broadcast([128, 16, 4, 64]),
                              in1=q4.unsqueeze(1).to_broadcast([128, 16, 4, 64]), op=ALU.mult)
                            V('tensor_reduce', [prod.k()], [Ss.k()], out=Ss.ap[:, :, kv * 4:(kv + 1) * 4],
                              in_=prod.ap.rearrange("p k (h d) -> p k h d", h=4), axis=AX.X, op=ALU.add)
                            V('tensor_tensor', [zrep.k(), Ss.k()], [prod.k()], out=prod.ap[:, 0, :].rearrange("p (h d) -> p h d", h=4),
                              in0=q4, in1=kz[:, kv * 64:(kv + 1) * 64].unsqueeze(1).to_broadcast([128, 4, 64]), op=ALU.mult)
                            V('tensor_reduce', [prod.k()], [smk], out=snew[:, kv * 4:(kv + 1) * 4],
                              in_=prod.ap[:, 0, :].rearrange("p (h d) -> p h d", h=4), axis=AX.X, op=ALU.add)
                    else:
                        V('tensor_tensor', Kk + [zrep.k(), sbs.k()], [prod.k()], out=prod.ap, in0=Ks.ap,
                          in1=qz.unsqueeze(1).to_broadcast([128, 16, 256]), op=ALU.mult)
                        V('tensor_reduce', [prod.k()], [Ss.k()], out=Ss.ap, in_=prod.ap.rearrange("p k (h d) -> p k h d", h=4),
                          axis=AX.X, op=ALU.add)
                        V('tensor_tensor', [zrep.k(), Ss.k()], [prod.k()], out=prod.ap[:, 0, :], in0=qz, in1=kz, op=ALU.mult)
                        V('tensor_reduce', [prod.k()], [smk], out=snew, in_=prod.ap[:, 0, :].rearrange("p (h d) -> p h d", h=4),
                          axis=AX.X, op=ALU.add)
                    V('scalar_tensor_tensor', [Ss.k(), sbs.k()], [Ss.k()], out=Ss.ap, in0=Ss.ap, scalar=0.125, in1=sbs.ap,
                      op0=ALU.mult, op1=ALU.add)
                    V('tensor_scalar', [smk], [smk], out=snew, in0=snew, scalar1=0.125, scalar2=None, op0=ALU.mult)
                    V('tensor_reduce', [Ss.k()], [smk], out=lm, in_=Ss.ap.rearrange("p k h -> p h k"), axis=AX.X, op=ALU.max)
                    V('tensor_tensor', [smk], [smk], out=lm, in0=lm, in1=snew, op=ALU.max)
                    if a == 0:
                        V('tensor_tensor', [smk, sink.k()], [smk], out=lm, in0=lm, in1=sink.ap, op=ALU.max)
                    tp(ps[2][0:hq, 0:128], lm, identf.ap, [smk, identf.k()], ['ps2'])
                    gm = opart.ap[0:hq, 0:16]
                    V('tensor_reduce', ['ps2'], [opart.k()], out=gm, in_=ps[2][0:hq, 0:128].rearrange("h (kg b) -> h b kg", kg=8),
                      axis=AX.X, op=ALU.max)
                    gmr = opart.ap[0:hq, 128:256]
                    V('tensor_copy', [opart.k()], [opart.k()], out=gmr.rearrange("h (kg b) -> h kg b", kg=8),
                      in_=gm.unsqueeze(1).to_broadcast([hq, 8, 16]))
                    tp(ps[2][:, 256:256 + hq], gmr, identf.ap[0:hq, 0:hq], [opart.k(), identf.k()], ['ps2'])
                    V('tensor_scalar', ['ps2'], [smk], out=negM, in0=ps[2][:, 256:256 + hq], scalar1=-1.0, scalar2=None, op0=ALU.mult)
                    V('tensor_tensor', [Ss.k(), smk], [Ss.k()], out=Ss.ap, in0=Ss.ap, in1=negM.unsqueeze(1).to_broadcast([128, 16, hq]),
                      op=ALU.add)
                    act(Ss.ap, Ss.ap, AF.Exp, [Ss.k()], [Ss.k()])
                    V('tensor_tensor', [smk], [smk], out=enew, in0=snew, in1=negM, op=ALU.add)
                    act(enew, enew, AF.Exp, [smk], [smk])
                    V('tensor_reduce', [Ss.k()], [smk], out=lsum, in_=Ss.ap.rearrange("p k h -> p h k"), axis=AX.X, op=ALU.add)
                    V('tensor_scalar', [smk], [smk], out=enew, in0=enew, scalar1=0.125, scalar2=None, op0=ALU.mult)
                    V('tensor_tensor', [smk], [smk], out=tot, in0=lsum, in1=enew, op=ALU.add)
                    if a == 0:
                        esk = sm.ap[:, 80:88]
                        V('tensor_tensor', [smk, sink.k()], [smk], out=esk, in0=sink.ap, in1=negM, op=ALU.add)
                        act(esk, esk, AF.Exp, [smk], [smk])
                        V('scalar_tensor_tensor', [smk], [smk], out=tot, in0=esk, scalar=0.125, in1=tot, op0=ALU.mult, op1=ALU.add)
                    mm(ps[3][:, 0:hq], Gm.ap, tot, True, True, [Gm.k(), smk], ['ps3'])
                    V('tensor_copy', ['ps3'], [smk], out=denr, in_=ps[3][:, 0:hq])
                    V('reciprocal', [smk], [smk], out=rdn, in_=denr)
                    if a == 0:
                        for kv in range(2):
                            V('tensor_tensor', Vk + [Ss.k(), smk], [prod.k()], out=prod.ap.rearrange("p k (h d) -> p k h d", h=4),
                              in0=Vs.ap[:, :, kv * 64:(kv + 1) * 64].unsqueeze(2).to_broadcast([128, 16, 4, 64]),
                              in1=Ss.ap[:, :, kv * 4:(kv + 1) * 4].unsqueeze(3).to_broadcast([128, 16, 4, 64]), op=ALU.mult)
                            V('tensor_reduce', [prod.k()], [opart.k()], out=opart.ap[:, kv * 256:(kv + 1) * 256],
                              in_=prod.ap.rearrange("p k f -> p f k"), axis=AX.X, op=ALU.add)
                            V('tensor_tensor', [zrep.k(), smk, opart.k()], [prod.k()], out=prod.ap[:, 0, :].rearrange("p (h d) -> p h d", h=4),
                              in0=vz[:, kv * 64:(kv + 1) * 64].unsqueeze(1).to_broadcast([128, 4, 64]),
                              in1=enew[:, kv * 4:(kv + 1) * 4].unsqueeze(2).to_broadcast([128, 4, 64]), op=ALU.mult)
                            V('tensor_tensor', [prod.k(), opart.k()], [opart.k()], out=opart.ap[:, kv * 256:(kv + 1) * 256],
                              in0=opart.ap[:, kv * 256:(kv + 1) * 256], in1=prod.ap[:, 0, :], op=ALU.add)
                    else:
                        V('tensor_tensor', Vk + [Ss.k(), smk], [prod.k()], out=prod.ap.rearrange("p k (h d) -> p k h d", h=4),
                          in0=Vs.ap.rearrange("p k (h d) -> p k h d", h=4), in1=Ss.ap.unsqueeze(3).to_broadcast([128, 16, 4, 64]),
                          op=ALU.mult)
                        V('tensor_reduce', [prod.k()], [opart.k()], out=opart.ap[:, 0:256], in_=prod.ap.rearrange("p k f -> p f k"),
                          axis=AX.X, op=ALU.add)
                        V('tensor_tensor', [zrep.k(), smk, opart.k()], [prod.k()], out=prod.ap[:, 0, :].rearrange("p (h d) -> p h d", h=4),
                          in0=vz.rearrange("p (h d) -> p h d", h=4), in1=enew.unsqueeze(2).to_broadcast([128, 4, 64]), op=ALU.mult)
                        V('tensor_tensor', [prod.k(), opart.k()], [opart.k()], out=opart.ap[:, 0:256], in0=opart.ap[:, 0:256],
                          in1=prod.ap[:, 0, :], op=ALU.add)
                    mm(ps[0][:, 0:nq], Gm.ap, opart.ap[:, 0:nq], True, True, [Gm.k(), opart.k()], ['ps0'])
                    ots = o_tok[0]
                    V('tensor_tensor', ['ps0', smk], [ots.k()], out=ots.ap[0:16, 0:nq].rearrange("p (h d) -> p h d", h=hq),
                      in0=ps[0][0:16, 0:nq].rearrange("p (h d) -> p h d", h=hq),
                      in1=rdn[0:16, :].unsqueeze(2).to_broadcast([16, hq, 64]), op=ALU.mult)
                    for c in range(nq // 128):
                        tp(psb[3][:, c * 16:(c + 1) * 16], ots.ap[0:16, c * 128:(c + 1) * 128], identb.ap[0:16, 0:16],
                           [ots.k(), identb.k()], ['ps3'])
                    V('tensor_copy', ['ps3'], [oT[a].k('s')], out=oT[a].ap[:, :, SEQ:TC],
                      in_=psb[3][:, 0:(nq // 128) * 16].rearrange("p (c s) -> p c s", c=nq // 128))
                    if a > 0:
                        act(lnd_s, denr, AF.Ln, [smk], [smk])
                        lts = lse_tok[0]
                        V('tensor_tensor', [smk], [lts.k()], out=lts.ap[0:16, 0:4], in0=lnd_s[0:16, :], in1=negM[0:16, :], op=ALU.subtract)
                        tp(ps[2][0:4, 0:16], lts.ap[0:16, 0:4], identf.ap[0:16, 0:16], [lts.k(), identf.k()], ['ps2'])
                        V('tensor_copy', ['ps2'], [lseT.k(a, 's')], out=lseT.ap[0:4, a - 1, SEQ:TC], in_=ps[2][0:4, 0:16])

            chk('B%d' % p)
            GT_LO = R1_LO + 17 * 4096
            gT = A.carve("gT", GT_LO, [128, 8, TC], BF16)
            RM = Region(A, GT_LO + 8 * TC * 2, END)
            wbs = RM.alloc("wbs", [128, 4, 1024], BF16)
            wbd = RM.alloc("wbd", [128, 2, 1024], BF16)
            wga = RM.alloc("wga", [128, 8, 512], BF16)
            wgb = RM.alloc("wgb", [128, 8, 512], BF16)
            cM = RM.alloc("cM", [4, 512], F32)
            cS = RM.alloc("cS", [4, 512], F32)
            tf = [RM.alloc("tf%d" % i, [128, 512], F32) for i in range(4)]
            DM('pool', wbs.ap, w_br_swa.rearrange("(k p) n -> p k n", p=128), w=[wbs.k()])
            DM('pool', wbd.ap, w_br_dil.rearrange("(k p) n -> p k n", p=128), w=[wbd.k()])
            for ti, (c0, wd) in enumerate(mtiles):
                lk = [lseT.k()]
                L = lseT.ap[0:4, :, c0:c0 + wd]
                lse_keys = []
                for a_ in (1, 2, 3):
                    dd = TYPES[a_]['d']
                    nbb = 16 // dd
                    if c0 >= SEQ:
                        lse_keys.append(lseT.k(a_, 's'))
                    else:
                        for r_ in range(dd):
                            for n in range(nbb):
                                t0 = r_ + dd * 128 * n
                                if t0 < c0 + wd and t0 + dd * 127 >= c0:
                                    lse_keys.append(lseT.k(a_, r_ * nbb + n))
                ck = lseT.k('c', ti)
                V('tensor_tensor', lse_keys, [cM.k()], out=cM.ap[:, 0:wd], in0=L[:, 0, :], in1=L[:, 1, :], op=ALU.max)
                V('tensor_tensor', lse_keys + [cM.k()], [cM.k()], out=cM.ap[:, 0:wd], in0=cM.ap[:, 0:wd], in1=L[:, 2, :], op=ALU.max)
                V('tensor_tensor', lse_keys + [cM.k()], [ck], out=L, in0=L, in1=cM.ap[:, 0:wd].unsqueeze(1).to_broadcast([4, 3, wd]),
                  op=ALU.subtract)
                act(L, L, AF.Exp, [ck], [ck])
                V('tensor_tensor', [ck], [cS.k()], out=cS.ap[:, 0:wd], in0=L[:, 0, :], in1=L[:, 1, :], op=ALU.add)
                V('tensor_tensor', [ck, cS.k()], [cS.k()], out=cS.ap[:, 0:wd], in0=cS.ap[:, 0:wd], in1=L[:, 2, :], op=ALU.add)
                V('reciprocal', [cS.k()], [cS.k()], out=cS.ap[:, 0:wd], in_=cS.ap[:, 0:wd])
                V('tensor_tensor', [ck, cS.k()], [ck], out=L, in0=L, in1=cS.ap[:, 0:wd].unsqueeze(1).to_broadcast([4, 3, wd]), op=ALU.mult)

                def okeys(a_):
                    dd = TYPES[a_]['d']
                    nbb = 16 // dd
                    if c0 >= SEQ:
                        return [oT[a_].k('s')]
                    out = []
                    for r_ in range(dd):
                        for n in range(nbb):
                            t0 = r_ + dd * 128 * n
                            if t0 < c0 + wd and t0 + dd * 127 >= c0:
                                out.append(oT[a_].k(r_ * nbb + n))
                    return out
                for c in range(2):
                    for g in range(3):
                        mm(ps[g][:, 0:wd], selH[c].ap, lseT.ap[0:4, g, c0:c0 + wd], True, True, [selH[c].k(), ck], ['ps%d' % g])
                    t0_, t1_ = tf[0], tf[1]
                    V('tensor_tensor', okeys(1) + ['ps0'], [t0_.k()], out=t0_.ap[:, 0:wd], in0=oT[1].ap[:, c, c0:c0 + wd], in1=ps[0][:, 0:wd], op=ALU.mult)
                    V('tensor_tensor', okeys(2) + ['ps1'], [t1_.k()], out=t1_.ap[:, 0:wd], in0=oT[2].ap[:, c, c0:c0 + wd], in1=ps[1][:, 0:wd], op=ALU.mult)
                    V('tensor_tensor', [t0_.k(), t1_.k()], [t0_.k()], out=t0_.ap[:, 0:wd], in0=t0_.ap[:, 0:wd], in1=t1_.ap[:, 0:wd], op=ALU.add)
                    V('tensor_tensor', okeys(3) + ['ps2'], [t1_.k()], out=t1_.ap[:, 0:wd], in0=oT[3].ap[:, c, c0:c0 + wd], in1=ps[2][:, 0:wd], op=ALU.mult)
                    V('tensor_tensor', [t0_.k(), t1_.k()] + okeys(1), [oT[1].k('comb', ti, c)], out=oT[1].ap[:, c, c0:c0 + wd],
                      in0=t0_.ap[:, 0:wd], in1=t1_.ap[:, 0:wd], op=ALU.add)

            def swa_keys(c0, wd):
                if c0 >= SEQ:
                    return [oT[0].k('s')]
                return [oT[0].k(t) for t in range(c0 // 128, (c0 + wd - 1) // 128 + 1)]

            it = 0
            for q4 in range(2):
                DM('pool', wga.ap, w_in[:, 3072 + q4 * 512:3072 + (q4 + 1) * 512].rearrange("(k p) n -> p k n", p=128), w=[wga.k()])
                DM('pool', wgb.ap, w_in[:, 4096 + q4 * 512:4096 + (q4 + 1) * 512].rearrange("(k p) n -> p k n", p=128), w=[wgb.k()])
                for ti, (c0, wd) in enumerate(mtiles):
                    for oc4 in range(4):
                        oc = q4 * 4 + oc4
                        b0 = (it % 2) * 4
                        it += 1
                        pk = ['ps%d' % (b0 + i) for i in range(4)]
                        for kc in range(8):
                            mm(ps[b0][:, 0:wd], wga.ap[:, kc, oc4 * 128:(oc4 + 1) * 128], hT.ap[:, kc, c0:c0 + wd], kc == 0, kc == 7,
                               [wga.k()] + hk(c0, c0 + wd), [pk[0]])
                        for kc in range(8):
                            mm(ps[b0 + 1][:, 0:wd], wgb.ap[:, kc, oc4 * 128:(oc4 + 1) * 128], hT.ap[:, kc, c0:c0 + wd], kc == 0, kc == 7,
                               [wgb.k()] + hk(c0, c0 + wd), [pk[1]])
                        for k4 in range(4):
                            mm(ps[b0 + 2][:, 0:wd], wbs.ap[:, k4, oc * 128:(oc + 1) * 128], oT[0].ap[:, k4, c0:c0 + wd], k4 == 0, k4 == 3,
                               [wbs.k()] + swa_keys(c0, wd), [pk[2]])
                        for k2 in range(2):
                            mm(ps[b0 + 3][:, 0:wd], wbd.ap[:, k2, oc * 128:(oc + 1) * 128], oT[1].ap[:, k2, c0:c0 + wd], k2 == 0, k2 == 1,
                               [wbd.k(), oT[1].k('comb', ti, 0), oT[1].k('comb', ti, 1)], [pk[3]])
                        sa, sb2, ta, tb2 = tf[0], tf[1], tf[2], tf[3]
                        act(sa.ap[:, 0:wd], ps[b0][:, 0:wd], AF.Sigmoid, [pk[0]], [sa.k()])
                        act(sb2.ap[:, 0:wd], ps[b0 + 1][:, 0:wd], AF.Sigmoid, [pk[1]], [sb2.k()])
                        V('tensor_tensor', [sa.k(), pk[2]], [ta.k()], out=ta.ap[:, 0:wd], in0=sa.ap[:, 0:wd], in1=ps[b0 + 2][:, 0:wd], op=ALU.mult)
                        V('tensor_tensor', [sb2.k(), pk[3]], [tb2.k()], out=tb2.ap[:, 0:wd], in0=sb2.ap[:, 0:wd], in1=ps[b0 + 3][:, 0:wd], op=ALU.mult)
                        V('tensor_tensor', [ta.k(), tb2.k()], [gT.k(oc, ti)], out=gT.ap[:, oc, c0:c0 + wd], in0=ta.ap[:, 0:wd],
                          in1=tb2.ap[:, 0:wd], op=ALU.add)

            chk('M1_%d' % p)
            Yacc = A.carve("Yacc", R1_LO, [128, 17, 1024], F32)
            RM2 = Region(A, GT_LO + 8 * TC * 2, END)
            gbc = RM2.alloc("gbc", [128, 2, 1024], F32)
            lnbc = RM2.alloc("lnbc", [128, 2, 1024], F32)
            xt2 = RM2.alloc("xt2", [128, 1024], F32)
            xm = RM2.alloc("xm", [128, 1024], F32)
            wts = RM2.alloc("wts", [128, 17, 64], F32)
            RE2_LO = RM2.p
            wout = RM2.alloc("wout", [128, 8, 1024], BF16)
            h2f = RM2.alloc("h2f", [128, 8, 128], F32)
            wr = RM2.alloc("wr", [128, 8, 64], F32)
            rbias = RM2.alloc("rbias", [128, 64], F32)
            rt = RM2.alloc("rt", [128, 6, 64], F32)
            DM('pool', wout.ap, w_out.rearrange("(k p) n -> p k n", p=128), w=[wout.k()])
            DM('sp', lnbc.ap[:, 0, :], ln1_g.to_broadcast([128, 1024]), w=[lnbc.k()])
            DM('sp', lnbc.ap[:, 1, :], ln1_b.to_broadcast([128, 1024]), w=[lnbc.k()])
            DM('sp', wr.ap, w_router.rearrange("(k p) n -> p k n", p=128), w=[wr.k()])
            DM('sp', rbias.ap, router_bias.to_broadcast([128, 64]), w=[rbias.k()])
            for n4 in range(4):
                mm(ps[n4][:, :], selP[p].ap, modg.ap[0:18, n4 * 512:(n4 + 1) * 512], True, True, [selP[p].k(), modg.k()], ['ps%d' % n4])
                evac(n4, gbc.ap[:, n4 // 2, (n4 % 2) * 512:(n4 % 2 + 1) * 512], ps[n4][:, :], ['ps%d' % n4], [gbc.k()])
            for t, (c0, P_) in enumerate(stiles):
                smp = (c0 >= SEQ)
                ti = 4 if smp else c0 // 512
                if smp:
                    DM('sp', xt2.ap[0:16, :], xs[:, :], w=[xt2.k()])
                else:
                    DM('sp', xt2.ap, xp[p, c0:c0 + 128, :], w=[xt2.k()])
                b0 = (t % 2) * 4
                for nh in range(2):
                    for kc in range(8):
                        mm(ps[b0 + nh][0:P_, :], gT.ap[:, kc, c0:c0 + P_], wout.ap[:, kc, nh * 512:(nh + 1) * 512], kc == 0, kc == 7,
                           [gT.k(kc, ti), wout.k()], ['ps%d' % (b0 + nh)])
                    g1 = modg.ap[0:16, nh * 512:(nh + 1) * 512] if smp else gbc.ap[:, 0, nh * 512:(nh + 1) * 512]
                    V('tensor_tensor', ['ps%d' % (b0 + nh), gbc.k(), modg.k()], [xm.k()], out=xm.ap[0:P_, nh * 512:(nh + 1) * 512],
                      in0=ps[b0 + nh][0:P_, :], in1=g1, op=ALU.mult)
                V('scalar_tensor_tensor', [xt2.k(), xm.k()], [xm.k()], out=xm.ap[0:P_, :], in0=xt2.ap[0:P_, :], scalar=ALPHA,
                  in1=xm.ap[0:P_, :], op0=ALU.mult, op1=ALU.add)
                rstd, nmr, k = ln_stats(xm.ap[0:P_, :], P_, [xm.k()])
                act(xm.ap[0:P_, :], xm.ap[0:P_, :], AF.Identity, [xm.k(), k], [xm.k()], bias=nmr, scale=rstd)
                V('tensor_tensor', [xm.k(), lnbc.k()], [xm.k()], out=xm.ap[0:P_, :], in0=xm.ap[0:P_, :], in1=lnbc.ap[0:P_, 0, :], op=ALU.mult)
                V('tensor_tensor', [xm.k(), lnbc.k()], [xm.k()], out=xm.ap[0:P_, :], in0=xm.ap[0:P_, :], in1=lnbc.ap[0:P_, 1, :], op=ALU.add)
                act(Yacc.ap[0:P_, t, :], xm.ap[0:P_, :], AF.Identity, [xm.k()], [Yacc.k(t)], scale=ALPHA)
                rstd, nmr, k = ln_stats(xm.ap[0:P_, :], P_, [xm.k()])
                act(xt2.ap[0:P_, :], xm.ap[0:P_, :], AF.Identity, [xm.k(), k], [xt2.k()], bias=nmr, scale=rstd)
                for c in range(8):
                    tp(ps[b0 + 2 + c // 4][:, (c % 4) * 128:(c % 4) * 128 + P_], xt2.ap[0:P_, c * 128:(c + 1) * 128],
                       identf.ap[0:P_, 0:P_], [xt2.k(), identf.k()], ['ps%d' % (b0 + 2 + c // 4)])
                for c in range(8):
                    src = ps[b0 + 2 + c // 4][:, (c % 4) * 128:(c % 4) * 128 + P_]
                    pk = 'ps%d' % (b0 + 2 + c // 4)
                    if smp:
                        V('tensor_tensor', [pk, opsc2T.k()], [h2f.k(c)], out=h2f.ap[:, c, 0:16], in0=src, in1=opsc2T.ap[:, c, 0:16], op=ALU.mult)
                        V('tensor_tensor', [h2f.k(c), modT.k()], [h2f.k(c)], out=h2f.ap[:, c, 0:16], in0=h2f.ap[:, c, 0:16],
                          in1=modT.ap[:, 24 + c, 0:16], op=ALU.add)
                    else:
                        sc_ = opsc2T.ap[:, c, pcol:pcol + 1]
                        sh_ = modT.ap[:, 24 + c, pcol:pcol + 1]
                        if c % 2 == 0:
                            act(h2f.ap[:, c, :], src, AF.Identity, [pk, opsc2T.k(), modT.k()], [h2f.k(c)], bias=sh_, scale=sc_)
                        else:
                            V('tensor_scalar', [pk, opsc2T.k(), modT.k()], [h2f.k(c)], out=h2f.ap[:, c, :], in0=src, scalar1=sc_,
                              scalar2=sh_, op0=ALU.mult, op1=ALU.add)
                h2k = [h2f.k(c) for c in range(8)]
                V('tensor_copy', h2k + [gT.k(kc, ti) for kc in range(0)], [hT.k(ti)], out=hT.ap[:, :, c0:c0 + P_], in_=h2f.ap[:, :, 0:P_])
                pr = 'ps%d' % (b0 + 2)
                for c in range(8):
                    mm(ps[b0 + 2][0:P_, 0:64], h2f.ap[:, c, 0:P_], wr.ap[:, c, :], c == 0, c == 7, h2k + [wr.k()], [pr])
                sc = rt.ap[0:P_, 0, :]
                bi = rt.ap[0:P_, 1, :]
                eq = rt.ap[0:P_, 2, :]
                msk = rt.ap[0:P_, 3, :]
                m1 = rt.ap[0:P_, 4, 0:8]
                m2 = rt.ap[0:P_, 4, 8:16]
                gs = rt.ap[0:P_, 4, 16:24]
                gs8 = rt.ap[0:P_, 4, 24:32]
                gmk = rt.ap[0:P_, 4, 32:40]
                s8 = rt.ap[0:P_, 4, 40:48]
                wsum = rt.ap[0:P_, 4, 48:49]
                rk_ = rt.k()
                act(sc, ps[b0 + 2][0:P_, 0:64], AF.Sigmoid, [pr], [rk_])
                V('tensor_tensor', [rk_, rbias.k()], [rk_], out=bi, in0=sc, in1=rbias.ap[0:P_, :], op=ALU.add)
                bi3 = bi.rearrange("p (g e) -> p g e", g=8)
                V('tensor_reduce', [rk_], [rk_], out=m1, in_=bi3, axis=AX.X, op=ALU.max)
                V('tensor_tensor', [rk_], [rk_], out=eq.rearrange("p (g e) -> p g e", g=8), in0=bi3,
                  in1=m1.unsqueeze(2).to_broadcast([P_, 8, 8]), op=ALU.is_equal)
                V('scalar_tensor_tensor', [rk_], [rk_], out=eq, in0=eq, scalar=-1e9, in1=bi, op0=ALU.mult, op1=ALU.add)
                V('tensor_reduce', [rk_], [rk_], out=m2, in_=eq.rearrange("p (g e) -> p g e", g=8), axis=AX.X, op=ALU.max)
                V('tensor_tensor', [rk_], [rk_], out=gs, in0=m1, in1=m2, op=ALU.add)
                V('max', [rk_], [rk_], out=gs8, in_=gs)
                V('tensor_scalar', [rk_], [rk_], out=gmk, in0=gs, scalar1=gs8[:, 3:4], scalar2=None, op0=ALU.is_ge)
                V('tensor_scalar', [rk_], [rk_], out=gmk, in0=gmk, scalar1=1e9, scalar2=-1e9, op0=ALU.mult, op1=ALU.add)
                V('tensor_tensor', [rk_], [rk_], out=msk.rearrange("p (g e) -> p g e", g=8), in0=bi3,
                  in1=gmk.unsqueeze(2).to_broadcast([P_, 8, 8]), op=ALU.add)
                V('max', [rk_], [rk_], out=s8, in_=msk)
                V('tensor_scalar', [rk_], [rk_], out=msk, in0=msk, scalar1=s8[:, 7:8], scalar2=None, op0=ALU.is_ge)
                V('tensor_tensor', [rk_], [rk_], out=msk, in0=msk, in1=sc, op=ALU.mult)
                V('tensor_reduce', [rk_], [rk_], out=wsum, in_=msk, axis=AX.X, op=ALU.add)
                V('reciprocal', [rk_], [rk_], out=wsum, in_=wsum)
                V('tensor_scalar', [rk_], [wts.k(t)], out=wts.ap[0:P_, t, :], in0=msk, scalar1=wsum, scalar2=2.5, op0=ALU.mult, op1=ALU.mult)

            chk('M2_%d' % p)
            RE = Region(A, GT_LO, GT_LO + 8 * TC * 2)
            Wg = [RE.alloc("Wg%d" % i, [128, 8, 256], BF16) for i in range(2)]
            Wu = [RE.alloc("Wu%d" % i, [128, 8, 256], BF16) for i in range(2)]
            Wd = [RE.alloc("Wd%d" % i, [128, 2, 1024], BF16) for i in range(2)]
            sg = [RE.alloc("sg%d" % i, [128, 512], F32) for i in range(2)]
            Hb = [RE.alloc("Hb%d" % i, [128, 2, 512], BF16) for i in range(2)]
            RE2 = Region(A, RE2_LO, END)
            Wds = [RE2.alloc("Wds%d" % i, [128, 2, 1024], BF16) for i in range(2)]
            ytmp = RE2.alloc("ytmp", [16, 512], F32)
            hi = 0
            yi = 0
            for e_ in range(NEXP + 1):
                sl = e_ % 2
                if p == 0 and e_ == 3:
                    for a_, T_ in enumerate(TYPES):
                        W_ = T_['W']
                        src = caches[a_][:, 1:W_].rearrange("b w t h d -> b (w t h d)")
                        dst = st_s[a_][:, 0:W_ - 1].rearrange("b w t h d -> b (w t h d)")
                        DM('act', dst, src, w=['bulkout%d' % a_], bulk=True)
                        outkeys.append('bulkout%d' % a_)
                if e_ < NEXP:
                    srcs = (w_eg[e_], w_eu[e_], w_ed[e_])
                else:
                    srcs = (w_sg, w_su, w_sd)
                DM('pool', Wg[sl].ap, srcs[0].rearrange("(k p) n -> p k n", p=128), w=[Wg[sl].k()])
                DM('pool', Wu[sl].ap, srcs[1].rearrange("(k p) n -> p k n", p=128), w=[Wu[sl].k()])
                DM('pool', Wd[sl].ap, srcs[2].rearrange("(k p) n -> p k n", p=128), w=[Wd[sl].k()])
                V('tensor_tensor', [Wd[sl].k(), gbc.k()], [Wds[sl].k()], out=Wds[sl].ap, in0=Wd[sl].ap,
                  in1=gbc.ap[:, 1, :].unsqueeze(1).to_broadcast([128, 2, 1024]), op=ALU.mult)
                for ti, (c0, wd) in enumerate(mtiles):
                    smp = (c0 >= SEQ)
                    hb = Hb[hi % 2]
                    hi += 1
                    for oc in range(2):
                        pg, pu = 'ps%d' % (oc * 2), 'ps%d' % (oc * 2 + 1)
                        for kc in range(8):
                            mm(ps[oc * 2][:, 0:wd], Wg[sl].ap[:, kc, oc * 128:(oc + 1) * 128], hT.ap[:, kc, c0:c0 + wd], kc == 0, kc == 7,
                               [Wg[sl].k(), hT.k(ti)], [pg])
                        for kc in range(8):
                            mm(ps[oc * 2 + 1][:, 0:wd], Wu[sl].ap[:, kc, oc * 128:(oc + 1) * 128], hT.ap[:, kc, c0:c0 + wd], kc == 0, kc == 7,
                               [Wu[sl].k(), hT.k(ti)], [pu])
                        act(sg[oc].ap[:, 0:wd], ps[oc * 2][:, 0:wd], AF.Silu, [pg], [sg[oc].k()])
                        V('tensor_tensor', [sg[oc].k(), pu], [hb.k(oc)], out=hb.ap[:, oc, 0:wd], in0=sg[oc].ap[:, 0:wd], in1=ps[oc * 2 + 1][:, 0:wd],
                          op=ALU.mult)
                    nsub = 1 if smp else 4
                    for j in range(nsub):
                        t = 16 if smp else ti * 4 + j
                        P_ = 16 if smp else 128
                        wsc = 1.0 if e_ == NEXP else wts.ap[0:P_, t, e_:e_ + 1]
                        wk = [] if e_ == NEXP else [wts.k(t)]
                        for nh in range(2):
                            bk = 4 + yi % 4
                            yi += 1
                            py = 'ps%d' % bk
                            wdn = Wd[sl] if smp else Wds[sl]
                            for k2 in range(2):
                                mm(ps[bk][0:P_, :], hb.ap[:, k2, j * 128:j * 128 + P_], wdn.ap[:, k2, nh * 512:(nh + 1) * 512], k2 == 0, k2 == 1,
                                   [hb.k(0), hb.k(1), wdn.k()], [py])
                            ydst = Yacc.ap[0:P_, t, nh * 512:(nh + 1) * 512]
                            if smp:
                                V('tensor_tensor', [py, modg.k()], [ytmp.k()], out=ytmp.ap, in0=ps[bk][0:16, :],
                                  in1=modg.ap[0:16, 1024 + nh * 512:1024 + (nh + 1) * 512], op=ALU.mult)
                                V('scalar_tensor_tensor', [ytmp.k(), Yacc.k(t, nh), Yacc.k(t)] + wk, [Yacc.k(t, nh)], out=ydst, in0=ytmp.ap, scalar=wsc,
                                  in1=ydst, op0=ALU.mult, op1=ALU.add)
                            else:
                                V('scalar_tensor_tensor', [py, Yacc.k(t, nh), Yacc.k(t)] + wk, [Yacc.k(t, nh)], out=ydst, in0=ps[bk][0:P_, :],
                                  scalar=wsc, in1=ydst, op0=ALU.mult, op1=ALU.add)

            chk('E_%d' % p)
            DM('sp', lnbc.ap[:, 0, :], ln2_g.to_broadcast([128, 1024]), w=[lnbc.k()])
            DM('sp', lnbc.ap[:, 1, :], ln2_b.to_broadcast([128, 1024]), w=[lnbc.k()])
            obuf = [xt2, xm]
            for t, (c0, P_) in enumerate(stiles):
                smp = (c0 >= SEQ)
                yk = [Yacc.k(t), Yacc.k(t, 0), Yacc.k(t, 1)]
                ya = Yacc.ap[0:P_, t, :]
                rstd, nmr, k = ln_stats(ya, P_, yk)
                ob = obuf[t % 2]
                act(ob.ap[0:P_, :], ya, AF.Identity, yk + [k], [ob.k()], bias=nmr, scale=rstd)
                V('tensor_tensor', [ob.k(), lnbc.k()], [ob.k()], out=ob.ap[0:P_, :], in0=ob.ap[0:P_, :], in1=lnbc.ap[0:P_, 0, :], op=ALU.mult)
                V('tensor_tensor', [ob.k(), lnbc.k()], [ob.k()], out=ob.ap[0:P_, :], in0=ob.ap[0:P_, :], in1=lnbc.ap[0:P_, 1, :], op=ALU.add)
                ok = 'y_%d_%d' % (p, t)
                if smp:
                    DM('sp', ys[:, :], ob.ap[0:16, :], r=[ob.k()], w=[ok])
                else:
                    DM('sp', yp[p, c0:c0 + 128, :], ob.ap, r=[ob.k()], w=[ok])
                outkeys.append(ok)

        R.stopped = False
        R.op('sp', lambda e: None, r=outkeys)
        R.emit(nc, st)
    return nc, R


_CACHE = {}


def kernel(x_prompt, x_sample, cache_swa_kv, cache_dil1_kv, cache_dil2_kv, cache_dil3_kv, c_prompt, c_sample,
           w_ada, b_ada, w_in, attn_sinks, w_br_swa, w_br_dil, w_out, ln1_g, ln1_b, w_router, router_bias,
           w_exp_gate, w_exp_up, w_exp_down, w_sh_gate, w_sh_up, w_sh_down, ln2_g, ln2_b):
    f = lambda a_: np.ascontiguousarray(np.asarray(a_, dtype=np.float32))
    if 'nc' not in _CACHE:
        _CACHE['nc'] = build_nc()[0]
    nc = _CACHE['nc']
    cach = [f(cache_swa_kv)[0], f(cache_dil1_kv)[0], f(cache_dil2_kv)[0], f(cache_dil3_kv)[0]]
    xpr, xsa = f(x_prompt), f(x_sample)
    cp, cs_ = f(c_prompt), f(c_sample)
    shared = dict(
        w_ada=f(w_ada)[0], b_ada=f(b_ada), w_in=f(w_in)[0], sinks=f(attn_sinks), w_br_swa=f(w_br_swa)[0],
        w_br_dil=f(w_br_dil)[0], w_out=f(w_out)[0], ln1_g=f(ln1_g), ln1_b=f(ln1_b), w_router=f(w_router)[0],
        router_bias=f(router_bias), w_eg=f(w_exp_gate)[0], w_eu=f(w_exp_up)[0], w_ed=f(w_exp_down)[0],
        w_sg=f(w_sh_gate)[0], w_su=f(w_sh_up)[0], w_sd=f(w_sh_down)[0], ln2_g=f(ln2_g), ln2_b=f(ln2_b))
    in_maps = []
    for c in range(NCORES):
        m = dict(shared)
        m['xp'] = xpr[2 * c:2 * c + 2]
        m['xs'] = xsa[16 * c:16 * c + 16, 0]
        m['c_all'] = np.ascontiguousarray(np.concatenate([cs_[16 * c:16 * c + 16], cp[2 * c:2 * c + 2]], axis=0))
        for a in range(4):
            m['cache%d' % a] = cach[a][16 * c:16 * c + 16]
        in_maps.append(m)
    res = run_bass_kernel_spmd(nc, in_maps, core_ids=list(range(NCORES)))
    rs = res.results
    y_prompt = np.concatenate([r['yp'] for r in rs], axis=0)
    y_sample = np.concatenate([r['ys'] for r in rs], axis=0)[:, None, :]
    outs = [y_prompt, y_sample]
    for a in range(4):
        outs.append(np.concatenate([r['stp%d' % a] for r in rs], axis=0)[None])
    for a in range(4):
        outs.append(np.concatenate([r['sts%d' % a] for r in rs], axis=0)[None])
    return tuple(np.ascontiguousarray(o, dtype=np.float32) for o in outs)
```

```python
import math
from contextlib import ExitStack

import numpy as np
import concourse.bass as bass
import concourse.mybir as mybir
from concourse.bass_utils import run_bass_kernel_spmd

F32 = mybir.dt.float32
BF16 = mybir.dt.bfloat16
I32 = mybir.dt.int32
AF = mybir.ActivationFunctionType
ALU = mybir.AluOpType
AX = mybir.AxisListType

NCORES = 8
D = 1024
SEQ = 2048
NSAMP = 16
TC = SEQ + NSAMP
NEXP = 64
ALPHA = 2.0 ** 0.25
LN_EPS = 1e-5
NEGBIG = -30000.0
import os
SAME_ENGINE_SYNC = os.environ.get('SES', '0') == '1'
NS = 8

TYPES = [
    dict(d=1, hq=8, hk=2, qo=0, ko=512, vo=640, W=128),
    dict(d=1, hq=4, hk=4, qo=768, ko=1536, vo=2304, W=128),
    dict(d=4, hq=4, hk=4, qo=1024, ko=1792, vo=2560, W=512),
    dict(d=16, hq=4, hk=4, qo=1280, ko=2048, vo=2816, W=2048),
]


def slopes_for(a):
    if a == 0:
        return [2.0 ** (-8.0 * (i + 1) / 8) for i in range(8)]
    g = a - 1
    return [2.0 ** (-8.0 * (4 * g + i + 1) / 12) for i in range(4)]


class Rec:
    def __init__(self):
        self.ops = []
        self.alias = {}
        self.stopped = False

    @staticmethod
    def _excl(r, w):
        r, w = list(r), list(w)
        w = w + [k for k in r if k.startswith('ps') and k not in w]
        r = [k for k in r if not k.startswith('ps')]
        return r, w

    def op(self, eng, fn, r=(), w=()):
        if self.stopped:
            return
        r, w = self._excl(r, w)
        self.ops.append(dict(eng=eng, fn=fn, r=tuple(r), w=tuple(w), dma=False, bulk=False))

    def dma(self, q, fn, r=(), w=(), bulk=False):
        if self.stopped:
            return
        r, w = self._excl(r, w)
        self.ops.append(dict(eng=q, fn=fn, r=tuple(r), w=tuple(w), dma=True, bulk=bulk))

    def emit(self, nc, stack):
        ops = self.ops
        last_w = {}
        readers = {}
        dreaders = {}
        by_base = {}
        def conf(k):
            b = k.split(':')[0]
            if k == b:
                return list(by_base.get(b, ())) + [k]
            return [k, b]

        for i, op in enumerate(ops):
            deps = set()
            for k in op['r']:
                for kk in conf(k):
                    if kk in last_w:
                        deps.add(last_w[kk])
            raw = set(deps)
            for k in op['w']:
                ks = conf(k)
                base = k.split(':')[0]
                for ob in self.alias.get(base, ()):
                    ks.extend(by_base.get(ob, ()))
                    ks.append(ob)
                for kk in ks:
                    if kk in last_w:
                        deps.add(last_w[kk])
                    deps.update(readers.get(kk, {}).values())
                    deps.update(dreaders.get(kk, ()))
            deps.discard(i)
            fdeps = set()
            for j in deps:
                oj = ops[j]
                if (not oj['dma']) and (not op['dma']) and oj['eng'] == op['eng']:
                    if op['eng'] == 'pe' or (op['eng'] != 'pool' and not SAME_ENGINE_SYNC and j not in raw):
                        continue
                fdeps.add(j)
            op['deps'] = fdeps
            for k in op['r']:
                by_base.setdefault(k.split(':')[0], set()).add(k)
                if op['dma']:
                    dreaders.setdefault(k, []).append(i)
                else:
                    readers.setdefault(k, {})[op['eng']] = i
            for k in op['w']:
                by_base.setdefault(k.split(':')[0], set()).add(k)
                last_w[k] = i
                readers[k] = {}
                dreaders[k] = []
        fin = ops[-1]
        lastc = {}
        lastd = {}
        for i, op in enumerate(ops[:-1]):
            if op['dma']:
                lastd.setdefault(op['eng'], []).append(i)
            else:
                lastc[op['eng']] = i
        fin['deps'].update(lastc.values())
        for q, lst in lastd.items():
            fin['deps'].update(lst[-NS:])
        needs = set()
        for op in ops:
            needs.update(op['deps'])
        engs = ['pe', 'act', 'dve', 'pool', 'sp']
        esem = {e: stack.enter_context(nc.semaphore('es_' + e)) for e in engs}
        dsem = {q: [stack.enter_context(nc.semaphore('ds_%s%d' % (q, i))) for i in range(NS)]
                for q in ['sp', 'act', 'pool']}
        bsem = stack.enter_context(nc.semaphore('bulk'))
        cnt = {e: 0 for e in engs}
        dcnt = {q: 0 for q in dsem}
        nbulk = sum(1 for op in ops if op['bulk'])
        for i, op in enumerate(ops):
            if op['bulk']:
                op['sig'] = (bsem, 16 * nbulk)
                op['pre'] = None
            elif op['dma']:
                q = op['eng']
                k = dcnt[q]
                dcnt[q] += 1
                op['sig'] = (dsem[q][k % NS], 16 * (k // NS + 1))
                op['pre'] = (dsem[q][k % NS], 16 * (k // NS)) if k >= NS else None
            elif i in needs:
                cnt[op['eng']] += 1
                op['sig'] = (esem[op['eng']], cnt[op['eng']])
            else:
                op['sig'] = None
        self.maxcnt = dict(cnt)
        block = stack.enter_context(nc.Block())

        def run(ename, eng):
            waited = {}
            for op in ops:
                if op['eng'] != ename:
                    continue
                ws = {}
                for j in op['deps']:
                    s, v = ops[j]['sig']
                    ws[id(s)] = (s, max(v, ws.get(id(s), (s, 0))[1]))
                if op['dma'] and op['pre'] is not None:
                    s, v = op['pre']
                    ws[id(s)] = (s, max(v, ws.get(id(s), (s, 0))[1]))
                for sid, (s, v) in ws.items():
                    if waited.get(sid, 0) >= v:
                        continue
                    eng.wait_ge(s, v)
                    waited[sid] = v
                inst = op['fn'](eng)
                if op['sig'] is not None and inst is not None:
                    inst.then_inc(op['sig'][0], 16 if op['dma'] else 1)

        @block.tensor
        def _(e):
            run('pe', e)

        @block.scalar
        def _(e):
            run('act', e)

        @block.vector
        def _(e):
            run('dve', e)

        @block.gpsimd
        def _(e):
            run('pool', e)

        @block.sync
        def _(e):
            run('sp', e)


class Buf:
    def __init__(self, key, ap, lo, hi):
        self.key, self.ap, self.lo, self.hi = key, ap, lo, hi

    def k(self, *sub):
        return self.key if not sub else self.key + ':' + '_'.join(str(s) for s in sub)


class Arena:
    def __init__(self, R, arena_ap, nbytes):
        self.R, self.arena, self.nbytes = R, arena_ap, nbytes
        self.live = []
        self.n = 0

    def carve(self, name, off, shape, dt):
        n = 1
        for s in shape[1:]:
            n *= s
        esz = 4 if dt in (F32, I32) else 2
        assert off % 4 == 0 and off + n * esz <= self.nbytes, (name, off, n * esz, self.nbytes)
        P = shape[0]
        if esz == 4:
            a = self.arena[0:P, off // 2: off // 2 + 2 * n].bitcast(dt)
        else:
            a = self.arena[0:P, off // 2: off // 2 + n]
        if len(shape) == 3:
            a = a.rearrange("p (a b) -> p a b", a=shape[1])
        elif len(shape) == 4:
            a = a.rearrange("p (a b c) -> p a b c", a=shape[1], b=shape[2])
        self.n += 1
        key = '%s#%d' % (name, self.n)
        lo, hi = off, off + n * esz
        al = [k for (k, l, h) in self.live if l < hi and lo < h]
        if al:
            self.R.alias[key] = al
        self.live.append((key, lo, hi))
        return Buf(key, a, lo, hi)


class Region:
    def __init__(self, arena, lo, hi):
        self.A, self.lo, self.hi, self.p = arena, lo, hi, lo

    def alloc(self, name, shape, dt):
        n = 1
        for s in shape[1:]:
            n *= s
        esz = 4 if dt in (F32, I32) else 2
        off = (self.p + 31) // 32 * 32
        assert off + n * esz <= self.hi, (name, off, n * esz, self.hi)
        self.p = off + n * esz
        return self.A.carve(name, off, shape, dt)


class _Stop(Exception):
    pass


STAGE = None


def build_nc():
    nc = bass.Bass("TRN2", target_bir_lowering=False)

    def din(name, shape):
        return nc.dram_tensor(name, list(shape), F32, kind="ExternalInput").ap()

    def dout(name, shape):
        return nc.dram_tensor(name, list(shape), F32, kind="ExternalOutput").ap()

    xp = din("xp", [2, SEQ, D])
    xs = din("xs", [NSAMP, D])
    c_all = din("c_all", [18, D])
    caches = [din("cache%d" % a, [NSAMP, T['W'], 2, T['hk'], 64]) for a, T in enumerate(TYPES)]
    w_ada = din("w_ada", [D, 6 * D])
    b_ada = din("b_ada", [1, 6 * D])
    w_in = din("w_in", [D, 5120])
    sinks = din("sinks", [1, 8])
    w_br_swa = din("w_br_swa", [512, D])
    w_br_dil = din("w_br_dil", [256, D])
    w_out = din("w_out", [D, D])
    ln1_g = din("ln1_g", [1, D])
    ln1_b = din("ln1_b", [1, D])
    w_router = din("w_router", [D, NEXP])
    router_bias = din("router_bias", [1, NEXP])
    w_eg = din("w_eg", [NEXP, D, 256])
    w_eu = din("w_eu", [NEXP, D, 256])
    w_ed = din("w_ed", [NEXP, 256, D])
    w_sg = din("w_sg", [D, 256])
    w_su = din("w_su", [D, 256])
    w_sd = din("w_sd", [256, D])
    ln2_g = din("ln2_g", [1, D])
    ln2_b = din("ln2_b", [1, D])

    yp = dout("yp", [2, SEQ, D])
    ys = dout("ys", [NSAMP, D])
    st_p = [dout("stp%d" % a, [2, T['W'], 2, T['hk'], 64]) for a, T in enumerate(TYPES)]
    st_s = [dout("sts%d" % a, [NSAMP, T['W'], 2, T['hk'], 64]) for a, T in enumerate(TYPES)]

    R = Rec()
    outkeys = []
    with ExitStack() as st:
        ARENA_BYTES = 206 * 1024
        arena_t = st.enter_context(nc.sbuf_tensor("arena", [128, ARENA_BYTES // 2], BF16))
        A = Arena(R, arena_t[:], ARENA_BYTES)
        ps = [st.enter_context(nc.psum_tensor("ps%d" % i, [128, 512], F32)) for i in range(8)]
        psb = [p[:].bitcast(BF16) for p in ps]

        def chk(name):
            if STAGE == name:
                R.stopped = True

        def mm(out, lhsT, rhs, start, stop, r, w):
            R.op('pe', lambda e: e.matmul(out, lhsT=lhsT, rhs=rhs, start=start, stop=stop), r, w)

        def tp(out, in_, ident, r, w):
            R.op('pe', lambda e: e.transpose(out=out, in_=in_, identity=ident), r, w)

        def act(out, in_, func, r, w, bias=None, scale=None, accum=None):
            kw = {}
            if bias is not None:
                kw['bias'] = bias
            if scale is not None:
                kw['scale'] = scale
            if accum is not None:
                kw['accum_out'] = accum
            R.op('act', lambda e: e.activation(out=out, in_=in_, func=func, **kw), r, w)

        def V(name, r, w, *args, **kw):
            R.op('dve', lambda e: getattr(e, name)(*args, **kw), r, w)

        def DM(q, out, in_, r=(), w=(), bulk=False):
            R.dma(q, lambda e: e.dma_start(out=out, in_=in_), r, w, bulk)

        def PL(name, r, w, *args, **kw):
            R.op('pool', lambda e: getattr(e, name)(*args, **kw), r, w)

        def evac(i, out, in_, r, w):
            if i % 2 == 0:
                R.op('act', lambda e: e.copy(out=out, in_=in_), r, w)
            else:
                R.op('dve', lambda e: e.tensor_copy(out=out, in_=in_), r, w)

        PS = Region(A, 0, 17408)
        identb = PS.alloc("identb", [128, 128], BF16)
        identf = PS.alloc("identf", [128, 128], F32)
        modT = PS.alloc("modT", [128, 48, 18], F32)
        opsc1T = PS.alloc("opsc1T", [128, 8, 18], F32)
        opsc2T = PS.alloc("opsc2T", [128, 8, 18], F32)
        sink = PS.alloc("sink", [128, 8], F32)
        nsink = PS.alloc("nsink", [128, 8], F32)
        modg = PS.alloc("modg", [18, 2048], F32)
        Gm = PS.alloc("Gm", [128, 128], F32)
        epst = PS.alloc("epst", [128, 1], F32)
        selH = [PS.alloc("selH%d" % c, [4, 128], F32) for c in range(2)]
        selP = [PS.alloc("selP%d" % p, [18, 128], F32) for p in range(2)]
        stat = PS.alloc("stat", [128, 64], F32)
        HT_LO = 17408
        hT = A.carve("hT", HT_LO, [128, 8, TC], BF16)
        R1_LO = HT_LO + 8 * TC * 2
        WORK_LO_A = R1_LO + 66048
        END = ARENA_BYTES

        def hk(c0, c1):
            return [hT.k(g) for g in range(c0 // 512, min((c1 - 1) // 512, 4) + 1)]


        PL('memset', [], [identf.k()], identf.ap, 0.0)
        PL('affine_select', [identf.k()], [identf.k()], out=identf.ap, in_=identf.ap, pattern=[[-1, 128]],
           compare_op=ALU.not_equal, fill=1.0, base=0, channel_multiplier=1)
        V('tensor_copy', [identf.k()], [identb.k()], out=identb.ap, in_=identf.ap)
        V('memset', [], [epst.k()], epst.ap, LN_EPS)
        e16 = stat.ap[:, 0:16]
        V('tensor_reduce', [identf.k()], [stat.k()], out=e16, in_=identf.ap.rearrange("p (kg b) -> p b kg", kg=8),
          axis=AX.X, op=ALU.add)
        V('tensor_copy', [stat.k()], [Gm.k()], out=Gm.ap.rearrange("p (kg b) -> p kg b", kg=8),
          in_=e16.unsqueeze(1).to_broadcast([128, 8, 16]))
        for c in range(2):
            V('tensor_copy', [identf.k()], [selH[c].k()], out=selH[c].ap.rearrange("p (h d) -> p h d", h=2),
              in_=identf.ap[0:4, 2 * c:2 * c + 2].unsqueeze(2).to_broadcast([4, 2, 64]))
        for p in range(2):
            V('tensor_copy', [identf.k()], [selP[p].k()], out=selP[p].ap,
              in_=identf.ap[0:18, 16 + p:17 + p].to_broadcast([18, 128]))
        DM('sp', sink.ap, sinks.to_broadcast([128, 8]), w=[sink.k()])
        V('tensor_scalar', [sink.k()], [nsink.k()], out=nsink.ap, in0=sink.ap, scalar1=-1.0, scalar2=None, op0=ALU.mult)

        B0 = Region(A, R1_LO, END)
        mod_tm = B0.alloc("mod_tm", [18, 6144], F32)
        bada = B0.alloc("bada", [1, 6144], F32)
        wa = [B0.alloc("wa%d" % i, [128, 8, 512], F32) for i in range(2)]
        cs = B0.alloc("cs", [18, 1024], F32)
        scT = B0.alloc("scT", [128, 8, 18], F32)
        ones18 = B0.alloc("ones18", [1, 18], F32)
        DM('sp', cs.ap, c_all[:, :], w=[cs.k()])
        DM('sp', bada.ap, b_ada[:, :], w=[bada.k()])
        V('memset', [], [ones18.k()], ones18.ap, 1.0)
        act(cs.ap, cs.ap, AF.Silu, [cs.k()], [cs.k()])
        for kc in range(8):
            tp(ps[0][:, kc * 18:(kc + 1) * 18], cs.ap[0:18, kc * 128:(kc + 1) * 128], identf.ap[0:18, 0:18],
               [cs.k(), identf.k()], ['ps0'])
        V('tensor_copy', ['ps0'], [scT.k()], out=scT.ap.rearrange("p a b -> p (a b)"), in_=ps[0][:, 0:144])
        for n in range(12):
            wb_ = wa[n % 2]
            DM('sp', wb_.ap, w_ada[:, n * 512:(n + 1) * 512].rearrange("(k p) n -> p k n", p=128), w=[wb_.k()])
            pk = 'ps%d' % (1 + n % 2)
            pt = ps[1 + n % 2]
            for kc in range(8):
                mm(pt[0:18, :], scT.ap[:, kc, :], wb_.ap[:, kc, :], kc == 0, False, [scT.k(), wb_.k()], [pk])
            mm(pt[0:18, :], ones18.ap, bada.ap[0:1, n * 512:(n + 1) * 512], False, True, [ones18.k(), bada.k()], [pk])
            evac(n, mod_tm.ap[:, n * 512:(n + 1) * 512], pt[0:18, :], [pk], [mod_tm.k(n)])
        mt_all = [mod_tm.k(n) for n in range(12)]
        V('tensor_copy', mt_all, [modg.k()], out=modg.ap[:, 0:1024], in_=mod_tm.ap[:, 2048:3072])
        V('tensor_copy', mt_all + [modg.k()], [modg.k()], out=modg.ap[:, 1024:2048], in_=mod_tm.ap[:, 5120:6144])
        for g3 in range(3):
            pk = 'ps%d' % (3 + g3 % 2)
            pt = ps[3 + g3 % 2]
            for j in range(16):
                c = g3 * 16 + j
                tp(pt[:, j * 18:(j + 1) * 18], mod_tm.ap[0:18, c * 128:(c + 1) * 128], identf.ap[0:18, 0:18],
                   mt_all + [identf.k()], [pk])
            V('tensor_copy', [pk], [modT.k()], out=modT.ap[:, g3 * 16:(g3 + 1) * 16, :].rearrange("p a b -> p (a b)"),
              in_=pt[:, 0:288])
        V('tensor_scalar', [modT.k()], [opsc1T.k()], out=opsc1T.ap, in0=modT.ap[:, 8:16, :], scalar1=1.0, scalar2=None,
          op0=ALU.add)
        V('tensor_scalar', [modT.k()], [opsc2T.k()], out=opsc2T.ap, in0=modT.ap[:, 32:40, :], scalar1=1.0, scalar2=None,
          op0=ALU.add)

        stat_slot = [0]

        def ln_stats(xap, P, rkeys):
            s = stat_slot[0] % 2
            stat_slot[0] += 1
            base = 16 + s * 24
            st6 = stat.ap[0:P, base:base + 12]
            mv = stat.ap[0:P, base + 12:base + 14]
            lnv = stat.ap[0:P, base + 14:base + 15]
            rstd = stat.ap[0:P, base + 15:base + 16]
            nmr = stat.ap[0:P, base + 16:base + 17]
            k = stat.k('ln', s)
            V('bn_stats', rkeys, [k], out=st6[:, 0:6], in_=xap[:, 0:512])
            V('bn_stats', rkeys + [k], [k], out=st6[:, 6:12], in_=xap[:, 512:1024])
            V('bn_aggr', [k], [k], out=mv, in_=st6)
            act(lnv, mv[:, 1:2], AF.Ln, [k, epst.k()], [k], bias=epst.ap[0:P, :], scale=1.0)
            act(rstd, lnv, AF.Exp, [k], [k], scale=-0.5)
            V('tensor_scalar', [k], [k], out=nmr, in0=mv[:, 0:1], scalar1=rstd, scalar2=-1.0, op0=ALU.mult, op1=ALU.mult)
            return rstd, nmr, k

        chk('p0')
        for p in range(2):
            has_s = (p == 1)
            ncol = TC if has_s else SEQ
            pcol = 16 + p
            mtiles = [(tt * 512, 512) for tt in range(4)] + ([(SEQ, NSAMP)] if has_s else [])
            stiles = [(t * 128, 128) for t in range(16)] + ([(SEQ, NSAMP)] if has_s else [])

            oT = [A.carve("oT0", R1_LO, [128, 4, TC], BF16)]
            off = R1_LO + 4 * TC * 2
            for g in range(3):
                oT.append(A.carve("oT%d" % (g + 1), off, [128, 2, TC], BF16))
                off += 2 * TC * 2
            lseT = A.carve("lseT", off, [4, 3, TC], F32)
            off += 3 * TC * 4
            assert off == WORK_LO_A

            RA = Region(A, WORK_LO_A, END)
            xt = [RA.alloc("xt%d" % i, [128, 1024], F32) for i in range(2)]
            xnb = [RA.alloc("xnb%d" % i, [128, 1024], BF16) for i in range(4)]
            for g4 in range(4):
                for j in range(4):
                    t = g4 * 4 + j
                    xb_ = xt[t % 2]
                    DM('sp', xb_.ap, xp[p, t * 128:(t + 1) * 128, :], w=[xb_.k()])
                    rstd, nmr, k = ln_stats(xb_.ap, 128, [xb_.k()])
                    chk('Aa')
                    act(xnb[j].ap, xb_.ap, AF.Identity, [xb_.k(), k], [xnb[j].k()], bias=nmr, scale=rstd)
                    chk('Ab')
                for c in range(8):
                    for j in range(4):
                        tp(psb[c][:, j * 128:(j + 1) * 128],
                           xnb[j].ap[:, c * 128:(c + 1) * 128], identb.ap, [xnb[j].k(), identb.k()], ['ps%d' % c])
                chk('Ac')
                for c in range(8):
                    src = psb[c][:, 0:512]
                    dst = hT.ap[:, c, g4 * 512:(g4 + 1) * 512]
                    sc_ = opsc1T.ap[:, c, pcol:pcol + 1]
                    sh_ = modT.ap[:, c, pcol:pcol + 1]
                    rk = ['ps%d' % c, opsc1T.k(), modT.k()]
                    if c % 2 == 0:
                        act(dst, src, AF.Identity, rk, [hT.k(g4)], bias=sh_, scale=sc_)
                    else:
                        V('tensor_scalar', rk, [hT.k(g4)], out=dst, in0=src, scalar1=sc_, scalar2=sh_, op0=ALU.mult, op1=ALU.add)
                chk('Ad')
            if has_s:
                xs_t = xt[0]
                DM('sp', xs_t.ap[0:16, :], xs[:, :], w=[xs_t.k()])
                rstd, nmr, k = ln_stats(xs_t.ap[0:16, :], 16, [xs_t.k()])
                act(xnb[0].ap[0:16, :], xs_t.ap[0:16, :], AF.Identity, [xs_t.k(), k], [xnb[0].k()], bias=nmr, scale=rstd)
                for c in range(8):
                    tp(psb[0][:, c * 16:(c + 1) * 16], xnb[0].ap[0:16, c * 128:(c + 1) * 128], identb.ap[0:16, 0:16],
                       [xnb[0].k(), identb.k()], ['ps0'])
                tmp_s = RA.alloc("tmp_s", [128, 8, 16], F32)
                V('tensor_tensor', ['ps0', opsc1T.k()], [tmp_s.k()], out=tmp_s.ap,
                  in0=psb[0][:, 0:128].rearrange("p (c s) -> p c s", c=8), in1=opsc1T.ap[:, :, 0:16], op=ALU.mult)
                V('tensor_tensor', [tmp_s.k(), modT.k()], [hT.k(4)], out=hT.ap[:, :, SEQ:TC], in0=tmp_s.ap,
                  in1=modT.ap[:, 0:8, 0:16], op=ALU.add)

            chk('A%d' % p)
            for a, T in enumerate(TYPES):
                d, hq, hk_, W = T['d'], T['hq'], T['hk'], T['W']
                nq, nk = hq * 64, hk_ * 64
                nb = 16 // d
                slopes = slopes_for(a)
                RB = Region(A, WORK_LO_A, END)
                wblk = RB.alloc("wblk", [128, 8, 768], BF16)
                nqc = nq // 128
                nkc = 1 if a == 0 else 2
                o_tok = [RB.alloc("o_tok%d" % i, [128, 512], BF16) for i in range(2)]
                lse_tok = [RB.alloc("lse_tok%d" % i, [128, 4], F32) for i in range(2)]
                hst = [RB.alloc("hst%d" % i, [128, 16], F32) for i in range(6)]
                RS_LO = RB.p
                QK = RB.alloc("QK", [128, nqc + nkc, TC], BF16)
                Vt = RB.alloc("Vt", [128, 16, hk_, 65], BF16)
                bias = RB.alloc("bias", [128, hq, 256], F32)
                rv = RB.alloc("rv", [128, 256], F32)
                pen = RB.alloc("pen", [128, 256], F32)
                S_sb = [RB.alloc("S_sb%d" % i, [128, 512], F32) for i in range(3)]
                Pb = [RB.alloc("Pb%d" % i, [128, 512], BF16) for i in range(3)]
                PT = [RB.alloc("PT%d" % i, [128, 512], BF16) for i in range(3)]
                kvst = [RB.alloc("kvst%d" % i, [128, 512], F32) for i in range(2)]

                for (o0, n0, c0) in ((T['qo'], nq, 0), (T['ko'], nk, nq), (T['vo'], nk, nq + nk)):
                    if a == 0 and c0 == 0:
                        for hh in range(8):
                            dc = ((hh % 4) * 2 + hh // 4) * 64
                            DM('pool', wblk.ap[:, :, dc:dc + 64],
                               w_in[:, hh * 64:(hh + 1) * 64].rearrange("(k p) n -> p k n", p=128), w=[wblk.k()])
                        continue
                    DM('pool', wblk.ap[:, :, c0:c0 + n0], w_in[:, o0:o0 + n0].rearrange("(k p) n -> p k n", p=128), w=[wblk.k()])
                ko_, vo_ = nq, nq + nk

                PL('iota', [], [rv.k()], rv.ap, pattern=[[-1, 256]], base=128, channel_multiplier=1,
                   allow_small_or_imprecise_dtypes=True)
                V('tensor_scalar', [rv.k()], [pen.k()], out=pen.ap, in0=rv.ap, scalar1=0.0, scalar2=None, op0=ALU.is_ge)
                V('tensor_scalar', [rv.k()], [S_sb[0].k()], out=S_sb[0].ap[:, 0:256], in0=rv.ap, scalar1=128.0, scalar2=None, op0=ALU.is_le)
                V('tensor_tensor', [pen.k(), S_sb[0].k()], [pen.k()], out=pen.ap, in0=pen.ap, in1=S_sb[0].ap[:, 0:256], op=ALU.mult)
                V('tensor_tensor', [rv.k(), pen.k()], [rv.k()], out=rv.ap, in0=rv.ap, in1=pen.ap, op=ALU.mult)
                V('tensor_scalar', [pen.k()], [pen.k()], out=pen.ap, in0=pen.ap, scalar1=-NEGBIG, scalar2=NEGBIG,
                  op0=ALU.mult, op1=ALU.add)
                for h in range(hq):
                    V('scalar_tensor_tensor', [rv.k(), pen.k()], [bias.k(h)], out=bias.ap[:, h, :], in0=rv.ap,
                      scalar=-slopes[h] * d, in1=pen.ap, op0=ALU.mult, op1=ALU.add)
                V('memset', [], [Vt.k('ones')], Vt.ap[:, :, :, 64:65], 1.0)

                chk('Ba')
                def qk_lhsT(ci, kc):
                    if a == 0:
                        if ci < 4:
                            return wblk.ap[:, kc, ci * 128:(ci + 1) * 128]
                        return wblk.ap[:, kc, 512:640]
                    if ci < 2:
                        return wblk.ap[:, kc, ci * 128:(ci + 1) * 128]
                    return wblk.ap[:, kc, 256 + (ci - 2) * 128:256 + (ci - 1) * 128]

                ei = 0
                for ci in range(nqc + nkc):
                    for (c0, wd) in mtiles:
                        bk = ei % 2
                        for kc in range(8):
                            mm(ps[bk][:, 0:wd], qk_lhsT(ci, kc), hT.ap[:, kc, c0:c0 + wd], kc == 0, kc == 7,
                               [wblk.k()] + hk(c0, c0 + wd), ['ps%d' % bk])
                        evac(ei, QK.ap[:, ci, c0:c0 + wd], ps[bk][:, 0:wd], ['ps%d' % bk], [QK.k(ci, c0 // 512)])
                        ei += 1

                chk('Bb')

                def qkk(ci, c0, c1):
                    return [QK.k(ci, g) for g in range(c0 // 512, (c1 - 1) // 512 + 1)]

                vi = 0
                for r_ in range(d):
                    for n in range(nb):
                        idx = r_ * nb + n
                        t0 = r_ + d * 128 * n
                        t1 = t0 + d * 127 + 1
                        need = (n == nb - 1)
                        bk = 2 + vi % 2
                        if need:
                            c0_, nn = ko_, 2 * nk
                        else:
                            c0_, nn = vo_, nk
                        for kc in range(8):
                            mm(ps[bk][:, 0:nn], hT.ap[:, kc, t0:t1:d], wblk.ap[:, kc, c0_:c0_ + nn], kc == 0, kc == 7,
                               [wblk.k()] + hk(t0, t1), ['ps%d' % bk])
                        vsrc = ps[bk][:, nn - nk:nn].rearrange("p (h d) -> p h d", h=hk_)
                        import os
                        EXP = os.environ.get('EXP', '0')
                        if EXP != 'a':
                            evac(vi, Vt.ap[:, idx, :, 0:64], vsrc, ['ps%d' % bk], [Vt.k(idx)])
                        if need and EXP != 'b':
                            kb = kvst[vi % 2]
                            evac(vi + 1, kb.ap[:, 0:nn], ps[bk][:, 0:nn], ['ps%d' % bk], [kb.k()])
                            keep = W
                            rs = t0 - (SEQ - keep)
                            dst = st_p[a][p, rs:rs + d * 127 + 1:d].rearrange("w t h d -> w (t h d)")
                            ok = 'stp%d_%d_%d' % (a, p, idx)
                            DM('sp', dst, kb.ap[:, 0:nn], r=[kb.k()], w=[ok])
                            outkeys.append(ok)
                        vi += 1

                chk('Bc')
                SA, PB = [4, 5, 0], [6, 7, 1]
                pairs = [(0, 1), (2, 1), (4, 1), (6, 1)] if a == 0 else [(0, 2), (1, 2)]

                def make_unit(u, idx, t0, t1, k0, nkeys, boff, vt_idx, ot, lt, h0, hstep, last):
                    sl = u % 3
                    sb_, pb_, ptb = S_sb[sl], Pb[sl], PT[sl]
                    hs = hst[u % 6]
                    negm2, es2, den2, rden2, lnd2, rs2 = (hs.ap[:, 2 * i:2 * i + 2] for i in range(6))
                    hsl = slice(h0, h0 + hstep + 1, hstep)
                    nbk = nkeys // 128
                    bS, bP = SA[sl], PB[sl]
                    pS, pT = 'ps%d' % bS, 'ps%d' % bP
                    heads = []
                    for j in range(2):
                        h = h0 + j * hstep
                        if a == 0:
                            heads.append((h % 4, 4, 64 * (h // 4), h // 4))
                        else:
                            heads.append((h // 2, 2 + h // 2, 64 * (h % 2), h))
                    sb3 = sb_.ap.rearrange("p (j k) -> p j k", j=2)[:, :, 0:nkeys]
                    pb3 = pb_.ap.rearrange("p (j k) -> p j k", j=2)

                    def s1a():
                        for j, (qci, kci, base, kvh) in enumerate(heads):
                            mm(ps[bS][:, j * 256:j * 256 + nkeys], QK.ap[base:base + 64, qci, t0:t1:d],
                               QK.ap[base:base + 64, kci, k0:t1:d], True, True, qkk(qci, t0, t1) + qkk(kci, k0, t1), [pS])

                    def s1b():
                        V('scalar_tensor_tensor', [pS, bias.k(h0), bias.k(h0 + hstep)], [sb_.k()], out=sb3,
                          in0=ps[bS][:, :].rearrange("p (j k) -> p j k", j=2)[:, :, 0:nkeys],
                          scalar=0.125, in1=bias.ap[:, hsl, boff:boff + nkeys], op0=ALU.mult, op1=ALU.add)
                        V('tensor_reduce', [sb_.k()], [hs.k()], out=negm2, in_=sb3, axis=AX.X, op=ALU.max, negate=True)
                        if a == 0:
                            V('tensor_tensor', [hs.k(), nsink.k()], [hs.k()], out=negm2, in0=negm2, in1=nsink.ap[:, hsl], op=ALU.min)
                            V('tensor_tensor', [hs.k(), sink.k()], [hs.k()], out=es2, in0=negm2, in1=sink.ap[:, hsl], op=ALU.add)
                        for j in range(2):
                            act(pb3[:, j, 0:nkeys], sb3[:, j, :], AF.Exp, [sb_.k(), hs.k()], [pb_.k(), hs.k('rs')], bias=negm2[:, j:j + 1],
                                scale=1.0, accum=rs2[:, j:j + 1])
                        if a == 0:
                            act(es2, es2, AF.Exp, [hs.k()], [hs.k()])

                    def s2b():
                        if a == 0:
                            V('tensor_tensor', [hs.k(), hs.k('rs')], [hs.k()], out=den2, in0=rs2, in1=es2, op=ALU.add)
                        else:
                            V('tensor_copy', [hs.k(), hs.k('rs')], [hs.k()], out=den2, in_=rs2)
                        V('reciprocal', [hs.k()], [hs.k()], out=rden2, in_=den2)
                        if a > 0:
                            act(lnd2, den2, AF.Ln, [hs.k()], [hs.k()])
                            V('tensor_tensor', [hs.k()], [lt.k()], out=lt.ap[:, hsl], in0=lnd2, in1=negm2, op=ALU.subtract)

                    def s2a():
                        for j in range(2):
                            for bi in range(nbk):
                                tp(psb[bP][:, (j * nbk + bi) * 128:(j * nbk + bi + 1) * 128], pb3[:, j, bi * 128:(bi + 1) * 128],
                                   identb.ap, [pb_.k(), identb.k()], [pT])
                        evac(0, ptb.ap[:, 0:2 * nkeys], psb[bP][:, 0:2 * nkeys], [pT], [ptb.k()])

                    def s2c():
                        for j, (qci, kci, base, kvh) in enumerate(heads):
                            for bi, vix in enumerate(vt_idx):
                                mm(ps[bS][:, j * 128:j * 128 + 64], ptb.ap[:, (j * nbk + bi) * 128:(j * nbk + bi + 1) * 128],
                                   Vt.ap[:, vix, kvh, 0:64], bi == 0, bi == len(vt_idx) - 1, [ptb.k(), Vt.k(vix)], [pS])

                    def s3():
                        po3 = ps[bS][:, 0:256].rearrange("p (j k) -> p j k", j=2)
                        V('tensor_tensor', [pS, hs.k()], [ot.k()], out=ot.ap[:, 0:hq * 64].rearrange("p (j k) -> p j k", j=hq)[:, hsl, :],
                          in0=po3[:, :, 0:64], in1=rden2.unsqueeze(2).to_broadcast([128, 2, 64]), op=ALU.mult)
                        if last:
                            for c in range(nq // 128):
                                tp(psb[3][:, c * 128:(c + 1) * 128], ot.ap[:, c * 128:(c + 1) * 128], identb.ap,
                                   [ot.k(), identb.k()], ['ps3'])
                            for c in range(nq // 128):
                                evac(c + idx, oT[a].ap[:, c, t0:t1:d], psb[3][:, c * 128:(c + 1) * 128], ['ps3'], [oT[a].k(idx)])
                            if a > 0:
                                tp(ps[2][0:4, 0:128], lt.ap[:, 0:4], identf.ap, [lt.k(), identf.k()], ['ps2'])
                                V('tensor_copy', ['ps2'], [lseT.k(a, idx)], out=lseT.ap[0:4, a - 1, t0:t1:d], in_=ps[2][0:4, 0:128])
                    return s1a, s1b, s2a, s2b, s2c, s3

                units = []
                for r_ in range(d):
                    for n in range(nb):
                        idx = r_ * nb + n
                        t0 = r_ + d * 128 * n
                        t1 = t0 + d * 127 + 1
                        if n > 0:
                            k0, nkeys, boff, vt_idx = t0 - d * 128, 256, 0, [idx - 1, idx]
                        else:
                            k0, nkeys, boff, vt_idx = t0, 128, 128, [idx]
                        for pi_, (h0, hstep) in enumerate(pairs):
                            units.append(make_unit(len(units), idx, t0, t1, k0, nkeys, boff, vt_idx, o_tok[idx % 2], lse_tok[idx % 2],
                                                   h0, hstep, pi_ == len(pairs) - 1))
                nu = len(units)
                for ui in range(nu + 2):
                    if ui < nu:
                        units[ui][0]()
                    if 1 <= ui <= nu:
                        units[ui - 1][2]()
                    if ui < nu:
                        units[ui][1]()
                    if 1 <= ui <= nu:
                        units[ui - 1][3]()
                        units[ui - 1][4]()
                    if ui >= 2:
                        units[ui - 2][5]()
                chk('B%d_%dp' % (p, a))
                if has_s:
                    RS = Region(A, RS_LO, END)
                    hsrep = RS.alloc("hsrep", [128, 8, 128], BF16)
                    zrep = RS.alloc("zrep", [128, 768], F32)
                    Ks = RS.alloc("Ks", [128, 16, nk], F32)
                    Vs = RS.alloc("Vs", [128, 16, nk], F32)
                    prod = RS.alloc("prod", [128, 16, 256], F32)
                    Ss = RS.alloc("Ss", [128, 16, hq], F32)
                    sbs = RS.alloc("sbs", [128, 16, hq], F32)
                    sm = RS.alloc("sm", [128, 160], F32)
                    opart = RS.alloc("opart", [128, 512], F32)
                    pi = RS.alloc("pi", [128, 2], I32)
                    V('tensor_copy', [hT.k(4)], [hsrep.k()], out=hsrep.ap.rearrange("p c (kg b) -> p c kg b", kg=8),
                      in_=hT.ap[:, :, SEQ:TC].unsqueeze(2).to_broadcast([128, 8, 8, 16]))
                    for (c0_, nn, bk) in ((0, 512, 0), (512, 256, 1)):
                        for kc in range(8):
                            mm(ps[bk][:, 0:nn], hsrep.ap[:, kc, :], wblk.ap[:, kc, c0_:c0_ + nn], kc == 0, kc == 7,
                               [hsrep.k(), wblk.k()], ['ps%d' % bk])
                        evac(bk, zrep.ap[:, c0_:c0_ + nn], ps[bk][:, 0:nn], ['ps%d' % bk], [zrep.k()])
                    ok = 'stsnew%d' % a
                    dstn = st_s[a][:, W - 1].rearrange("b t h d -> b (t h d)")
                    DM('sp', dstn, zrep.ap[0:16, ko_:ko_ + 2 * nk], r=[zrep.k()], w=[ok])
                    outkeys.append(ok)
                    for kg in range(8):
                        r0 = kg * 16 * d
                        srcK = caches[a][:, r0:r0 + 15 * d + 1:d, 0].rearrange("b j h d -> b j (h d)")
                        srcV = caches[a][:, r0:r0 + 15 * d + 1:d, 1].rearrange("b j h d -> b j (h d)")
                        DM('sp', Ks.ap[kg * 16:(kg + 1) * 16], srcK, w=[Ks.k(kg)])
                        DM('sp', Vs.ap[kg * 16:(kg + 1) * 16], srcV, w=[Vs.k(kg)])
                    Kk = [Ks.k(kg) for kg in range(8)]
                    Vk = [Vs.k(kg) for kg in range(8)]
                    PL('iota', [], [pi.k()], pi.ap[:, 0:1], pattern=[[0, 1]], base=0, channel_multiplier=1)
                    V('tensor_single_scalar', [pi.k()], [pi.k()], out=pi.ap[:, 1:2], in_=pi.ap[:, 0:1], scalar=4, op=ALU.arith_shift_right)
                    kgf = sm.ap[:, 150:151]
                    V('tensor_copy', [pi.k()], [sm.k('kg')], out=kgf, in_=pi.ap[:, 1:2])
                    dist = prod.ap[:, 0, 0:16]
                    PL('iota', [], [prod.k()], dist, pattern=[[-1, 16]], base=128, channel_multiplier=0,
                       allow_small_or_imprecise_dtypes=True)
                    V('tensor_scalar', [sm.k('kg')], [sm.k('kg')], out=kgf, in0=kgf, scalar1=16.0, scalar2=None, op0=ALU.mult)
                    V('tensor_scalar', [prod.k(), sm.k('kg')], [prod.k()], out=dist, in0=dist, scalar1=kgf, scalar2=None, op0=ALU.subtract)
                    for h in range(hq):
                        V('tensor_scalar', [prod.k()], [sbs.k()], out=sbs.ap[:, :, h], in0=dist, scalar1=-slopes[h] * d, scalar2=None,
                          op0=ALU.mult)
                    snew, lm, negM, enew, lsum, tot, denr, rdn, lnd_s = (sm.ap[:, i * 8:i * 8 + hq] for i in range(9))
                    smk = sm.k('s')
                    qz = zrep.ap[:, 0:nq]
                    kz = zrep.ap[:, ko_:ko_ + nk]
                    vz = zrep.ap[:, vo_:vo_ + nk]
                    if a == 0:
                        for kv in range(2):
                            q4 = qz.rearrange("p (g two d) -> p two g d", g=4, two=2, d=64)[:, kv]
                            V('tensor_tensor', Kk + [zrep.k(), sbs.k()], [prod.k()], out=prod.ap.rearrange("p k (h d) -> p k h d", h=4),
                              in0=Ks.ap[:, :, kv * 64:(kv + 1) * 64].unsqueeze(2).to_broadcast([128, 16, 4, 64]),
                              in1=q4.unsqueeze(1).to_broadcast([128, 16, 4, 64]), op=ALU.mult)
                            V('tensor_reduce', [prod.k()], [Ss.k()], out=Ss.ap[:, :, kv * 4:(kv + 1) * 4],
                              in_=prod.ap.rearrange("p k (h d) -> p k h d", h=4), axis=AX.X, op=ALU.add)
                            V('tensor_tensor', [zrep.k(), Ss.k()], [prod.k()], out=prod.ap[:, 0, :].rearrange("p (h d) -> p h d", h=4),
                              in0=q4, in1=kz[:, kv * 64:(kv + 1) * 64].unsqueeze(1).to_broadcast([128, 4, 64]), op=ALU.mult)
                            V('tensor_reduce', [prod.k()], [smk], out=snew[:, kv * 4:(kv + 1) * 4],
                              in_=prod.ap[:, 0, :].rearrange("p (h d) -> p h d", h=4), axis=AX.X, op=ALU.add)
                    else:
                        V('tensor_tensor', Kk + [zrep.k(), sbs.k()], [prod.k()], out=prod.ap, in0=Ks.ap,
                          in1=qz.unsqueeze(1).to_broadcast([128, 16, 256]), op=ALU.mult)
                        V('tensor_reduce', [prod.k()], [Ss.k()], out=Ss.ap, in_=prod.ap.rearrange("p k (h d) -> p k h d", h=4),
                          axis=AX.X, op=ALU.add)
                        V('tensor_tensor', [zrep.k(), Ss.k()], [prod.k()], out=prod.ap[:, 0, :], in0=qz, in1=kz, op=ALU.mult)
                        V('tensor_reduce', [prod.k()], [smk], out=snew, in_=prod.ap[:, 0, :].rearrange("p (h d) -> p h d", h=4),
                          axis=AX.X, op=ALU.add)
                    V('scalar_tensor_tensor', [Ss.k(), sbs.k()], [Ss.k()], out=Ss.ap, in0=Ss.ap, scalar=0.125, in1=sbs.ap,
                      op0=ALU.mult, op1=ALU.add)
                    V('tensor_scalar', [smk], [smk], out=snew, in0=snew, scalar1=0.125, scalar2=None, op0=ALU.mult)
                    V('tensor_reduce', [Ss.k()], [smk], out=lm, in_=Ss.ap.rearrange("p k h -> p h k"), axis=AX.X, op=ALU.max)
                    V('tensor_tensor', [smk], [smk], out=lm, in0=lm, in1=snew, op=ALU.max)
                    if a == 0:
                        V('tensor_tensor', [smk, sink.k()], [smk], out=lm, in0=lm, in1=sink.ap, op=ALU.max)
                    tp(ps[2][0:hq, 0:128], lm, identf.ap, [smk, identf.k()], ['ps2'])
                    gm = opart.ap[0:hq, 0:16]
                    V('tensor_reduce', ['ps2'], [opart.k()], out=gm, in_=ps[2][0:hq, 0:128].rearrange("h (kg b) -> h b kg", kg=8),
                      axis=AX.X, op=ALU.max)
                    gmr = opart.ap[0:hq, 128:256]
                    V('tensor_copy', [opart.k()], [opart.k()], out=gmr.rearrange("h (kg b) -> h kg b", kg=8),
                      in_=gm.unsqueeze(1).to_broadcast([hq, 8, 16]))
                    tp(ps[2][:, 256:256 + hq], gmr, identf.ap[0:hq, 0:hq], [opart.k(), identf.k()], ['ps2'])
                    V('tensor_scalar', ['ps2'], [smk], out=negM, in0=ps[2][:, 256:256 + hq], scalar1=-1.0, scalar2=None, op0=ALU.mult)
                    V('tensor_tensor', [Ss.k(), smk], [Ss.k()], out=Ss.ap, in0=Ss.ap, in1=negM.unsqueeze(1).to_broadcast([128, 16, hq]),
                      op=ALU.add)
                    act(Ss.ap, Ss.ap, AF.Exp, [Ss.k()], [Ss.k()])
                    V('tensor_tensor', [smk], [smk], out=enew, in0=snew, in1=negM, op=ALU.add)
                    act(enew, enew, AF.Exp, [smk], [smk])
                    V('tensor_reduce', [Ss.k()], [smk], out=lsum, in_=Ss.ap.rearrange("p k h -> p h k"), axis=AX.X, op=ALU.add)
                    V('tensor_scalar', [smk], [smk], out=enew, in0=enew, scalar1=0.125, scalar2=None, op0=ALU.mult)
                    V('tensor_tensor', [smk], [smk], out=tot, in0=lsum, in1=enew, op=ALU.add)
                    if a == 0:
                        esk = sm.ap[:, 80:88]
                        V('tensor_tensor', [smk, sink.k()], [smk], out=esk, in0=sink.ap, in1=negM, op=ALU.add)
                        act(esk, esk, AF.Exp, [smk], [smk])
                        V('scalar_tensor_tensor', [smk], [smk], out=tot, in0=esk, scalar=0.125, in1=tot, op0=ALU.mult, op1=ALU.add)
                    mm(ps[3][:, 0:hq], Gm.ap, tot, True, True, [Gm.k(), smk], ['ps3'])
                    V('tensor_copy', ['ps3'], [smk], out=denr, in_=ps[3][:, 0:hq])
                    V('reciprocal', [smk], [smk], out=rdn, in_=denr)
                    if a == 0:
                        for kv in range(2):
                            V('tensor_tensor', Vk + [Ss.k(), smk], [prod.k()], out=prod.ap.rearrange("p k (h d) -> p k h d", h=4),
                              in0=Vs.ap[:, :, kv * 64:(kv + 1) * 64].unsqueeze(2).to_broadcast([128, 16, 4, 64]),
                              in1=Ss.ap[:, :, kv * 4:(kv + 1) * 4].unsqueeze(3).to_broadcast([128, 16, 4, 64]), op=ALU.mult)
                            V('tensor_reduce', [prod.k()], [opart.k()], out=opart.ap[:, kv * 256:(kv + 1) * 256],
                              in_=prod.ap.rearrange("p k f -> p f k"), axis=AX.X, op=ALU.add)
                            V('tensor_tensor', [zrep.k(), smk, opart.k()], [prod.k()], out=prod.ap[:, 0, :].rearrange("p (h d) -> p h d", h=4),
                              in0=vz[:, kv * 64:(kv + 1) * 64].unsqueeze(1).to_broadcast([128, 4, 64]),
                              in1=enew[:, kv * 4:(kv + 1) * 4].unsqueeze(2).to_broadcast([128, 4, 64]), op=ALU.mult)
                            V('tensor_tensor', [prod.k(), opart.k()], [opart.k()], out=opart.ap[:, kv * 256:(kv + 1) * 256],
                              in0=opart.ap[:, kv * 256:(kv + 1) * 256], in1=prod.ap[:, 0, :], op=ALU.add)
                    else:
                        V('tensor_tensor', Vk + [Ss.k(), smk], [prod.k()], out=prod.ap.rearrange("p k (h d) -> p k h d", h=4),
                          in0=Vs.ap.rearrange("p k (h d) -> p k h d", h=4), in1=Ss.ap.unsqueeze(3).to_broadcast([128, 16, 4, 64]),
                          op=ALU.mult)
                        V('tensor_reduce', [prod.k()], [opart.k()], out=opart.ap[:, 0:256], in_=prod.ap.rearrange("p k f -> p f k"),
                          axis=AX.X, op=ALU.add)
                        V('tensor_tensor', [zrep.k(), smk, opart.k()], [prod.k()], out=prod.ap[:, 0, :].rearrange("p (h d) -> p h d", h=4),
                          in0=vz.rearrange("p (h d) -> p h d", h=4), in1=enew.unsqueeze(2).to_broadcast([128, 4, 64]), op=ALU.mult)
                        V('tensor_tensor', [prod.k(), opart.k()], [opart.k()], out=opart.ap[:, 0:256], in0=opart.ap[:, 0:256],
                          in1=prod.ap[:, 0, :], op=ALU.add)
                    mm(ps[0][:, 0:nq], Gm.ap, opart.ap[:, 0:nq], True, True, [Gm.k(), opart.k()], ['ps0'])
                    ots = o_tok[0]
                    V('tensor_tensor', ['ps0', smk], [ots.k()], out=ots.ap[0:16, 0:nq].rearrange("p (h d) -> p h d", h=hq),
                      in0=ps[0][0:16, 0:nq].rearrange("p (h d) -> p h d", h=hq),
                      in1=rdn[0:16, :].unsqueeze(2).to_broadcast([16, hq, 64]), op=ALU.mult)
                    for c in range(nq // 128):
                        tp(psb[3][:, c * 16:(c + 1) * 16], ots.ap[0:16, c * 128:(c + 1) * 128], identb.ap[0:16, 0:16],
                           [ots.k(), identb.k()], ['ps3'])
                    V('tensor_copy', ['ps3'], [oT[a].k('s')], out=oT[a].ap[:, :, SEQ:TC],
                      in_=psb[3][:, 0:(nq // 128) * 16].rearrange("p (c s) -> p c s", c=nq // 128))
                    if a > 0:
                        act(lnd_s, denr, AF.Ln, [smk], [smk])
                        lts = lse_tok[0]
                        V('tensor_tensor', [smk], [lts.k()], out=lts.ap[0:16, 0:4], in0=lnd_s[0:16, :], in1=negM[0:16, :], op=ALU.subtract)
                        tp(ps[2][0:4, 0:16], lts.ap[0:16, 0:4], identf.ap[0:16, 0:16], [lts.k(), identf.k()], ['ps2'])
                        V('tensor_copy', ['ps2'], [lseT.k(a, 's')], out=lseT.ap[0:4, a - 1, SEQ:TC], in_=ps[2][0:4, 0:16])

            chk('B%d' % p)
            GT_LO = R1_LO + 17 * 4096
            gT = A.carve("gT", GT_LO, [128, 8, TC], BF16)
            RM = Region(A, GT_LO + 8 * TC * 2, END)
            wbs = RM.alloc("wbs", [128, 4, 1024], BF16)
            wbd = RM.alloc("wbd", [128, 2, 1024], BF16)
            wga = RM.alloc("wga", [128, 8, 512], BF16)
            wgb = RM.alloc("wgb", [128, 8, 512], BF16)
            cM = RM.alloc("cM", [4, 512], F32)
            cS = RM.alloc("cS", [4, 512], F32)
            tf = [RM.alloc("tf%d" % i, [128, 512], F32) for i in range(4)]
            DM('pool', wbs.ap, w_br_swa.rearrange("(k p) n -> p k n", p=128), w=[wbs.k()])
            DM('pool', wbd.ap, w_br_dil.rearrange("(k p) n -> p k n", p=128), w=[wbd.k()])
            for ti, (c0, wd) in enumerate(mtiles):
                lk = [lseT.k()]
                L = lseT.ap[0:4, :, c0:c0 + wd]
                lse_keys = []
                for a_ in (1, 2, 3):
                    dd = TYPES[a_]['d']
                    nbb = 16 // dd
                    if c0 >= SEQ:
                        lse_keys.append(lseT.k(a_, 's'))
                    else:
                        for r_ in range(dd):
                            for n in range(nbb):
                                t0 = r_ + dd * 128 * n
                                if t0 < c0 + wd and t0 + dd * 127 >= c0:
                                    lse_keys.append(lseT.k(a_, r_ * nbb + n))
                ck = lseT.k('c', ti)
                V('tensor_tensor', lse_keys, [cM.k()], out=cM.ap[:, 0:wd], in0=L[:, 0, :], in1=L[:, 1, :], op=ALU.max)
                V('tensor_tensor', lse_keys + [cM.k()], [cM.k()], out=cM.ap[:, 0:wd], in0=cM.ap[:, 0:wd], in1=L[:, 2, :], op=ALU.max)
                V('tensor_tensor', lse_keys + [cM.k()], [ck], out=L, in0=L, in1=cM.ap[:, 0:wd].unsqueeze(1).to_broadcast([4, 3, wd]),
                  op=ALU.subtract)
                act(L, L, AF.Exp, [ck], [ck])
                V('tensor_tensor', [ck], [cS.k()], out=cS.ap[:, 0:wd], in0=L[:, 0, :], in1=L[:, 1, :], op=ALU.add)
                V('tensor_tensor', [ck, cS.k()], [cS.k()], out=cS.ap[:, 0:wd], in0=cS.ap[:, 0:wd], in1=L[:, 2, :], op=ALU.add)
                V('reciprocal', [cS.k()], [cS.k()], out=cS.ap[:, 0:wd], in_=cS.ap[:, 0:wd])
                V('tensor_tensor', [ck, cS.k()], [ck], out=L, in0=L, in1=cS.ap[:, 0:wd].unsqueeze(1).to_broadcast([4, 3, wd]), op=ALU.mult)

                def okeys(a_):
                    dd = TYPES[a_]['d']
                    nbb = 16 // dd
                    if c0 >= SEQ:
                        return [oT[a_].k('s')]
                    out = []
                    for r_ in range(dd):
                        for n in range(nbb):
                            t0 = r_ + dd * 128 * n
                            if t0 < c0 + wd and t0 + dd * 127 >= c0:
                                out.append(oT[a_].k(r_ * nbb + n))
                    return out
                for c in range(2):
                    for g in range(3):
                        mm(ps[g][:, 0:wd], selH[c].ap, lseT.ap[0:4, g, c0:c0 + wd], True, True, [selH[c].k(), ck], ['ps%d' % g])
                    t0_, t1_ = tf[0], tf[1]
                    V('tensor_tensor', okeys(1) + ['ps0'], [t0_.k()], out=t0_.ap[:, 0:wd], in0=oT[1].ap[:, c, c0:c0 + wd], in1=ps[0][:, 0:wd], op=ALU.mult)
                    V('tensor_tensor', okeys(2) + ['ps1'], [t1_.k()], out=t1_.ap[:, 0:wd], in0=oT[2].ap[:, c, c0:c0 + wd], in1=ps[1][:, 0:wd], op=ALU.mult)
                    V('tensor_tensor', [t0_.k(), t1_.k()], [t0_.k()], out=t0_.ap[:, 0:wd], in0=t0_.ap[:, 0:wd], in1=t1_.ap[:, 0:wd], op=ALU.add)
                    V('tensor_tensor', okeys(3) + ['ps2'], [t1_.k()], out=t1_.ap[:, 0:wd], in0=oT[3].ap[:, c, c0:c0 + wd], in1=ps[2][:, 0:wd], op=ALU.mult)
                    V('tensor_tensor', [t0_.k(), t1_.k()] + okeys(1), [oT[1].k('comb', ti, c)], out=oT[1].ap[:, c, c0:c0 + wd],
                      in0=t0_.ap[:, 0:wd], in1=t1_.ap[:, 0:wd], op=ALU.add)

            def swa_keys(c0, wd):
                if c0 >= SEQ:
                    return [oT[0].k('s')]
                return [oT[0].k(t) for t in range(c0 // 128, (c0 + wd - 1) // 128 + 1)]

            it = 0
            for q4 in range(2):
                DM('pool', wga.ap, w_in[:, 3072 + q4 * 512:3072 + (q4 + 1) * 512].rearrange("(k p) n -> p k n", p=128), w=[wga.k()])
                DM('pool', wgb.ap, w_in[:, 4096 + q4 * 512:4096 + (q4 + 1) * 512].rearrange("(k p) n -> p k n", p=128), w=[wgb.k()])
                for ti, (c0, wd) in enumerate(mtiles):
                    for oc4 in range(4):
                        oc = q4 * 4 + oc4
                        b0 = (it % 2) * 4
                        it += 1
                        pk = ['ps%d' % (b0 + i) for i in range(4)]
                        for kc in range(8):
                            mm(ps[b0][:, 0:wd], wga.ap[:, kc, oc4 * 128:(oc4 + 1) * 128], hT.ap[:, kc, c0:c0 + wd], kc == 0, kc == 7,
                               [wga.k()] + hk(c0, c0 + wd), [pk[0]])
                        for kc in range(8):
                            mm(ps[b0 + 1][:, 0:wd], wgb.ap[:, kc, oc4 * 128:(oc4 + 1) * 128], hT.ap[:, kc, c0:c0 + wd], kc == 0, kc == 7,
                               [wgb.k()] + hk(c0, c0 + wd), [pk[1]])
                        for k4 in range(4):
                            mm(ps[b0 + 2][:, 0:wd], wbs.ap[:, k4, oc * 128:(oc + 1) * 128], oT[0].ap[:, k4, c0:c0 + wd], k4 == 0, k4 == 3,
                               [wbs.k()] + swa_keys(c0, wd), [pk[2]])
                        for k2 in range(2):
                            mm(ps[b0 + 3][:, 0:wd], wbd.ap[:, k2, oc * 128:(oc + 1) * 128], oT[1].ap[:, k2, c0:c0 + wd], k2 == 0, k2 == 1,
                               [wbd.k(), oT[1].k('comb', ti, 0), oT[1].k('comb', ti, 1)], [pk[3]])
                        sa, sb2, ta, tb2 = tf[0], tf[1], tf[2], tf[3]
                        act(sa.ap[:, 0:wd], ps[b0][:, 0:wd], AF.Sigmoid, [pk[0]], [sa.k()])
                        act(sb2.ap[:, 0:wd], ps[b0 + 1][:, 0:wd], AF.Sigmoid, [pk[1]], [sb2.k()])
                        V('tensor_tensor', [sa.k(), pk[2]], [ta.k()], out=ta.ap[:, 0:wd], in0=sa.ap[:, 0:wd], in1=ps[b0 + 2][:, 0:wd], op=ALU.mult)
                        V('tensor_tensor', [sb2.k(), pk[3]], [tb2.k()], out=tb2.ap[:, 0:wd], in0=sb2.ap[:, 0:wd], in1=ps[b0 + 3][:, 0:wd], op=ALU.mult)
                        V('tensor_tensor', [ta.k(), tb2.k()], [gT.k(oc, ti)], out=gT.ap[:, oc, c0:c0 + wd], in0=ta.ap[:, 0:wd],
                          in1=tb2.ap[:, 0:wd], op=ALU.add)

            chk('M1_%d' % p)
            Yacc = A.carve("Yacc", R1_LO, [128, 17, 1024], F32)
            RM2 = Region(A, GT_LO + 8 * TC * 2, END)
            gbc = RM2.alloc("gbc", [128, 2, 1024], F32)
            lnbc = RM2.alloc("lnbc", [128, 2, 1024], F32)
            xt2 = RM2.alloc("xt2", [128, 1024], F32)
            xm = RM2.alloc("xm", [128, 1024], F32)
            wts = RM2.alloc("wts", [128, 17, 64], F32)
            RE2_LO = RM2.p
            wout = RM2.alloc("wout", [128, 8, 1024], BF16)
            h2f = RM2.alloc("h2f", [128, 8, 128], F32)
            wr = RM2.alloc("wr", [128, 8, 64], F32)
            rbias = RM2.alloc("rbias", [128, 64], F32)
            rt = RM2.alloc("rt", [128, 6, 64], F32)
            DM('pool', wout.ap, w_out.rearrange("(k p) n -> p k n", p=128), w=[wout.k()])
            DM('sp', lnbc.ap[:, 0, :], ln1_g.to_broadcast([128, 1024]), w=[lnbc.k()])
            DM('sp', lnbc.ap[:, 1, :], ln1_b.to_broadcast([128, 1024]), w=[lnbc.k()])
            DM('sp', wr.ap, w_router.rearrange("(k p) n -> p k n", p=128), w=[wr.k()])
            DM('sp', rbias.ap, router_bias.to_broadcast([128, 64]), w=[rbias.k()])
            for n4 in range(4):
                mm(ps[n4][:, :], selP[p].ap, modg.ap[0:18, n4 * 512:(n4 + 1) * 512], True, True, [selP[p].k(), modg.k()], ['ps%d' % n4])
                evac(n4, gbc.ap[:, n4 // 2, (n4 % 2) * 512:(n4 % 2 + 1) * 512], ps[n4][:, :], ['ps%d' % n4], [gbc.k()])
            for t, (c0, P_) in enumerate(stiles):
                smp = (c0 >= SEQ)
                ti = 4 if smp else c0 // 512
                if smp:
                    DM('sp', xt2.ap[0:16, :], xs[:, :], w=[xt2.k()])
                else:
                    DM('sp', xt2.ap, xp[p, c0:c0 + 128, :], w=[xt2.k()])
                b0 = (t % 2) * 4
                for nh in range(2):
                    for kc in range(8):
                        mm(ps[b0 + nh][0:P_, :], gT.ap[:, kc, c0:c0 + P_], wout.ap[:, kc, nh * 512:(nh + 1) * 512], kc == 0, kc == 7,
                           [gT.k(kc, ti), wout.k()], ['ps%d' % (b0 + nh)])
                    g1 = modg.ap[0:16, nh * 512:(nh + 1) * 512] if smp else gbc.ap[:, 0, nh * 512:(nh + 1) * 512]
                    V('tensor_tensor', ['ps%d' % (b0 + nh), gbc.k(), modg.k()], [xm.k()], out=xm.ap[0:P_, nh * 512:(nh + 1) * 512],
                      in0=ps[b0 + nh][0:P_, :], in1=g1, op=ALU.mult)
                V('scalar_tensor_tensor', [xt2.k(), xm.k()], [xm.k()], out=xm.ap[0:P_, :], in0=xt2.ap[0:P_, :], scalar=ALPHA,
                  in1=xm.ap[0:P_, :], op0=ALU.mult, op1=ALU.add)
                rstd, nmr, k = ln_stats(xm.ap[0:P_, :], P_, [xm.k()])
                act(xm.ap[0:P_, :], xm.ap[0:P_, :], AF.Identity, [xm.k(), k], [xm.k()], bias=nmr, scale=rstd)
                V('tensor_tensor', [xm.k(), lnbc.k()], [xm.k()], out=xm.ap[0:P_, :], in0=xm.ap[0:P_, :], in1=lnbc.ap[0:P_, 0, :], op=ALU.mult)
                V('tensor_tensor', [xm.k(), lnbc.k()], [xm.k()], out=xm.ap[0:P_, :], in0=xm.ap[0:P_, :], in1=lnbc.ap[0:P_, 1, :], op=ALU.add)
                act(Yacc.ap[0:P_, t, :], xm.ap[0:P_, :], AF.Identity, [xm.k()], [Yacc.k(t)], scale=ALPHA)
                rstd, nmr, k = ln_stats(xm.ap[0:P_, :], P_, [xm.k()])
                act(xt2.ap[0:P_, :], xm.ap[0:P_, :], AF.Identity, [xm.k(), k], [xt2.k()], bias=nmr, scale=rstd)
                for c in range(8):
                    tp(ps[b0 + 2 + c // 4][:, (c % 4) * 128:(c % 4) * 128 + P_], xt2.ap[0:P_, c * 128:(c + 1) * 128],
                       identf.ap[0:P_, 0:P_], [xt2.k(), identf.k()], ['ps%d' % (b0 + 2 + c // 4)])
                for c in range(8):
                    src = ps[b0 + 2 + c // 4][:, (c % 4) * 128:(c % 4) * 128 + P_]
                    pk = 'ps%d' % (b0 + 2 + c // 4)
                    if smp:
                        V('tensor_tensor', [pk, opsc2T.k()], [h2f.k(c)], out=h2f.ap[:, c, 0:16], in0=src, in1=opsc2T.ap[:, c, 0:16], op=ALU.mult)
                        V('tensor_tensor', [h2f.k(c), modT.k()], [h2f.k(c)], out=h2f.ap[:, c, 0:16], in0=h2f.ap[:, c, 0:16],
                          in1=modT.ap[:, 24 + c, 0:16], op=ALU.add)
                    else:
                        sc_ = opsc2T.ap[:, c, pcol:pcol + 1]
                        sh_ = modT.ap[:, 24 + c, pcol:pcol + 1]
                        if c % 2 == 0:
                            act(h2f.ap[:, c, :], src, AF.Identity, [pk, opsc2T.k(), modT.k()], [h2f.k(c)], bias=sh_, scale=sc_)
                        else:
                            V('tensor_scalar', [pk, opsc2T.k(), modT.k()], [h2f.k(c)], out=h2f.ap[:, c, :], in0=src, scalar1=sc_,
                              scalar2=sh_, op0=ALU.mult, op1=ALU.add)
                h2k = [h2f.k(c) for c in range(8)]
                V('tensor_copy', h2k + [gT.k(kc, ti) for kc in range(0)], [hT.k(ti)], out=hT.ap[:, :, c0:c0 + P_], in_=h2f.ap[:, :, 0:P_])
                pr = 'ps%d' % (b0 + 2)
                for c in range(8):
                    mm(ps[b0 + 2][0:P_, 0:64], h2f.ap[:, c, 0:P_], wr.ap[:, c, :], c == 0, c == 7, h2k + [wr.k()], [pr])
                sc = rt.ap[0:P_, 0, :]
                bi = rt.ap[0:P_, 1, :]
                eq = rt.ap[0:P_, 2, :]
                msk = rt.ap[0:P_, 3, :]
                m1 = rt.ap[0:P_, 4, 0:8]
                m2 = rt.ap[0:P_, 4, 8:16]
                gs = rt.ap[0:P_, 4, 16:24]
                gs8 = rt.ap[0:P_, 4, 24:32]
                gmk = rt.ap[0:P_, 4, 32:40]
                s8 = rt.ap[0:P_, 4, 40:48]
                wsum = rt.ap[0:P_, 4, 48:49]
                rk_ = rt.k()
                act(sc, ps[b0 + 2][0:P_, 0:64], AF.Sigmoid, [pr], [rk_])
                V('tensor_tensor', [rk_, rbias.k()], [rk_], out=bi, in0=sc, in1=rbias.ap[0:P_, :], op=ALU.add)
                bi3 = bi.rearrange("p (g e) -> p g e", g=8)
                V('tensor_reduce', [rk_], [rk_], out=m1, in_=bi3, axis=AX.X, op=ALU.max)
                V('tensor_tensor', [rk_], [rk_], out=eq.rearrange("p (g e) -> p g e", g=8), in0=bi3,
                  in1=m1.unsqueeze(2).to_broadcast([P_, 8, 8]), op=ALU.is_equal)
                V('scalar_tensor_tensor', [rk_], [rk_], out=eq, in0=eq, scalar=-1e9, in1=bi, op0=ALU.mult, op1=ALU.add)
                V('tensor_reduce', [rk_], [rk_], out=m2, in_=eq.rearrange("p (g e) -> p g e", g=8), axis=AX.X, op=ALU.max)
                V('tensor_tensor', [rk_], [rk_], out=gs, in0=m1, in1=m2, op=ALU.add)
                V('max', [rk_], [rk_], out=gs8, in_=gs)
                V('tensor_scalar', [rk_], [rk_], out=gmk, in0=gs, scalar1=gs8[:, 3:4], scalar2=None, op0=ALU.is_ge)
                V('tensor_scalar', [rk_], [rk_], out=gmk, in0=gmk, scalar1=1e9, scalar2=-1e9, op0=ALU.mult, op1=ALU.add)
                V('tensor_tensor', [rk_], [rk_], out=msk.rearrange("p (g e) -> p g e", g=8), in0=bi3,
                  in1=gmk.unsqueeze(2).to_broadcast([P_, 8, 8]), op=ALU.add)
                V('max', [rk_], [rk_], out=s8, in_=msk)
                V('tensor_scalar', [rk_], [rk_], out=msk, in0=msk, scalar1=s8[:, 7:8], scalar2=None, op0=ALU.is_ge)
                V('tensor_tensor', [rk_], [rk_], out=msk, in0=msk, in1=sc, op=ALU.mult)
                V('tensor_reduce', [rk_], [rk_], out=wsum, in_=msk, axis=AX.X, op=ALU.add)
                V('reciprocal', [rk_], [rk_], out=wsum, in_=wsum)
                V('tensor_scalar', [rk_], [wts.k(t)], out=wts.ap[0:P_, t, :], in0=msk, scalar1=wsum, scalar2=2.5, op0=ALU.mult, op1=ALU.mult)

            chk('M2_%d' % p)
            RE = Region(A, GT_LO, GT_LO + 8 * TC * 2)
            Wg = [RE.alloc("Wg%d" % i, [128, 8, 256], BF16) for i in range(2)]
            Wu = [RE.alloc("Wu%d" % i, [128, 8, 256], BF16) for i in range(2)]
            Wd = [RE.alloc("Wd%d" % i, [128, 2, 1024], BF16) for i in range(2)]
            sg = [RE.alloc("sg%d" % i, [128, 512], F32) for i in range(2)]
            Hb = [RE.alloc("Hb%d" % i, [128, 2, 512], BF16) for i in range(2)]
            RE2 = Region(A, RE2_LO, END)
            Wds = [RE2.alloc("Wds%d" % i, [128, 2, 1024], BF16) for i in range(2)]
            ytmp = RE2.alloc("ytmp", [16, 512], F32)
            hi = 0
            yi = 0
            for e_ in range(NEXP + 1):
                sl = e_ % 2
                if p == 0 and e_ == 3:
                    for a_, T_ in enumerate(TYPES):
                        W_ = T_['W']
                        src = caches[a_][:, 1:W_].rearrange("b w t h d -> b (w t h d)")
                        dst = st_s[a_][:, 0:W_ - 1].rearrange("b w t h d -> b (w t h d)")
                        DM('act', dst, src, w=['bulkout%d' % a_], bulk=True)
                        outkeys.append('bulkout%d' % a_)
                if e_ < NEXP:
                    srcs = (w_eg[e_], w_eu[e_], w_ed[e_])
                else:
                    srcs = (w_sg, w_su, w_sd)
                DM('pool', Wg[sl].ap, srcs[0].rearrange("(k p) n -> p k n", p=128), w=[Wg[sl].k()])
                DM('pool', Wu[sl].ap, srcs[1].rearrange("(k p) n -> p k n", p=128), w=[Wu[sl].k()])
                DM('pool', Wd[sl].ap, srcs[2].rearrange("(k p) n -> p k n", p=128), w=[Wd[sl].k()])
                V('tensor_tensor', [Wd[sl].k(), gbc.k()], [Wds[sl].k()], out=Wds[sl].ap, in0=Wd[sl].ap,
                  in1=gbc.ap[:, 1, :].unsqueeze(1).to_broadcast([128, 2, 1024]), op=ALU.mult)
                for ti, (c0, wd) in enumerate(mtiles):
                    smp = (c0 >= SEQ)
                    hb = Hb[hi % 2]
                    hi += 1
                    for oc in range(2):
                        pg, pu = 'ps%d' % (oc * 2), 'ps%d' % (oc * 2 + 1)
                        for kc in range(8):
                            mm(ps[oc * 2][:, 0:wd], Wg[sl].ap[:, kc, oc * 128:(oc + 1) * 128], hT.ap[:, kc, c0:c0 + wd], kc == 0, kc == 7,
                               [Wg[sl].k(), hT.k(ti)], [pg])
                        for kc in range(8):
                            mm(ps[oc * 2 + 1][:, 0:wd], Wu[sl].ap[:, kc, oc * 128:(oc + 1) * 128], hT.ap[:, kc, c0:c0 + wd], kc == 0, kc == 7,
                               [Wu[sl].k(), hT.k(ti)], [pu])
                        act(sg[oc].ap[:, 0:wd], ps[oc * 2][:, 0:wd], AF.Silu, [pg], [sg[oc].k()])
                        V('tensor_tensor', [sg[oc].k(), pu], [hb.k(oc)], out=hb.ap[:, oc, 0:wd], in0=sg[oc].ap[:, 0:wd], in1=ps[oc * 2 + 1][:, 0:wd],
                          op=ALU.mult)
                    nsub = 1 if smp else 4
                    for j in range(nsub):
                        t = 16 if smp else ti * 4 + j
                        P_ = 16 if smp else 128
                        wsc = 1.0 if e_ == NEXP else wts.ap[0:P_, t, e_:e_ + 1]
                        wk = [] if e_ == NEXP else [wts.k(t)]
                        for nh in range(2):
                            bk = 4 + yi % 4
                            yi += 1
                            py = 'ps%d' % bk
                            wdn = Wd[sl] if smp else Wds[sl]
                            for k2 in range(2):
                                mm(ps[bk][0:P_, :], hb.ap[:, k2, j * 128:j * 128 + P_], wdn.ap[:, k2, nh * 512:(nh + 1) * 512], k2 == 0, k2 == 1,
                                   [hb.k(0), hb.k(1), wdn.k()], [py])
                            ydst = Yacc.ap[0:P_, t, nh * 512:(nh + 1) * 512]
                            if smp:
                                V('tensor_tensor', [py, modg.k()], [ytmp.k()], out=ytmp.ap, in0=ps[bk][0:16, :],
                                  in1=modg.ap[0:16, 1024 + nh * 512:1024 + (nh + 1) * 512], op=ALU.mult)
                                V('scalar_tensor_tensor', [ytmp.k(), Yacc.k(t, nh), Yacc.k(t)] + wk, [Yacc.k(t, nh)], out=ydst, in0=ytmp.ap, scalar=wsc,
                                  in1=ydst, op0=ALU.mult, op1=ALU.add)
                            else:
                                V('scalar_tensor_tensor', [py, Yacc.k(t, nh), Yacc.k(t)] + wk, [Yacc.k(t, nh)], out=ydst, in0=ps[bk][0:P_, :],
                                  scalar=wsc, in1=ydst, op0=ALU.mult, op1=ALU.add)

            chk('E_%d' % p)
            DM('sp', lnbc.ap[:, 0, :], ln2_g.to_broadcast([128, 1024]), w=[lnbc.k()])
            DM('sp', lnbc.ap[:, 1, :], ln2_b.to_broadcast([128, 1024]), w=[lnbc.k()])
            obuf = [xt2, xm]
            for t, (c0, P_) in enumerate(stiles):
                smp = (c0 >= SEQ)
                yk = [Yacc.k(t), Yacc.k(t, 0), Yacc.k(t, 1)]
                ya = Yacc.ap[0:P_, t, :]
                rstd, nmr, k = ln_stats(ya, P_, yk)
                ob = obuf[t % 2]
                act(ob.ap[0:P_, :], ya, AF.Identity, yk + [k], [ob.k()], bias=nmr, scale=rstd)
                V('tensor_tensor', [ob.k(), lnbc.k()], [ob.k()], out=ob.ap[0:P_, :], in0=ob.ap[0:P_, :], in1=lnbc.ap[0:P_, 0, :], op=ALU.mult)
                V('tensor_tensor', [ob.k(), lnbc.k()], [ob.k()], out=ob.ap[0:P_, :], in0=ob.ap[0:P_, :], in1=lnbc.ap[0:P_, 1, :], op=ALU.add)
                ok = 'y_%d_%d' % (p, t)
                if smp:
                    DM('sp', ys[:, :], ob.ap[0:16, :], r=[ob.k()], w=[ok])
                else:
                    DM('sp', yp[p, c0:c0 + 128, :], ob.ap, r=[ob.k()], w=[ok])
                outkeys.append(ok)

        R.stopped = False
        R.op('sp', lambda e: None, r=outkeys)
        R.emit(nc, st)
    return nc, R


_CACHE = {}


def kernel(x_prompt, x_sample, cache_swa_kv, cache_dil1_kv, cache_dil2_kv, cache_dil3_kv, c_prompt, c_sample,
           w_ada, b_ada, w_in, attn_sinks, w_br_swa, w_br_dil, w_out, ln1_g, ln1_b, w_router, router_bias,
           w_exp_gate, w_exp_up, w_exp_down, w_sh_gate, w_sh_up, w_sh_down, ln2_g, ln2_b):
    f = lambda a_: np.ascontiguousarray(np.asarray(a_, dtype=np.float32))
    if 'nc' not in _CACHE:
        _CACHE['nc'] = build_nc()[0]
    nc = _CACHE['nc']
    cach = [f(cache_swa_kv)[0], f(cache_dil1_kv)[0], f(cache_dil2_kv)[0], f(cache_dil3_kv)[0]]
    xpr, xsa = f(x_prompt), f(x_sample)
    cp, cs_ = f(c_prompt), f(c_sample)
    shared = dict(
        w_ada=f(w_ada)[0], b_ada=f(b_ada), w_in=f(w_in)[0], sinks=f(attn_sinks), w_br_swa=f(w_br_swa)[0],
        w_br_dil=f(w_br_dil)[0], w_out=f(w_out)[0], ln1_g=f(ln1_g), ln1_b=f(ln1_b), w_router=f(w_router)[0],
        router_bias=f(router_bias), w_eg=f(w_exp_gate)[0], w_eu=f(w_exp_up)[0], w_ed=f(w_exp_down)[0],
        w_sg=f(w_sh_gate)[0], w_su=f(w_sh_up)[0], w_sd=f(w_sh_down)[0], ln2_g=f(ln2_g), ln2_b=f(ln2_b))
    in_maps = []
    for c in range(NCORES):
        m = dict(shared)
        m['xp'] = xpr[2 * c:2 * c + 2]
        m['xs'] = xsa[16 * c:16 * c + 16, 0]
        m['c_all'] = np.ascontiguousarray(np.concatenate([cs_[16 * c:16 * c + 16], cp[2 * c:2 * c + 2]], axis=0))
        for a in range(4):
            m['cache%d' % a] = cach[a][16 * c:16 * c + 16]
        in_maps.append(m)
    res = run_bass_kernel_spmd(nc, in_maps, core_ids=list(range(NCORES)))
    rs = res.results
    y_prompt = np.concatenate([r['yp'] for r in rs], axis=0)
    y_sample = np.concatenate([r['ys'] for r in rs], axis=0)[:, None, :]
    outs = [y_prompt, y_sample]
    for a in range(4):
        outs.append(np.concatenate([r['stp%d' % a] for r in rs], axis=0)[None])
    for a in range(4):
        outs.append(np.concatenate([r['sts%d' % a] for r in rs], axis=0)[None])
    return tuple(np.ascontiguousarray(o, dtype=np.float32) for o in outs)
```
